# Optimizing a Trainium2 kernel written in Bass

```python
import math
import jax, jax.numpy as jnp
from jax import lax
import numpy as np

D_MODEL = 1024
BATCH = 4
SEQ = 8192
DEPTH = 4

NSA_HEAD_DIM = 64
NSA_HEADS = D_MODEL // 128
NSA_KV_GROUPS = 2
GQA_RATIO = NSA_HEADS // NSA_KV_GROUPS
NSA_WIDTH = NSA_HEADS * NSA_HEAD_DIM
KV_WIDTH = NSA_KV_GROUPS * NSA_HEAD_DIM
CMP_BLOCK = 32
CMP_STRIDE = 16
CMP_HIDDEN = 128
SEL_BLOCK = 64
SEL_TOPN = 16
WINDOW = 512
Q_BLOCK = 64
FORCE_SCORE = 1.0e4
NEG_INF = -1.0e30
POOL_WINDOWS = (2, 4, 8, 16)
POOL_WIDTH = D_MODEL // 2
POOL_GROUP = POOL_WIDTH // len(POOL_WINDOWS)
SSM_WIDTH = D_MODEL // 2
SSM_GROUP = 16
SSM_GROUPS = SSM_WIDTH // SSM_GROUP
SSM_STATE = 64
N_BRANCH = 3
IN_WIDTH = NSA_WIDTH + 6 * KV_WIDTH + 3 * NSA_HEADS + POOL_WIDTH + SSM_WIDTH + N_BRANCH * D_MODEL
MOE_GROUPS = 4
MOE_EXPERTS_PER_GROUP = 8
MOE_EXPERTS = MOE_GROUPS * MOE_EXPERTS_PER_GROUP
MOE_TOPK = 2
MOE_FF = D_MODEL // 2
MOE_BLOCK = 128
DN_ALPHA = (2 * DEPTH) ** 0.25
DN_BETA = (8 * DEPTH) ** -0.25

kernel_name = "hybrid_nsa_pool_s5_hmoe_deepnorm"


def layer_norm(h, g, b, eps=1e-5):
    hf = h.astype(jnp.float32)
    mu = jnp.mean(hf, -1, keepdims=True)
    var = jnp.mean(jnp.square(hf - mu), -1, keepdims=True)
    return ((hf - mu) * lax.rsqrt(var + eps)).astype(h.dtype) * g + b


def masked_softmax(s, mask):
    s = jnp.where(mask, s.astype(jnp.float32), NEG_INF)
    m = jnp.max(s, -1, keepdims=True)
    e = jnp.where(mask, jnp.exp(s - m), 0.0)
    return e / jnp.maximum(jnp.sum(e, -1, keepdims=True), 1e-30)


def alibi_slopes(n):
    return jnp.exp2(-8.0 * jnp.arange(1, n + 1, dtype=jnp.float32) / n)


def block_overlap(nc, nsel):
    i0 = jnp.arange(nc)[:, None] * CMP_STRIDE
    j0 = jnp.arange(nsel)[None, :] * SEL_BLOCK
    return ((i0 < j0 + SEL_BLOCK) & (i0 + CMP_BLOCK > j0)).astype(jnp.float32)


def compress_blocks(k, pe, w1, w2):
    B_, S_, G, hd = k.shape
    nc = S_ // CMP_STRIDE
    kp = jnp.pad(k, ((0, 0), (0, CMP_BLOCK - CMP_STRIDE), (0, 0), (0, 0)))
    pieces = [kp[:, r * CMP_STRIDE: r * CMP_STRIDE + S_].reshape(B_, nc, CMP_STRIDE, G, hd)
              for r in range(CMP_BLOCK // CMP_STRIDE)]
    blk = jnp.concatenate(pieces, axis=2) + pe[None, None, :, None, :]
    blk = blk.transpose(0, 1, 3, 2, 4).reshape(B_, nc, G, CMP_BLOCK * hd)
    return jax.nn.gelu(blk @ w1) @ w2


def gather_blocks(blocks, idx):
    return jax.vmap(jax.vmap(lambda bl, ix: bl[ix]))(blocks, idx)


def nsa_attention(q, k_cmp, v_cmp, k_slc, v_slc, k_win, v_win, gate):
    B_, S_, G, R, hd = q.shape
    nc = k_cmp.shape[1]
    nsel = S_ // SEL_BLOCK
    topn = min(SEL_TOPN, nsel)
    scale = hd ** -0.5
    slopes = alibi_slopes(NSA_HEADS).reshape(G, R)
    cmp_end = jnp.arange(nc) * CMP_STRIDE + CMP_BLOCK - 1
    overlap = block_overlap(nc, nsel)
    k_blocks = k_slc.reshape(B_, nsel, SEL_BLOCK, G, hd).transpose(0, 3, 1, 2, 4)
    v_blocks = v_slc.reshape(B_, nsel, SEL_BLOCK, G, hd).transpose(0, 3, 1, 2, 4)
    kw_pad = jnp.pad(k_win, ((0, 0), (WINDOW, 0), (0, 0), (0, 0)))
    vw_pad = jnp.pad(v_win, ((0, 0), (WINDOW, 0), (0, 0), (0, 0)))
    jb = jnp.arange(nsel)

    def block(i):
        q0 = i * Q_BLOCK
        t = q0 + jnp.arange(Q_BLOCK)
        qb = lax.dynamic_slice_in_dim(q, q0, Q_BLOCK, axis=1)
        gb = lax.dynamic_slice_in_dim(gate, q0, Q_BLOCK, axis=1)
        dist_c = (t[:, None] - cmp_end[None, :])
        s_c = jnp.einsum('bqgrd,bcgd->bgrqc', qb, k_cmp).astype(jnp.float32) * scale \
            - slopes[:, :, None, None] * dist_c.astype(jnp.float32)
        p_c = masked_softmax(s_c, dist_c >= 0)
        o_c = jnp.einsum('bgrqc,bcgd->bqgrd', p_c.astype(v_cmp.dtype), v_cmp)
        imp = jnp.einsum('bgrqc,cj->bgqj', p_c, overlap)
        tb = t // SEL_BLOCK
        forced = (jb[None, :] == 0) | (jb[None, :] == tb[:, None]) | (jb[None, :] == tb[:, None] - 1)
        score = jnp.where(forced, FORCE_SCORE, jnp.where(jb[None, :] <= tb[:, None], imp, -1.0))
        _, idx = lax.top_k(score, topn)
        flat = idx.reshape(B_, G, Q_BLOCK * topn)
        kg = gather_blocks(k_blocks, flat).reshape(B_, G, Q_BLOCK, topn * SEL_BLOCK, hd)
        vg = gather_blocks(v_blocks, flat).reshape(B_, G, Q_BLOCK, topn * SEL_BLOCK, hd)
        pos = (idx[..., None] * SEL_BLOCK + jnp.arange(SEL_BLOCK)).reshape(B_, G, Q_BLOCK, topn * SEL_BLOCK)
        dist_s = t[None, None, :, None] - pos
        s_s = jnp.einsum('bqgrd,bgqkd->bgrqk', qb, kg).astype(jnp.float32) * scale \
            - slopes[None, :, :, None, None] * dist_s[:, :, None].astype(jnp.float32)
        p_s = masked_softmax(s_s, (dist_s >= 0)[:, :, None])
        o_s = jnp.einsum('bgrqk,bgqkd->bqgrd', p_s.astype(vg.dtype), vg)
        kwb = lax.dynamic_slice_in_dim(kw_pad, q0, WINDOW + Q_BLOCK, axis=1)
        vwb = lax.dynamic_slice_in_dim(vw_pad, q0, WINDOW + Q_BLOCK, axis=1)
        spos = q0 - WINDOW + jnp.arange(WINDOW + Q_BLOCK)
        dist_w = t[:, None] - spos[None, :]
        valid_w = (dist_w >= 0) & (dist_w < WINDOW) & (spos[None, :] >= 0)
        s_w = jnp.einsum('bqgrd,bkgd->bgrqk', qb, kwb).astype(jnp.float32) * scale \
            - slopes[:, :, None, None] * dist_w.astype(jnp.float32)
        p_w = masked_softmax(s_w, valid_w)
        o_w = jnp.einsum('bgrqk,bkgd->bqgrd', p_w.astype(vwb.dtype), vwb)
        return gb[..., 0:1] * o_c + gb[..., 1:2] * o_s + gb[..., 2:3] * o_w

    out = lax.map(block, jnp.arange(S_ // Q_BLOCK))
    return out.transpose(1, 0, 2, 3, 4, 5).reshape(B_, S_, G * R * hd)


def pool_mixer(u, pool_w, pool_scale):
    B_, S_, _ = u.shape
    cs = jnp.pad(jnp.cumsum(u.astype(jnp.float32), axis=1), ((0, 0), (1, 0), (0, 0)))
    t = jnp.arange(S_)
    outs = []
    for g, w in enumerate(POOL_WINDOWS):
        sl = slice(g * POOL_GROUP, (g + 1) * POOL_GROUP)
        csg = cs[..., sl]
        lo = jnp.maximum(t + 1 - w, 0)
        cnt = jnp.minimum(t + 1, w).astype(jnp.float32)
        pooled = ((csg[:, 1:] - csg[:, lo]) / cnt[None, :, None]).astype(u.dtype) - u[..., sl]
        outs.append(pooled @ pool_w[g])
    return jnp.concatenate(outs, -1) * pool_scale


def _ssm_combine(e1, e2):
    a1r, a1i, b1r, b1i = e1
    a2r, a2i, b2r, b2i = e2
    return (a2r * a1r - a2i * a1i, a2r * a1i + a2i * a1r,
            a2r * b1r - a2i * b1i + b2r, a2r * b1i + a2i * b1r + b2i)


def ssm_mixer(u, lam_re, lam_im, log_dt, b_re, b_im, c_re, c_im, d_skip, w_glu, b_glu):
    B_, S_, _ = u.shape
    f32 = jnp.float32
    lr, li = lam_re.astype(f32), lam_im.astype(f32)
    dt = jnp.exp(log_dt.astype(f32))[:, None]
    mag = jnp.exp(lr * dt)
    ar, ai = mag * jnp.cos(li * dt), mag * jnp.sin(li * dt)
    den = lr * lr + li * li
    cr = ((ar - 1.0) * lr + ai * li) / den
    ci = (ai * lr - (ar - 1.0) * li) / den
    bbr = cr[..., None] * b_re.astype(f32) - ci[..., None] * b_im.astype(f32)
    bbi = cr[..., None] * b_im.astype(f32) + ci[..., None] * b_re.astype(f32)
    cre, cim = c_re.astype(f32), c_im.astype(f32)

    def one(ub):
        ug = ub.reshape(S_, SSM_GROUPS, SSM_GROUP).astype(f32)
        xr = jnp.einsum('sgh,gph->sgp', ug, bbr)
        xi = jnp.einsum('sgh,gph->sgp', ug, bbi)
        a_r = jnp.broadcast_to(ar, xr.shape)
        a_i = jnp.broadcast_to(ai, xr.shape)
        _, _, hr, hi = lax.associative_scan(_ssm_combine, (a_r, a_i, xr, xi), axis=0)
        y = jnp.einsum('ghp,sgp->sgh', cre, hr) - jnp.einsum('ghp,sgp->sgh', cim, hi)
        return y.reshape(S_, SSM_WIDTH)

    y = lax.map(one, u).astype(u.dtype) + d_skip * u
    z = jax.nn.gelu(y)
    return z * jax.nn.sigmoid(z @ w_glu + b_glu)


def token_mixer(x, w_in, pe_k, pe_v, ck1, ck2, cv1, cv2, pool_w, pool_scale,
                lam_re, lam_im, log_dt, b_re, b_im, c_re, c_im, d_skip, w_glu, b_glu,
                w_up_nsa, w_up_pool, w_up_ssm, w_out):
    B_, S_, D = x.shape
    proj = jnp.einsum('bsd,dn->bsn', x, w_in)
    splits = np.cumsum([NSA_WIDTH] + [KV_WIDTH] * 6 + [3 * NSA_HEADS, POOL_WIDTH, SSM_WIDTH])
    q, kc, vc, ks, vs, kw, vw, ng, u_pool, u_ssm, bg = jnp.split(proj, splits, axis=-1)
    G, R, hd = NSA_KV_GROUPS, GQA_RATIO, NSA_HEAD_DIM
    kv = lambda a: a.reshape(B_, S_, G, hd)
    o_nsa = nsa_attention(q.reshape(B_, S_, G, R, hd),
                          compress_blocks(kv(kc), pe_k, ck1, ck2),
                          compress_blocks(kv(vc), pe_v, cv1, cv2),
                          kv(ks), kv(vs), kv(kw), kv(vw),
                          jax.nn.sigmoid(ng).reshape(B_, S_, G, R, 3))
    o_pool = pool_mixer(u_pool, pool_w, pool_scale)
    o_ssm = ssm_mixer(u_ssm, lam_re, lam_im, log_dt, b_re, b_im, c_re, c_im, d_skip, w_glu, b_glu)
    gates = jax.nn.sigmoid(bg).reshape(B_, S_, N_BRANCH, D)
    merged = (gates[:, :, 0] * (o_nsa @ w_up_nsa)
              + gates[:, :, 1] * (o_pool @ w_up_pool)
              + gates[:, :, 2] * (o_ssm @ w_up_ssm))
    return merged @ w_out


def moe_ffn(x, w_grp, b_grp, w_exp, b_exp, w_gate, w_up, w_down):
    B_, S_, D = x.shape
    T = B_ * S_
    xf = x.reshape(T, D)
    p_grp = jax.nn.softmax((xf @ w_grp + b_grp).astype(jnp.float32), -1)
    g_sel = jnp.argmax(p_grp, -1)
    p_g = jnp.max(p_grp, -1)
    logit_e = (xf @ w_exp + b_exp).astype(jnp.float32).reshape(T, MOE_GROUPS, MOE_EXPERTS_PER_GROUP)
    p_in = jax.nn.softmax(logit_e[jnp.arange(T), g_sel], -1)
    w_top, e_top = lax.top_k(p_in, MOE_TOPK)
    w_top = w_top / jnp.sum(w_top, -1, keepdims=True) * p_g[:, None]
    expert = (g_sel[:, None] * MOE_EXPERTS_PER_GROUP + e_top).astype(jnp.int32)
    N = T * MOE_TOPK
    flat_e = expert.reshape(N)
    flat_t = jnp.repeat(jnp.arange(T, dtype=jnp.int32), MOE_TOPK)
    flat_w = w_top.reshape(N)
    order = jnp.argsort(flat_e)
    se, st, sw = flat_e[order], flat_t[order], flat_w[order]
    counts = jax.ops.segment_sum(jnp.ones((N,), jnp.int32), flat_e, num_segments=MOE_EXPERTS)
    starts = jnp.cumsum(counts) - counts
    padded = (counts + MOE_BLOCK - 1) // MOE_BLOCK * MOE_BLOCK
    pend = jnp.cumsum(padded)
    pstart = pend - padded
    dest = pstart[se] + jnp.arange(N, dtype=jnp.int32) - starts[se]
    nblk = -(-N // MOE_BLOCK) + MOE_EXPERTS
    P = nblk * MOE_BLOCK
    buf_t = jnp.zeros((P,), jnp.int32).at[dest].set(st)
    buf_w = jnp.zeros((P,), jnp.float32).at[dest].set(sw)
    blk_e = jnp.minimum(jnp.searchsorted(pend, jnp.arange(nblk, dtype=jnp.int32) * MOE_BLOCK, side='right'),
                        MOE_EXPERTS - 1)
    xs = xf[buf_t].reshape(nblk, MOE_BLOCK, D)

    def expert_block(args):
        xb, e = args
        h = jax.nn.silu(xb @ w_gate[e]) * (xb @ w_up[e])
        return h @ w_down[e]

    ys = lax.map(expert_block, (xs, blk_e)).reshape(P, D)
    out = jax.ops.segment_sum(ys * buf_w[:, None].astype(ys.dtype), buf_t, num_segments=T)
    return out.reshape(B_, S_, D)


def setup_inputs(seed: int = 0) -> dict:
    key = jax.random.key(seed)
    ks = iter(jax.random.split(key, 40))
    f32 = jnp.float32

    def nrm(shape, scale):
        return jax.random.normal(next(ks), shape, f32) * scale

    L, D, hd = DEPTH, D_MODEL, NSA_HEAD_DIM
    E = MOE_EXPERTS
    return {
        "x": nrm((BATCH, SEQ, D), 1.0),
        "w_in": nrm((L, D, IN_WIDTH), D ** -0.5),
        "cmp_pe_k": nrm((L, CMP_BLOCK, hd), 0.1),
        "cmp_pe_v": nrm((L, CMP_BLOCK, hd), 0.1),
        "cmp_w1_k": nrm((L, CMP_BLOCK * hd, CMP_HIDDEN), (CMP_BLOCK * hd) ** -0.5),
        "cmp_w2_k": nrm((L, CMP_HIDDEN, hd), CMP_HIDDEN ** -0.5),
        "cmp_w1_v": nrm((L, CMP_BLOCK * hd, CMP_HIDDEN), (CMP_BLOCK * hd) ** -0.5),
        "cmp_w2_v": nrm((L, CMP_HIDDEN, hd), CMP_HIDDEN ** -0.5),
        "pool_w": nrm((L, len(POOL_WINDOWS), POOL_GROUP, POOL_GROUP), POOL_GROUP ** -0.5),
        "pool_scale": 1.0 + nrm((L, POOL_WIDTH), 0.1),
        "ssm_lam_re": -0.5 + nrm((L, SSM_GROUPS, SSM_STATE), 0.01),
        "ssm_lam_im": math.pi * jnp.arange(SSM_STATE, dtype=f32) + nrm((L, SSM_GROUPS, SSM_STATE), 0.01),
        "ssm_log_dt": jax.random.uniform(next(ks), (L, SSM_GROUPS), f32, math.log(1e-3), math.log(1e-1)),
        "ssm_b_re": nrm((L, SSM_GROUPS, SSM_STATE, SSM_GROUP), (2 * SSM_GROUP) ** -0.5),
        "ssm_b_im": nrm((L, SSM_GROUPS, SSM_STATE, SSM_GROUP), (2 * SSM_GROUP) ** -0.5),
        "ssm_c_re": nrm((L, SSM_GROUPS, SSM_GROUP, SSM_STATE), SSM_STATE ** -0.5),
        "ssm_c_im": nrm((L, SSM_GROUPS, SSM_GROUP, SSM_STATE), SSM_STATE ** -0.5),
        "ssm_d": nrm((L, SSM_WIDTH), 1.0),
        "ssm_w_glu": nrm((L, SSM_WIDTH, SSM_WIDTH), SSM_WIDTH ** -0.5),
        "ssm_b_glu": nrm((L, SSM_WIDTH), 0.02),
        "w_up_nsa": nrm((L, NSA_WIDTH, D), NSA_WIDTH ** -0.5),
        "w_up_pool": nrm((L, POOL_WIDTH, D), POOL_WIDTH ** -0.5),
        "w_up_ssm": nrm((L, SSM_WIDTH, D), SSM_WIDTH ** -0.5),
        "w_out": nrm((L, D, D), D ** -0.5 * DN_BETA),
        "ln1_g": 1.0 + nrm((L, D), 0.02),
        "ln1_b": nrm((L, D), 0.02),
        "router_w_grp": nrm((L, D, MOE_GROUPS), D ** -0.5),
        "router_b_grp": nrm((L, MOE_GROUPS), 0.01),
        "router_w_exp": nrm((L, D, E), D ** -0.5),
        "router_b_exp": nrm((L, E), 0.01),
        "moe_w_gate": nrm((L, E, D, MOE_FF), D ** -0.5),
        "moe_w_up": nrm((L, E, D, MOE_FF), D ** -0.5),
        "moe_w_down": nrm((L, E, MOE_FF, D), MOE_FF ** -0.5 * DN_BETA),
        "ln2_g": 1.0 + nrm((L, D), 0.02),
        "ln2_b": nrm((L, D), 0.02),
    }


def reference(x, w_in, cmp_pe_k, cmp_pe_v, cmp_w1_k, cmp_w2_k, cmp_w1_v, cmp_w2_v,
              pool_w, pool_scale, ssm_lam_re, ssm_lam_im, ssm_log_dt, ssm_b_re, ssm_b_im,
              ssm_c_re, ssm_c_im, ssm_d, ssm_w_glu, ssm_b_glu, w_up_nsa, w_up_pool, w_up_ssm,
              w_out, ln1_g, ln1_b, router_w_grp, router_b_grp, router_w_exp, router_b_exp,
              moe_w_gate, moe_w_up, moe_w_down, ln2_g, ln2_b):
    for l in range(DEPTH):
        mix = token_mixer(x, w_in[l], cmp_pe_k[l], cmp_pe_v[l], cmp_w1_k[l], cmp_w2_k[l],
                          cmp_w1_v[l], cmp_w2_v[l], pool_w[l], pool_scale[l],
                          ssm_lam_re[l], ssm_lam_im[l], ssm_log_dt[l], ssm_b_re[l], ssm_b_im[l],
                          ssm_c_re[l], ssm_c_im[l], ssm_d[l], ssm_w_glu[l], ssm_b_glu[l],
                          w_up_nsa[l], w_up_pool[l], w_up_ssm[l], w_out[l])
        x = layer_norm(DN_ALPHA * x + mix, ln1_g[l], ln1_b[l])
        ffn = moe_ffn(x, router_w_grp[l], router_b_grp[l], router_w_exp[l], router_b_exp[l],
                      moe_w_gate[l], moe_w_up[l], moe_w_down[l])
        x = layer_norm(DN_ALPHA * x + ffn, ln2_g[l], ln2_b[l])
    return x
```

```python
import numpy as np
import concourse.bass as bass
import concourse.mybir as mybir
from contextlib import ExitStack

F32 = mybir.dt.float32
BF16 = mybir.dt.bfloat16
I32 = mybir.dt.int32
AF = mybir.ActivationFunctionType
ALU = mybir.AluOpType
AX = mybir.AxisListType


class Tile:
    def __init__(self, ap, name=""):
        self.ap = ap[:]
        self.name = name
        self.w = None
        self.r = {}

    def __getitem__(self, idx):
        return View(self, self.ap[idx])

    def v(self):
        return View(self, self.ap)


class View:
    def __init__(self, tile, ap):
        self.tile = tile
        self.ap = ap

    def __getitem__(self, idx):
        return View(self.tile, self.ap[idx])

    def rearrange(self, pat, **kw):
        return View(self.tile, self.ap.rearrange(pat, **kw))

    def unsqueeze(self, ax):
        return View(self.tile, self.ap.unsqueeze(ax))

    def broadcast_to(self, shp):
        return View(self.tile, self.ap.broadcast_to(list(shp)))

    def bitcast(self, dt):
        return View(self.tile, self.ap.bitcast(dt))


def _ap(v):
    return v.ap if isinstance(v, (View, Tile)) else v


def _tile(v):
    if isinstance(v, View):
        return v.tile
    if isinstance(v, Tile):
        return v
    return None


class FW:
    NDMA = 6
    import os as _os
    LIMIT = int(_os.environ.get('FWLIMIT', '40000'))

    def __init__(self, nc):
        self.nc = nc
        self.es = ExitStack()
        self.eng = {"pe": nc.tensor, "act": nc.scalar, "dve": nc.vector, "pool": nc.gpsimd, "sp": nc.sync}
        self.semlist = {}
        self.val = {}
        self.last = {}
        self.nsem = 0
        for k in ["pe", "act", "dve", "pool"]:
            self.semlist[k] = [self._newsem(k)]
            self.val[k] = 0
        self.dmaq = {}
        for q in ["sp", "pool"]:
            keys = []
            for i in range(self.NDMA):
                k = "d_%s%d" % (q, i)
                self.semlist[k] = [self._newsem(k)]
                self.val[k] = 0
                keys.append(k)
            self.dmaq[q] = [keys, 0]
        self.seen = {e: {} for e in self.eng}
        self.ninst = 0

    def _newsem(self, k):
        self.nsem += 1
        return self.es.enter_context(self.nc.semaphore("s_%s_%d" % (k, self.nsem)))

    def _bump(self, k, inc):
        if self.val[k] + inc > self.LIMIT:
            self.semlist[k].append(self._newsem(k))
            self.val[k] = 0
        self.val[k] += inc
        idx = len(self.semlist[k]) - 1
        ev = (k, idx, self.val[k])
        self.last[k] = ev
        return self.semlist[k][idx], ev

    def sb(self, shape, dt, name=None, stack=None):
        st = stack if stack is not None else self.es
        self.nalloc = getattr(self, "nalloc", 0) + 1
        nm = "%s_%d" % (name or "t", self.nalloc)
        t = st.enter_context(self.nc.sbuf_tensor(nm, list(shape), dt))
        return Tile(t, nm)

    def ps(self, shape, dt=F32, name=None, stack=None):
        st = stack if stack is not None else self.es
        t = st.enter_context(self.nc.psum_tensor(list(shape), dt))
        return Tile(t, name or "")

    def _wait(self, e, ev):
        if ev is None:
            return
        k, idx, v = ev
        if self.seen[e].get((k, idx), 0) >= v:
            return
        for (k2, i2), v2 in self.seen[e].items():
            if k2 == k and i2 > idx:
                return
        self.eng[e].wait_ge(self.semlist[k][idx], v)
        self.seen[e][(k, idx)] = v

    def _deps(self, e, reads, writes):
        for t in reads:
            if t is None:
                continue
            if t.w is not None and not (e == "pe" and t.w[0] == "pe"):
                self._wait(e, t.w)
        for t in writes:
            if t is None:
                continue
            if t.w is not None and not (e == "pe" and t.w[0] == "pe"):
                self._wait(e, t.w)
            for k, ev in t.r.items():
                if e == "pe" and k == "pe":
                    continue
                self._wait(e, ev)

    def _commit(self, ev, reads, writes):
        for t in reads:
            if t is None:
                continue
            t.r[ev[0]] = ev
        for t in writes:
            if t is None:
                continue
            t.w = ev
            t.r = {}

    armed = False
    budget = 10 ** 9

    def arm(self):
        import os
        self.armed = True
        self.budget = int(os.environ.get("FWBUDGET", str(10 ** 9)))
        self.skipped = 0

    def _spend(self):
        if not self.armed:
            return True
        if self.budget <= 0:
            self.skipped += 1
            return False
        self.budget -= 1
        return True

    def op(self, e, ins_fn, reads, writes):
        if not self._spend():
            return None
        reads = [_tile(r) for r in reads]
        writes = [_tile(w) for w in writes]
        self._deps(e, reads, writes)
        ins = ins_fn()
        sem, ev = self._bump(e, 1)
        ins.then_inc(sem, 1)
        self._commit(ev, reads, writes)
        self.ninst += 1
        return ins

    def dma(self, q, out, in_, **kw):
        if not self._spend():
            return None
        keys, idx = self.dmaq[q]
        k = keys[idx % len(keys)]
        self.dmaq[q][1] = idx + 1
        e = q
        reads = [_tile(in_)]
        writes = [_tile(out)]
        if k in self.last:
            self._wait(e, self.last[k])
        self._deps(e, reads, writes)
        ins = self.eng[e].dma_start(out=_ap(out), in_=_ap(in_), **kw)
        sem, ev = self._bump(k, 16)
        ins.then_inc(sem, 16)
        self._commit(ev, reads, writes)
        self.ninst += 1
        return ins

    def barrier(self, engines=None):
        engines = engines or list(self.eng.keys())
        for e in engines:
            for k, ev in self.last.items():
                self._wait(e, ev)

    def maybe_switch(self, force=False):
        return

    def matmul(self, out, lhsT, rhs, start=True, stop=True):
        return self.op("pe", lambda: self.nc.tensor.matmul(_ap(out), _ap(lhsT), _ap(rhs), start=start, stop=stop),
                       [lhsT, rhs], [out])

    def transpose(self, out, in_, ident):
        return self.op("pe", lambda: self.nc.tensor.transpose(_ap(out), _ap(in_), _ap(ident)), [in_, ident], [out])

    def act(self, out, in_, func, bias=None, scale=None, accum_out=None, e="act"):
        def _fl(v):
            a = _ap(v)
            if len(a.shape) == 3:
                return View(_tile(v), a.rearrange("p a b -> p (a b)"))
            return v
        out = _fl(out)
        in_ = _fl(in_)
        kw = {}
        rd = [in_]
        wr = [out]
        if bias is not None:
            kw["bias"] = _ap(bias)
            if not isinstance(bias, (int, float)):
                rd.append(bias)
        if scale is not None:
            kw["scale"] = _ap(scale)
            if not isinstance(scale, (int, float)):
                rd.append(scale)
        if accum_out is not None:
            kw["accum_out"] = _ap(accum_out)
            wr.append(accum_out)
        return self.op(e, lambda: self.nc.scalar.activation(_ap(out), _ap(in_), func, **kw), rd, wr)

    def tt(self, e, out, a, b, op):
        return self.op(e, lambda: self.eng[e].tensor_tensor(_ap(out), _ap(a), _ap(b), op), [a, b], [out])

    def ts(self, e, out, a, s1, s2, op0, op1=None, accum_out=None):
        rd = [a]
        wr = [out]
        if not isinstance(s1, (int, float)):
            rd.append(s1)
        if s2 is not None and not isinstance(s2, (int, float)):
            rd.append(s2)
        kw = {}
        if op1 is not None:
            kw["op1"] = op1
        if accum_out is not None:
            kw["accum_out"] = _ap(accum_out)
            wr.append(accum_out)
        return self.op(e, lambda: self.eng[e].tensor_scalar(_ap(out), _ap(a), _ap(s1), _ap(s2) if s2 is not None else None, op0, **kw), rd, wr)

    def stt(self, e, out, a, s, b, op0, op1):
        rd = [a, b]
        if not isinstance(s, (int, float)):
            rd.append(s)
        return self.op(e, lambda: self.eng[e].scalar_tensor_tensor(_ap(out), _ap(a), _ap(s), _ap(b), op0, op1), rd, [out])

    def copy(self, e, out, in_):
        if e == "act":
            return self.act(out, in_, AF.Copy)
        return self.op(e, lambda: self.eng[e].tensor_copy(_ap(out), _ap(in_)), [in_], [out])

    def memset(self, e, out, val):
        return self.op(e, lambda: self.eng[e].memset(_ap(out), val), [], [out])

    def reduce(self, e, out, in_, op, axis=AX.X):
        return self.op(e, lambda: self.eng[e].tensor_reduce(_ap(out), _ap(in_), axis, op), [in_], [out])

    def finish(self):
        self.barrier()

import math
import ml_dtypes
from concourse.bass_utils import run_bass_kernel_spmd

D = 1024
NH = 8
HD = 64
NG = 2
RR = 4
INW = 5400
NEXP = 32
FF = 512
DEPTH_FULL = 4
ALPHA = (2 * DEPTH_FULL) ** 0.25
NEG = -30000.0
SLOPES = [2.0 ** (-(h + 1)) for h in range(8)]
C_Q, C_KC, C_VC, C_KS, C_VS, C_KW, C_VW, C_NG, C_UP, C_US, C_BG = 0, 512, 640, 768, 896, 1024, 1152, 1280, 1304, 1816, 2328

WNAMES = ["w_in", "cmp_pe_k", "cmp_pe_v", "cmp_w1_k", "cmp_w2_k", "cmp_w1_v", "cmp_w2_v", "pool_w", "pool_scale",
          "ssm_lam_re", "ssm_lam_im", "ssm_log_dt", "ssm_b_re", "ssm_b_im", "ssm_c_re", "ssm_c_im", "ssm_d",
          "ssm_w_glu", "ssm_b_glu", "w_up_nsa", "w_up_pool", "w_up_ssm", "w_out", "ln1_g", "ln1_b",
          "router_w_grp", "router_b_grp", "router_w_exp", "router_b_exp", "moe_w_gate", "moe_w_up", "moe_w_down",
          "ln2_g", "ln2_b"]


def host_consts(S):
    bf = ml_dtypes.bfloat16
    NC_ = S // 16
    NJ = S // 64
    NT = S // 128
    c = {}
    t = np.arange(S)
    qa = np.zeros((8, 4, S), np.float32)
    for h in range(8):
        sl = SLOPES[h]
        qa[h, 0] = sl * 128
        qa[h, 1] = sl
        qa[h, 2] = -sl * 128 * (t // 128)
        qa[h, 3] = -sl * (t % 128)
    c["qaug"] = qa.astype(bf)
    ka = np.zeros((4, S + 16), np.float32)
    ka[0, :S] = t // 128
    ka[1, :S] = t % 128
    ka[2] = 1
    ka[3] = 1
    c["kaug"] = ka.astype(bf)
    pc = np.arange(NC_) * 16 + 31
    kc = np.zeros((4, NC_), np.float32)
    kc[0] = pc // 128
    kc[1] = pc % 128
    kc[2] = 1
    kc[3] = 1
    c["kaugc"] = kc.astype(bf)
    p = np.arange(128)[:, None]
    q = np.arange(128)[None, :]
    nm = np.zeros((128, 16, 512), np.float32)
    for m in range(16):
        valid = (16 * p + 31) <= (128 * m + q)
        nm[:, m, :] = np.tile(np.where(valid, 0.0, NEG), (1, 4))
    c["negcmp"] = nm.astype(bf)
    c["negcausal"] = np.tile(np.where(p <= q, 0.0, NEG), (1, 4)).astype(bf)
    c["negwinlo"] = np.tile(np.where(p > q, 0.0, NEG), (1, 4)).astype(bf)
    E = np.zeros((128, NT, 128), np.float32)
    for kt in range(NT):
        for pp in range(128):
            j = 2 * kt + pp // 64
            if j < 128:
                E[j, kt, pp] = 1.0
    c["etab"] = E.astype(bf)
    ci = np.arange(NC_)[:, None] * 16
    j0 = np.arange(NJ)[None, :] * 64
    ov = ((ci < j0 + 64) & (ci + 32 > j0)).astype(np.float32)
    nct = max(1, NC_ // 128)
    ovp = np.zeros((nct * 128, 128), np.float32)
    ovp[:NC_, :NJ] = ov
    c["overlap"] = ovp.reshape(nct, 128, 128).transpose(1, 0, 2).copy().astype(bf)
    qq = np.arange(128)[:, None]
    jr = np.arange(256)[None, :] - 128
    tbr = (qq >= 64).astype(np.int64)
    forced = (jr == tbr) | (jr == tbr - 1)
    valid = jr <= tbr
    c["vm"] = (valid & ~forced).astype(np.float32)
    c["am"] = np.where(forced, 1.0e4, np.where(valid, 0.0, -1.0)).astype(np.float32)
    c["svals"] = np.arange(128, dtype=np.float32)[:, None].copy()
    c["tvals"] = np.tile(np.arange(129, dtype=np.float32)[None, :], (64, 1)).copy()
    tri = (np.arange(128)[:, None] <= np.arange(128)[None, :]).astype(np.float32)
    c["tri"] = tri.astype(bf)
    pf = np.ones((128, 4, 16), np.float32)
    for g, w in enumerate((2, 4, 8, 16)):
        tt_ = np.arange(16)
        pf[:, g, :] = (w / np.minimum(tt_ + 1, w))[None, :]
    c["poolfix"] = pf
    gm = np.zeros((128, 8), np.float32)
    for pp in range(128):
        gm[pp, pp // 16] = 1.0
    c["gmask"] = gm
    return c


CONST_SPECS = None


import os
P0MODE = int(os.environ.get('P0MODE', '0'))
P4BSTOP = int(os.environ.get('P4BSTOP', '0'))
SKIP3 = int(os.environ.get('SKIP3', '0'))
SKIP4B = int(os.environ.get('SKIP4B', '0'))
P6STOP = int(os.environ.get('P6STOP', '0'))
ARM = os.environ.get('ARM', '')


def build(S, L, dump=False, upto=None):
    NT = S // 128
    NC_ = S // 16
    NCT = max(1, NC_ // 128)
    NJ = S // 64
    NTB = S // 512
    hc = host_consts(S)
    nc = bass.Bass("TRN2", target_bir_lowering=False)
    f = FW(nc)
    inp = {}

    def din(name, shape, dt=F32):
        inp[name] = nc.dram_tensor(name, list(shape), dt, kind="ExternalInput").ap()
        return inp[name]

    x_in = din("x", [S, D])
    full_shapes = {
        "w_in": [L, D, INW], "cmp_pe_k": [L, 32, 64], "cmp_pe_v": [L, 32, 64], "cmp_w1_k": [L, 2048, 128],
        "cmp_w2_k": [L, 128, 64], "cmp_w1_v": [L, 2048, 128], "cmp_w2_v": [L, 128, 64], "pool_w": [L, 4, 128, 128],
        "pool_scale": [L, 512], "ssm_lam_re": [L, 32, 64], "ssm_lam_im": [L, 32, 64], "ssm_log_dt": [L, 32],
        "ssm_b_re": [L, 32, 64, 16], "ssm_b_im": [L, 32, 64, 16], "ssm_c_re": [L, 32, 16, 64], "ssm_c_im": [L, 32, 16, 64],
        "ssm_d": [L, 512], "ssm_w_glu": [L, 512, 512], "ssm_b_glu": [L, 512], "w_up_nsa": [L, 512, D],
        "w_up_pool": [L, 512, D], "w_up_ssm": [L, 512, D], "w_out": [L, D, D], "ln1_g": [L, D], "ln1_b": [L, D],
        "router_w_grp": [L, D, 4], "router_b_grp": [L, 4], "router_w_exp": [L, D, 32], "router_b_exp": [L, 32],
        "moe_w_gate": [L, NEXP, D, FF], "moe_w_up": [L, NEXP, D, FF], "moe_w_down": [L, NEXP, FF, D],
        "ln2_g": [L, D], "ln2_b": [L, D]}
    W = {k: din(k, full_shapes[k]) for k in WNAMES}
    C = {}
    for k, v in hc.items():
        C[k] = din("c_" + k, v.shape, BF16 if v.dtype == ml_dtypes.bfloat16 else F32)
    out_d = nc.dram_tensor("out", [S, D], F32, kind="ExternalOutput").ap()

    def scratch(name, shape, dt):
        return nc.dram_tensor(name, list(shape), dt, kind="ExternalOutput" if dump else "Internal").ap()

    xT_d = scratch("xT_d", [D, S], BF16)
    qT_d = scratch("qT_d", [8, 68, S], BF16)
    kT_d = scratch("kT_d", [8, 68, S + 16], BF16)
    uT_d = scratch("uT_d", [D, 16 + S], BF16)
    v_d = scratch("v_d", [S, 4, 65], BF16)
    gate_d = scratch("gate_d", [S, 24], F32)
    kcmp_d = scratch("kcmp_d", [2, 68, NCT * 128], BF16)
    vcmp_d = scratch("vcmp_d", [NCT * 128, 2, 65], BF16)
    onT_d = scratch("onT_d", [512, S], BF16)
    opT_d = scratch("opT_d", [512, S], BF16)
    osT_d = scratch("osT_d", [512, S], BF16)
    dbg_d = scratch("dbg_d", [8, 128, 512], BF16)
    dbg2_d = scratch("dbg2_d", [3, 128, 260], F32)
    x1_d = scratch("x1_d", [S, D], F32)
    xr_d = scratch("xr_d", [S, D], F32)

    identf = f.sb([128, 128], F32, "identf")
    identb = f.sb([128, 128], BF16, "identb")
    f.memset("pool", identf, 0.0)
    f.op("pool", lambda: nc.gpsimd.affine_select(out=identf.ap, in_=identf.ap, pattern=[[-1, 128]], compare_op=ALU.not_equal,
                                                 fill=1.0, base=0, channel_multiplier=1), [identf], [identf])
    f.copy("dve", identb, identf)
    PS = [f.ps([128, 512], F32, "ps%d" % i) for i in range(7)]
    PSB = f.ps([128, 1024], BF16, "psb")

    st = ExitStack()
    t_qa = f.sb([4, 8, S], BF16, "t_qa", stack=st)
    f.dma("sp", t_qa, C["qaug"].rearrange("h a s -> a h s"))
    f.dma("sp", qT_d.rearrange("h d s -> d h s")[64:68, :, :], t_qa)
    t_ka = f.sb([4, S + 16], BF16, "t_ka", stack=st)
    f.dma("sp", t_ka, C["kaug"])
    for i in range(8):
        f.dma("sp", kT_d[i, 64:68, :], t_ka)
    t_kc = f.sb([4, NC_], BF16, "t_kc", stack=st)
    f.dma("sp", t_kc, C["kaugc"])
    for g in range(2):
        f.dma("sp", kcmp_d[g, 64:68, 0:NC_], t_kc)
    zt = f.sb([128, 8, 16], BF16, "zt", stack=st)
    f.memset("dve", zt, 0.0)
    f.dma("sp", kT_d.rearrange("i d s -> d i s")[0:64, :, S:S + 16], zt[0:64])
    f.dma("sp", uT_d.rearrange("(i p) s -> p i s", p=128)[:, :, 0:16], zt)
    f.barrier()
    st.close()
    if upto == "PRE":
        f.finish()
        return nc, hc

    ei = [0]

    def rot(engs):
        ei[0] += 1
        return engs[ei[0] % len(engs)]

    lnscr = {}

    def layer_norm(st, src, gbc, bbc, dst, eps=1e-5):
        if id(st) not in lnscr:
            lnscr[id(st)] = [[f.sb([128, 2, 6], F32, stack=st), f.sb([128, 2], F32, stack=st)] for _ in range(2)] + [0]
        sc_ = lnscr[id(st)]
        sc_[2] += 1
        stt_, mv = sc_[sc_[2] % 2]
        for c2 in range(2):
            f.op("dve", lambda c2=c2: nc.vector.bn_stats(out=stt_.ap[:, c2, :], in_=src.ap[:, c2 * 512:(c2 + 1) * 512]), [src], [stt_])
        f.op("dve", lambda: nc.vector.bn_aggr(out=mv.ap, in_=stt_.ap), [stt_], [mv])
        f.ts("dve", mv[:, 1:2], mv[:, 1:2], eps, None, ALU.add)
        f.act(mv[:, 1:2], mv[:, 1:2], AF.Sqrt)
        f.op("dve", lambda: nc.vector.reciprocal(out=mv.ap[:, 1:2], in_=mv.ap[:, 1:2]), [mv], [mv])
        f.ts("dve", dst, src, mv[:, 0:1], mv[:, 1:2], ALU.subtract, ALU.mult)
        f.tt("pool", dst, dst, gbc, ALU.mult)
        f.tt("pool", dst, dst, bbc, ALU.add)

    class LNCtx:
        pass

    for l in range(L):
        xsrc = x_in if l == 0 else xr_d
        xdst = out_d if l == L - 1 else xr_d

        st = ExitStack()
        xt_b = [f.sb([128, D], F32, "p0x%d" % i, stack=st) for i in range(2)]
        xT_s = [f.sb([128, 8, 512], BF16, "p0t%d" % i, stack=st) for i in range(2)]
        for tb in range(NTB):
            xs = xT_s[tb % 2]
            for sub in range(4):
                tt0 = tb * 512 + sub * 128
                xb = xt_b[sub % 2]
                f.dma("sp", xb, xsrc[tt0:tt0 + 128, :])
                for hh in range(2):
                    ps = PS[(sub * 2 + hh) % 4]
                    for k4 in range(4):
                        kt = hh * 4 + k4
                        f.transpose(ps[:, k4 * 128:(k4 + 1) * 128], xb[:, kt * 128:(kt + 1) * 128], identf)
                    if P0MODE != 1:
                        f.copy("dve", xs[:, hh * 4:(hh + 1) * 4, sub * 128:(sub + 1) * 128],
                               ps.v().rearrange("p (k t) -> p k t", k=4))
            if P0MODE not in (1, 2):
                f.dma("sp", xT_d.rearrange("(k p) s -> p k s", p=128)[:, :, tb * 512:(tb + 1) * 512], xs)
        f.barrier()
        st.close()
        if upto == 'P0':
            f.finish()
            return nc, hc

        st = ExitStack()
        NP1 = C_BG
        wq = f.sb([128, 8, NP1], BF16, "wq", stack=st)
        wv = W["w_in"][l].rearrange("(k p) n -> p k n", p=128)
        for kt in range(8):
            f.dma("pool", wq[:, kt, :], wv[:, kt, 0:NP1])
        xs_b = [f.sb([128, 8, 512], BF16, "p1x%d" % i, stack=st) for i in range(2)]
        qst = [f.sb([64, 8, 512], BF16, "qst%d" % i, stack=st) for i in range(2)]
        kst = [f.sb([64, 8, 512], BF16, "kst%d" % i, stack=st) for i in range(2)]
        ust = [f.sb([128, 8, 512], BF16, "ust%d" % i, stack=st) for i in range(2)]
        vst = [f.sb([128, 4, 4, 65], BF16, "vst%d" % i, stack=st) for i in range(2)]
        gst = [f.sb([128, 4, 24], F32, "gst%d" % i, stack=st) for i in range(2)]
        ngt = f.sb([128, 24], F32, "ngt", stack=st)
        for i in range(2):
            f.memset("dve", vst[i], 1.0)
        kcols = [C_KC, C_KC + 64, C_VC, C_VC + 64, C_KS, C_KS + 64, C_KW, C_KW + 64]
        pi = 0
        for tb in range(NTB):
            xs = xs_b[tb % 2]
            f.dma("sp", xs, xT_d.rearrange("(k p) s -> p k s", p=128)[:, :, tb * 512:(tb + 1) * 512])
            q_, k_, u_, v_, g_ = qst[tb % 2], kst[tb % 2], ust[tb % 2], vst[tb % 2], gst[tb % 2]
            jobs = []
            for h in range(8):
                jobs.append((C_Q + 64 * h, 64, q_, h, 0.125))
            for i in range(8):
                jobs.append((kcols[i], 64, k_, i, 1.0))
            for i in range(4):
                jobs.append((C_UP + 128 * i, 128, u_, i, 1.0))
            for i in range(4):
                jobs.append((C_US + 128 * i, 128, u_, 4 + i, 1.0))
            for (c0, M, dst, di, sc) in jobs:
                ps = PS[pi % 4]
                pi += 1
                for kt in range(8):
                    f.matmul(ps[0:M, :], wq[:, kt, c0:c0 + M], xs[:, kt, :], start=(kt == 0), stop=(kt == 7))
                e = rot(["dve", "act"])
                if e == "act":
                    f.act(dst[0:M, di, :], ps[0:M, :], AF.Copy, scale=sc)
                else:
                    f.ts("dve", dst[0:M, di, :], ps[0:M, :], sc, None, ALU.mult)
            for sub in range(4):
                ps = PS[4 + sub % 2]
                for kt in range(8):
                    f.matmul(ps[:, 0:408], xs[:, kt, sub * 128:(sub + 1) * 128], wq[:, kt, C_VS:C_VS + 408], start=(kt == 0), stop=(kt == 7))
                f.copy("dve", v_[:, sub, 0:2, 0:64], ps[:, 0:128].rearrange("p (g d) -> p g d", g=2))
                f.copy("dve", v_[:, sub, 2:4, 0:64], ps[:, 256:384].rearrange("p (g d) -> p g d", g=2))
                f.copy("dve", ngt, ps[:, 384:408])
                f.act(g_[:, sub, :], ngt, AF.Sigmoid)
            ts_ = slice(tb * 512, (tb + 1) * 512)
            f.dma("sp", qT_d.rearrange("h d s -> d h s")[0:64, :, ts_], q_)
            f.dma("sp", kT_d.rearrange("i d s -> d i s")[0:64, :, ts_], k_)
            f.dma("sp", uT_d.rearrange("(i p) s -> p i s", p=128)[:, :, 16 + tb * 512:16 + (tb + 1) * 512], u_)
            f.dma("sp", v_d[ts_].rearrange("(n p) j c -> p n j c", p=128), v_)
            f.dma("sp", gate_d[ts_].rearrange("(n p) c -> p n c", p=128), g_)
        f.barrier()
        st.close()
        if upto == 'P1':
            f.finish()
            return nc, hc

        st = ExitStack()
        for which, (w1n, w2n, pen, base) in enumerate([("cmp_w1_k", "cmp_w2_k", "cmp_pe_k", 0), ("cmp_w1_v", "cmp_w2_v", "cmp_pe_v", 2)]):
            w1b = f.sb([64, 32, 128], BF16, "w1b%d" % which, stack=st)
            f.dma("pool", w1b, W[w1n][l].rearrange("(l d) h -> d l h", d=64))
            w2b = f.sb([128, 64], BF16, "w2b%d" % which, stack=st)
            f.dma("pool", w2b, W[w2n][l])
            pe_s = f.sb([32, 64], F32, "pe%d" % which, stack=st)
            f.dma("sp", pe_s, W[pen][l])
            f.transpose(PS[0][0:64, 0:32], pe_s, identf[0:32, 0:32])
            peT = f.sb([64, 32], BF16, "peT%d" % which, stack=st)
            f.copy("dve", peT, PS[0][0:64, 0:32])
            for li in range(32):
                f.matmul(PS[1][:, 0:1], w1b[:, li, :], peT[:, li:li + 1], start=(li == 0), stop=(li == 31))
            b1 = f.sb([128, 1], F32, "b1%d" % which, stack=st)
            f.copy("dve", b1, PS[1][:, 0:1])
            for g in range(2):
                kc_s = f.sb([64, S + 16], BF16, "kcs%d%d" % (which, g), stack=st)
                f.dma("sp", kc_s, kT_d[base + g, 0:64, :])
                kcv = kc_s.v().rearrange("d (n r) -> d n r", r=16)
                for cb in range(0, NC_, 512):
                    nb = min(512, NC_ - cb)
                    for li in range(32):
                        f.matmul(PS[2][:, 0:nb], w1b[:, li, :], kcv[:, cb + li // 16: cb + li // 16 + nb, li % 16], start=(li == 0), stop=(li == 31))
                    hT = f.sb([128, 512], BF16, "hT%d%d" % (which, g), stack=st)
                    f.act(hT[:, 0:nb], PS[2][:, 0:nb], AF.Gelu, bias=b1[:, 0:1])
                    if which == 0:
                        f.matmul(PS[3][0:64, 0:nb], w2b, hT[:, 0:nb])
                        kcm = f.sb([64, 512], BF16, "kcm%d" % g, stack=st)
                        f.copy("dve", kcm[:, 0:nb], PS[3][0:64, 0:nb])
                        f.dma("sp", kcmp_d[g, 0:64, cb:cb + nb], kcm[:, 0:nb])
                    else:
                        vcm = f.sb([128, 4, 65], BF16, "vcm%d" % g, stack=st)
                        f.memset("pool", vcm, 1.0)
                        nbt = (nb + 127) // 128
                        for bt in range(nbt):
                            w_ = min(128, nb - bt * 128)
                            f.matmul(PS[3][0:w_, bt * 64:(bt + 1) * 64], hT[:, bt * 128:bt * 128 + w_], w2b)
                            f.copy("dve", vcm[0:w_, bt, 0:64], PS[3][0:w_, bt * 64:(bt + 1) * 64])
                        if nb >= 128:
                            f.dma("sp", vcmp_d[cb:cb + nb, g, :].rearrange("(n p) c -> p n c", p=128), vcm[:, 0:nbt, :])
                        else:
                            f.dma("sp", vcmp_d[cb:cb + nb, g, :], vcm[0:nb, 0, :])
        f.barrier()
        st.close()
        if upto == 'P2':
            f.finish()
            return nc, hc

        st = ExitStack()
        kT_s = f.sb([68, 4, S], BF16, "kT_s", stack=st)
        for i in range(4):
            f.dma("sp", kT_s[:, i, :], kT_d[4 + i, :, 0:S])
        va_s = f.sb([128, NT, 4, 65], BF16, "va_s", stack=st)
        vdv = v_d.rearrange("(n p) j c -> p n j c", p=128)
        for n0 in range(0, NT, 16):
            n1 = min(NT, n0 + 16)
            f.dma("sp", va_s[:, n0:n1], vdv[:, n0:n1])
        kc_s = f.sb([68, 2, NCT * 128], BF16, "kc_s", stack=st)
        f.memset("dve", kc_s, 0.0)
        f.dma("sp", kc_s[:, :, 0:NC_], kcmp_d.rearrange("g d c -> d g c")[:, :, 0:NC_])
        vc_s = f.sb([128, NCT, 2, 65], BF16, "vc_s", stack=st)
        f.memset("dve", vc_s, 0.0)
        if NC_ >= 128:
            f.dma("sp", vc_s, vcmp_d.rearrange("(n p) g c -> p n g c", p=128))
        else:
            f.dma("sp", vc_s[0:NC_, 0], vcmp_d[0:NC_])
        negcmp = f.sb([128, 16, 512], BF16, "negcmp", stack=st)
        f.dma("sp", negcmp, C["negcmp"])
        negcau = f.sb([128, 512], BF16, "negcau", stack=st)
        f.dma("sp", negcau, C["negcausal"])
        negwl = f.sb([128, 512], BF16, "negwl", stack=st)
        f.dma("sp", negwl, C["negwinlo"])
        etab = f.sb([128, NT, 128], BF16, "etab", stack=st)
        f.dma("sp", etab, C["etab"])
        ovl = f.sb([128, NCT, 128], BF16, "ovl", stack=st)
        f.dma("sp", ovl, C["overlap"])
        vm = f.sb([128, 256], F32, "vm", stack=st)
        f.dma("sp", vm, C["vm"])
        am = f.sb([128, 256], F32, "am", stack=st)
        f.dma("sp", am, C["am"])
        qT_b = [f.sb([68, 8 * 128], BF16, "qTb%d" % i, stack=st) for i in range(2)]
        gt_b = [f.sb([128, 24], F32, "gtb%d" % i, stack=st) for i in range(2)]
        PTc = [f.sb([128, 512], BF16, "ptc%d" % i, stack=st) for i in range(4)]
        PT = [f.sb([128, 512], BF16, "pt%d" % i, stack=st) for i in range(4)]
        oacc = [f.sb([128, 512], F32, "oacc%d" % i, stack=st) for i in range(2)]
        onb = [f.sb([128, 512], BF16, "onb%d" % i, stack=st) for i in range(2)]
        onT = [f.sb([128, 4, 128], BF16, "onT%d" % i, stack=st) for i in range(2)]
        imp = f.sb([128, 128], F32, "imp", stack=st)
        sc1 = f.sb([128, 128], F32, "sc1", stack=st)
        sc2 = f.sb([128, 128], F32, "sc2", stack=st)
        m16 = f.sb([128, 16], F32, "m16", stack=st)
        nsb = f.sb([128, 128], BF16, "nsb", stack=st)
        nselT = [f.sb([128, 512], BF16, "nselT%d" % i, stack=st) for i in range(2)]
        rden = [f.sb([128, 4], F32, "rden%d" % i, stack=st) for i in range(3)]
        scl = [f.sb([128, 4], F32, "scl%d" % i, stack=st) for i in range(3)]
        tmpo = [f.sb([128, 4, 64], F32, "tmpo%d" % i, stack=st) for i in range(2)]
        ST = [PS[0], PS[1]]
        OC, OS_, OW, IMP = PS[2], PS[3], PS[4], PS[5]
        sti = [0]
        pti = [0]

        def score_tile(lhs_k, rhs_q, extra):
            ps = ST[sti[0] % 2]
            sti[0] += 1
            n = 1 + len(extra)
            f.matmul(ps, lhs_k, rhs_q, start=True, stop=(n == 1))
            for i, (a, b) in enumerate(extra):
                f.matmul(ps, a, b, start=False, stop=(i == len(extra) - 1))
            return ps

        for qt in range(0 if SKIP3 else NT):
            f.maybe_switch()
            qTt = qT_b[qt % 2]
            f.dma("sp", qTt.v().rearrange("d (h t) -> d h t", h=8), qT_d.rearrange("h d s -> d h s")[:, :, qt * 128:(qt + 1) * 128])
            gt = gt_b[qt % 2]
            f.dma("sp", gt, gate_d[qt * 128:(qt + 1) * 128, :])
            oa = oacc[qt % 2]
            for g in range(2):
                rq = qTt[:, g * 512:(g + 1) * 512]
                ctl = qt // 16
                for ct in range(ctl + 1):
                    extra = [(identb, negcmp[:, qt % 16, :])] if ct == ctl else []
                    ps = score_tile(kc_s[:, g, ct * 128:(ct + 1) * 128], rq, extra)
                    pt = PTc[ct]
                    f.act(pt, ps, AF.Exp)
                    for r in range(4):
                        f.matmul(OC[:, r * 65:(r + 1) * 65], pt[:, r * 128:(r + 1) * 128], vc_s[:, ct, g, :], start=(ct == 0 and r == 0), stop=(ct == ctl))
                        f.matmul(IMP[:, r * 128:(r + 1) * 128], pt[:, r * 128:(r + 1) * 128], ovl[:, ct, :], start=(ct == 0 and r == 0), stop=(ct == ctl))
                ocv = OC[:, 0:260].rearrange("p (r c) -> p r c", r=4)
                f.ts("dve", rden[0], ocv[:, :, 64], 1e-30, None, ALU.max)
                f.op("dve", lambda: nc.vector.reciprocal(out=rden[0].ap, in_=rden[0].ap), [rden[0]], [rden[0]])
                f.ts("dve", imp, IMP[:, 0:128], rden[0][:, 0:1], None, ALU.mult)
                for r in range(1, 4):
                    f.stt("dve", imp, IMP[:, r * 128:(r + 1) * 128], rden[0][:, r:r + 1], imp, ALU.mult, ALU.add)
                off = 128 - 2 * qt
                f.tt("dve", sc1[:, 0:NJ], imp[:, 0:NJ], vm[:, off:off + NJ], ALU.mult)
                f.tt("dve", sc1[:, 0:NJ], sc1[:, 0:NJ], am[:, off:off + NJ], ALU.add)
                f.memset("dve", sc1[:, 0:1], 1.0e4)
                if NJ < 128:
                    f.memset("dve", sc1[:, NJ:128], -2.0)
                f.op("dve", lambda: nc.vector.max(out=m16.ap[:, 0:8], in_=sc1.ap), [sc1], [m16])
                f.op("dve", lambda: nc.vector.match_replace(out=sc2.ap, in_to_replace=m16.ap[:, 0:8], in_values=sc1.ap, imm_value=-1e9), [sc1, m16], [sc2])
                f.op("dve", lambda: nc.vector.max(out=m16.ap[:, 8:16], in_=sc2.ap), [sc2], [m16])
                f.op("dve", lambda: nc.vector.match_replace(out=sc1.ap, in_to_replace=m16.ap[:, 8:16], in_values=sc2.ap, imm_value=-1e9), [sc2, m16], [sc1])
                f.ts("dve", nsb, sc1, -1e8, NEG, ALU.is_gt, ALU.mult)
                f.transpose(PSB[:, 0:128], nsb, identb)
                nsT = nselT[g]
                for r in range(4):
                    f.copy(rot(["dve", "pool"]) if False else "dve", nsT[:, r * 128:(r + 1) * 128], PSB[:, 0:128])
                for kt in range(qt + 1):
                    extra = [(etab[:, kt, :], nsT)]
                    if kt == qt:
                        extra.append((identb, negcau))
                    ps = score_tile(kT_s[:, g, kt * 128:(kt + 1) * 128], rq, extra)
                    pt = PT[pti[0] % 4]
                    pti[0] += 1
                    f.act(pt, ps, AF.Exp)
                    if dump and qt == 1 and g == 0:
                        f.dma("sp", dbg_d[kt], pt)
                        if kt == 0:
                            f.dma("sp", dbg_d[4], nsT)
                    for r in range(4):
                        f.matmul(OS_[:, r * 65:(r + 1) * 65], pt[:, r * 128:(r + 1) * 128], va_s[:, kt, g, :], start=(kt == 0 and r == 0), stop=(kt == qt))
                k0 = max(0, qt - 4)
                for kt in range(k0, qt + 1):
                    extra = []
                    if kt == qt - 4:
                        extra.append((identb, negwl))
                    if kt == qt:
                        extra.append((identb, negcau))
                    ps = score_tile(kT_s[:, 2 + g, kt * 128:(kt + 1) * 128], rq, extra)
                    pt = PT[pti[0] % 4]
                    pti[0] += 1
                    f.act(pt, ps, AF.Exp)
                    if dump and qt == 1 and g == 0:
                        f.dma("sp", dbg_d[2 + kt], pt)
                    for r in range(4):
                        f.matmul(OW[:, r * 65:(r + 1) * 65], pt[:, r * 128:(r + 1) * 128], va_s[:, kt, 2 + g, :], start=(kt == k0 and r == 0), stop=(kt == qt))
                if dump and qt == 1 and g == 0:
                    for bi_, O_ in enumerate([OC, OS_, OW]):
                        dt_ = f.sb([128, 260], F32, "dbgt", stack=st)
                        f.copy("dve", dt_, O_[:, 0:260])
                        f.dma("sp", dbg2_d[bi_], dt_)
                gv = gt.v().rearrange("p (g r b) -> p g r b", g=2, r=4)
                for br, O in enumerate([OC, OS_, OW]):
                    ov_ = O[:, 0:260].rearrange("p (r c) -> p r c", r=4)
                    if br > 0:
                        f.ts("dve", rden[br], ov_[:, :, 64], 1e-30, None, ALU.max)
                        f.op("dve", lambda br=br: nc.vector.reciprocal(out=rden[br].ap, in_=rden[br].ap), [rden[br]], [rden[br]])
                    f.tt("dve", scl[br], rden[br], gv[:, g, :, br], ALU.mult)
                    oav = oa[:, g * 256:(g + 1) * 256].rearrange("p (r d) -> p r d", r=4)
                    sb_ = scl[br].v().unsqueeze(2).broadcast_to([128, 4, 64])
                    if br == 0:
                        f.tt("dve", oav, ov_[:, :, 0:64], sb_, ALU.mult)
                    else:
                        tm = tmpo[br % 2]
                        f.tt("dve", tm, ov_[:, :, 0:64], sb_, ALU.mult)
                        f.tt("pool", oav, oav, tm, ALU.add)
            ob = onb[qt % 2]
            f.copy("act", ob, oa)
            oT = onT[qt % 2]
            for i in range(4):
                f.transpose(PSB[:, 512 + i * 128:512 + (i + 1) * 128], ob[:, i * 128:(i + 1) * 128], identb)
            f.copy("dve", oT, PSB[:, 512:1024].rearrange("p (i t) -> p i t", i=4))
            f.dma("sp", onT_d.rearrange("(i p) s -> p i s", p=128)[:, :, qt * 128:(qt + 1) * 128], oT)
        f.barrier()
        st.close()
        if upto == 'P3':
            f.finish()
            return nc, hc

        st = ExitStack()
        pw = f.sb([128, 4, 128], BF16, "pw", stack=st)
        f.dma("pool", pw, W["pool_w"][l].rearrange("g i o -> i g o"))
        psc = f.sb([128, 4], F32, "psc", stack=st)
        f.dma("sp", psc, W["pool_scale"][l].rearrange("(g p) -> p g", p=128), allow_slow_non_contiguous=True)
        pfix = f.sb([128, 4, 16], F32, "pfix", stack=st)
        f.dma("sp", pfix, C["poolfix"])
        ub_b = [f.sb([128, 4, 528], BF16, "ub%d" % i, stack=st) for i in range(2)]
        wa = [f.sb([128, 528], F32, "wa%d" % i, stack=st) for i in range(2)]
        wb = [f.sb([128, 528], F32, "wb%d" % i, stack=st) for i in range(2)]
        pin = [f.sb([128, 512], BF16, "pin%d" % i, stack=st) for i in range(2)]
        opst = [f.sb([128, 4, 512], BF16, "opst%d" % i, stack=st) for i in range(2)]
        uTv = uT_d.rearrange("(i p) s -> p i s", p=128)
        for tb in range(NTB):
            ub = ub_b[tb % 2]
            f.dma("sp", ub, uTv[:, 0:4, tb * 512:tb * 512 + 528])
            os_ = opst[tb % 2]
            for g in range(4):
                e = "dve" if g % 2 == 0 else "pool"
                a, b = wa[g % 2], wb[g % 2]
                src = ub[:, g, :]
                sh = 1
                cur = None
                for step in range(g + 1):
                    dst = a if step % 2 == 0 else b
                    s_in = src if cur is None else cur
                    f.tt(e, dst[:, sh:528], s_in[:, sh:528], s_in[:, 0:528 - sh], ALU.add)
                    cur = dst
                    sh *= 2
                w_ = 2 ** (g + 1)
                if tb == 0:
                    f.tt(e, cur[:, 16:32], cur[:, 16:32], pfix[:, g, :], ALU.mult)
                f.stt("dve", pin[g % 2], cur[:, 16:528], 1.0 / w_, ub[:, g, 16:528], ALU.mult, ALU.subtract)
                ps = PS[g % 2]
                f.matmul(ps, pw[:, g, :], pin[g % 2])
                f.act(os_[:, g, :], ps, AF.Copy, scale=psc[:, g:g + 1])
            f.dma("sp", opT_d.rearrange("(i p) s -> p i s", p=128)[:, :, tb * 512:(tb + 1) * 512], os_)
        f.barrier()
        st.close()
        if upto == 'P4a':
            f.finish()
            return nc, hc

        st = ExitStack()
        lam_n = f.sb([32, 2, 64], F32, "lam_n", stack=st)
        f.dma("sp", lam_n[:, 0, :], W["ssm_lam_re"][l])
        f.dma("sp", lam_n[:, 1, :], W["ssm_lam_im"][l])
        lrT = f.sb([64, 32], F32, "lrT", stack=st)
        liT = f.sb([64, 32], F32, "liT", stack=st)
        f.transpose(PS[0][0:64, 0:32], lam_n[:, 0, :], identf[0:32, 0:32])
        f.copy("dve", lrT, PS[0][0:64, 0:32])
        f.transpose(PS[0][0:64, 32:64], lam_n[:, 1, :], identf[0:32, 0:32])
        f.copy("dve", liT, PS[0][0:64, 32:64])
        dtT = f.sb([64, 32], F32, "dtT", stack=st)
        f.dma("sp", dtT, W["ssm_log_dt"][l:l + 1, :].partition_broadcast(64))
        f.act(dtT, dtT, AF.Exp)
        sv = f.sb([128, 1], F32, "sv", stack=st)
        f.dma("sp", sv, C["svals"])
        tv = f.sb([64, 129], F32, "tv", stack=st)
        f.dma("sp", tv, C["tvals"])
        lrdt = f.sb([64, 32], F32, "lrdt", stack=st)
        th = f.sb([64, 32], F32, "th", stack=st)
        f.tt("dve", lrdt, lrT, dtT, ALU.mult)
        f.tt("dve", th, liT, dtT, ALU.mult)
        INV2PI = 1.0 / (2 * math.pi)

        def sincos(st2, arg, shape, sin_out, cos_out, e="dve"):
            ki = f.sb(shape, I32, stack=st2)
            kf = f.sb(shape, F32, stack=st2)
            r_ = f.sb(shape, F32, stack=st2)
            for (outp, shift) in ((sin_out, 0.0), (cos_out, math.pi / 2)):
                f.ts(e, kf, arg, shift, INV2PI, ALU.add, ALU.mult)
                f.copy(e, ki, kf)
                f.copy(e, kf, ki)
                f.ts(e, r_, arg, shift, None, ALU.add)
                f.stt(e, r_, kf, -2 * math.pi, r_, ALU.mult, ALU.add)
                f.ts(e, r_, r_, -3.1415925, 3.1415925, ALU.max, ALU.min)
                f.act(outp, r_, AF.Sin)

        Dpr = f.sb([64, 32, 129], F32, "Dpr", stack=st)
        Dpi = f.sb([64, 32, 129], F32, "Dpi", stack=st)
        st2 = ExitStack()
        argp = f.sb([64, 32, 129], F32, stack=st2)
        magp = f.sb([64, 32, 129], F32, stack=st2)
        tvb = tv.v().unsqueeze(1).broadcast_to([64, 32, 129])
        f.tt("dve", argp, th.v().unsqueeze(2).broadcast_to([64, 32, 129]), tvb, ALU.mult)
        f.tt("pool", magp, lrdt.v().unsqueeze(2).broadcast_to([64, 32, 129]), tvb, ALU.mult)
        f.act(magp, magp, AF.Exp)
        sincos(st2, argp, [64, 32, 129], Dpi, Dpr)
        f.tt("dve", Dpr, Dpr, magp, ALU.mult)
        f.tt("dve", Dpi, Dpi, magp, ALU.mult)
        f.barrier()
        st2.close()
        if P4BSTOP == 1:
            f.finish()
            return nc, hc
        Dmr = f.sb([128, 2048], F32, "Dmr", stack=st)
        Dmi = f.sb([128, 2048], F32, "Dmi", stack=st)
        st2 = ExitStack()
        lrow = f.sb([128, 2, 2048], F32, stack=st2)
        f.dma("sp", lrow[:, 0, :], W["ssm_lam_re"][l:l + 1].rearrange("o g p -> o (g p)").partition_broadcast(128))
        f.dma("sp", lrow[:, 1, :], W["ssm_lam_im"][l:l + 1].rearrange("o g p -> o (g p)").partition_broadcast(128))
        dtr = f.sb([128, 32], F32, stack=st2)
        f.dma("sp", dtr, W["ssm_log_dt"][l:l + 1, :].partition_broadcast(128))
        f.act(dtr, dtr, AF.Exp)
        f.ts("dve", dtr, dtr, sv[:, 0:1], None, ALU.mult)
        dtb = dtr.v().unsqueeze(2).broadcast_to([128, 32, 64])
        argm = f.sb([128, 2048], F32, stack=st2)
        magm = f.sb([128, 2048], F32, stack=st2)
        f.tt("dve", argm.v().rearrange("p (g q) -> p g q", g=32), lrow[:, 1, :].rearrange("p (g q) -> p g q", g=32), dtb, ALU.mult)
        f.tt("pool", magm.v().rearrange("p (g q) -> p g q", g=32), lrow[:, 0, :].rearrange("p (g q) -> p g q", g=32), dtb, ALU.mult)
        f.act(magm, magm, AF.Exp, scale=-1.0)
        sincos(st2, argm, [128, 2048], Dmi, Dmr)
        f.tt("dve", Dmr, Dmr, magm, ALU.mult)
        f.ts("dve", Dmi, Dmi, -1.0, None, ALU.mult)
        f.tt("dve", Dmi, Dmi, magm, ALU.mult)
        f.barrier()
        st2.close()
        if P4BSTOP == 2:
            f.finish()
            return nc, hc
        BD = f.sb([128, 4, 2, 512], BF16, "BD", stack=st)
        CTp = f.sb([64, 2, 32, 128], BF16, "CTp", stack=st)
        st2 = ExitStack()
        arb = f.sb([64, 32], F32, stack=st2)
        aib = f.sb([64, 32], F32, stack=st2)
        f.copy("dve", arb, Dpr[:, :, 1])
        f.copy("dve", aib, Dpi[:, :, 1])
        den = f.sb([64, 32], F32, stack=st2)
        t1 = f.sb([64, 32], F32, stack=st2)
        t2 = f.sb([64, 32], F32, stack=st2)
        crr = f.sb([64, 32], F32, stack=st2)
        cii = f.sb([64, 32], F32, stack=st2)
        f.tt("dve", den, lrT, lrT, ALU.mult)
        f.tt("dve", t1, liT, liT, ALU.mult)
        f.tt("dve", den, den, t1, ALU.add)
        f.op("dve", lambda: nc.vector.reciprocal(out=den.ap, in_=den.ap), [den], [den])
        f.ts("dve", arb, arb, -1.0, None, ALU.add)
        f.tt("dve", t1, arb, lrT, ALU.mult)
        f.tt("dve", t2, aib, liT, ALU.mult)
        f.tt("dve", crr, t1, t2, ALU.add)
        f.tt("dve", crr, crr, den, ALU.mult)
        f.tt("dve", t1, aib, lrT, ALU.mult)
        f.tt("dve", t2, arb, liT, ALU.mult)
        f.tt("dve", cii, t1, t2, ALU.subtract)
        f.tt("dve", cii, cii, den, ALU.mult)
        bre = f.sb([64, 32, 16], F32, stack=st2)
        bim = f.sb([64, 32, 16], F32, stack=st2)
        f.dma("sp", bre, W["ssm_b_re"][l].rearrange("g p h -> p g h"))
        f.dma("sp", bim, W["ssm_b_im"][l].rearrange("g p h -> p g h"))
        crb = crr.v().unsqueeze(2).broadcast_to([64, 32, 16])
        cib = cii.v().unsqueeze(2).broadcast_to([64, 32, 16])
        bbr = f.sb([64, 32, 16], F32, stack=st2)
        bbi = f.sb([64, 32, 16], F32, stack=st2)
        tb1 = f.sb([64, 32, 16], F32, stack=st2)
        f.tt("dve", bbr, bre, crb, ALU.mult)
        f.tt("dve", tb1, bim, cib, ALU.mult)
        f.tt("dve", bbr, bbr, tb1, ALU.subtract)
        f.tt("dve", bbi, bim, crb, ALU.mult)
        f.tt("dve", tb1, bre, cib, ALU.mult)
        f.tt("dve", bbi, bbi, tb1, ALU.add)
        gmask = f.sb([128, 8], F32, stack=st2)
        f.dma("sp", gmask, C["gmask"])
        for o in range(4):
            for ri, bb in enumerate((bbr, bbi)):
                f.transpose(PS[ri][:, 0:64], bb[:, 8 * o:8 * o + 8, :].rearrange("p g h -> p (g h)"), identf[0:64, 0:64])
                for gg in range(8):
                    f.ts("dve", BD[:, o, ri, gg * 64:(gg + 1) * 64], PS[ri][:, 0:64], gmask[:, gg:gg + 1], None, ALU.mult)
        f.memset("pool", CTp, 0.0)
        cn = f.sb([128, 4, 2, 64], F32, stack=st2)
        f.dma("sp", cn[:, :, 0, :], W["ssm_c_re"][l].rearrange("(o g) h p -> (g h) o p", o=4))
        f.dma("sp", cn[:, :, 1, :], W["ssm_c_im"][l].rearrange("(o g) h p -> (g h) o p", o=4))
        for o in range(4):
            for ri in range(2):
                f.transpose(PS[2 + ri][0:64, 0:128], cn[:, o, ri, :], identf)
                for gg in range(8):
                    g_ = 8 * o + gg
                    if ri == 0:
                        f.copy("dve", CTp[:, 0, g_, gg * 16:(gg + 1) * 16], PS[2][0:64, gg * 16:(gg + 1) * 16])
                    else:
                        f.ts("dve", CTp[:, 1, g_, gg * 16:(gg + 1) * 16], PS[3][0:64, gg * 16:(gg + 1) * 16], -1.0, None, ALU.mult)
        f.barrier()
        st2.close()
        if P4BSTOP == 3:
            f.finish()
            return nc, hc
        wglu = f.sb([128, 4, 512], BF16, "wglu", stack=st)
        f.dma("pool", wglu, W["ssm_w_glu"][l].rearrange("(i p) o -> p i o", p=128))
        bglu = f.sb([128, 4], F32, "bglu", stack=st)
        f.dma("sp", bglu, W["ssm_b_glu"][l].rearrange("(g p) -> p g", p=128), allow_slow_non_contiguous=True)
        dsk = f.sb([128, 4], F32, "dsk", stack=st)
        f.dma("sp", dsk, W["ssm_d"][l].rearrange("(g p) -> p g", p=128), allow_slow_non_contiguous=True)
        trib = f.sb([128, 128], BF16, "trib", stack=st)
        f.dma("sp", trib, C["tri"])
        carry = f.sb([64, 2, 32], F32, "carry", stack=st)
        f.memset("dve", carry, 0.0)
        gcl = f.sb([64, 2, 32], F32, "gcl", stack=st)
        us_b = [f.sb([128, 4, 128], BF16, "usb%d" % i, stack=st) for i in range(2)]
        Zr = [f.sb([128, 512], BF16, "Zr%d" % i, stack=st) for i in range(2)]
        Zi = [f.sb([128, 512], BF16, "Zi%d" % i, stack=st) for i in range(2)]
        zt1 = [f.sb([128, 512], F32, "zt1%d" % i, stack=st) for i in range(2)]
        zt2 = [f.sb([128, 512], F32, "zt2%d" % i, stack=st) for i in range(2)]
        GCr = [f.sb([64, 8, 128], F32, "GCr%d" % i, stack=st) for i in range(2)]
        GCi = [f.sb([64, 8, 128], F32, "GCi%d" % i, stack=st) for i in range(2)]
        ht1 = [f.sb([64, 8, 128], F32, "ht1%d" % i, stack=st) for i in range(2)]
        ht2 = [f.sb([64, 8, 128], F32, "ht2%d" % i, stack=st) for i in range(2)]
        Hr = [f.sb([64, 8, 128], BF16, "Hr%d" % i, stack=st) for i in range(2)]
        Hi = [f.sb([64, 8, 128], BF16, "Hi%d" % i, stack=st) for i in range(2)]
        ysb = f.sb([128, 4, 128], F32, "ysb", stack=st)
        zT = [f.sb([128, 4, 128], BF16, "zT%d" % i, stack=st) for i in range(2)]
        sg = f.sb([128, 128], F32, "sg", stack=st)
        osst = [f.sb([128, 4, 128], BF16, "osst%d" % i, stack=st) for i in range(2)]
        ct1 = f.sb([64, 32], F32, "ct1", stack=st)
        ct2 = f.sb([64, 32], F32, "ct2", stack=st)
        usv = uT_d.rearrange("(i p) s -> p i s", p=128)
        if ARM == "P4b":
            f.arm()
        for ch in range(0 if SKIP4B else NT):
            f.maybe_switch()
            us = us_b[ch % 2]
            f.dma("sp", us, usv[:, 4:8, 16 + ch * 128:16 + (ch + 1) * 128])
            zt_ = zT[ch % 2]
            for o in range(4):
                k = o % 2
                f.matmul(PS[0], us[:, o, :], BD[:, o, 0, :])
                f.matmul(PS[1], us[:, o, :], BD[:, o, 1, :])
                dmr = Dmr[:, o * 512:(o + 1) * 512]
                dmi = Dmi[:, o * 512:(o + 1) * 512]
                f.tt("dve", zt1[k], PS[0], dmr, ALU.mult)
                f.tt("dve", zt2[k], PS[1], dmi, ALU.mult)
                f.tt("pool", Zr[k], zt1[k], zt2[k], ALU.subtract)
                f.tt("dve", zt1[k], PS[0], dmi, ALU.mult)
                f.tt("dve", zt2[k], PS[1], dmr, ALU.mult)
                f.tt("pool", Zi[k], zt1[k], zt2[k], ALU.add)
                for gg in range(8):
                    f.matmul(PS[2 + gg // 4][0:64, (gg % 4) * 128:(gg % 4 + 1) * 128], Zr[k][:, gg * 64:(gg + 1) * 64], trib)
                    f.matmul(PS[4 + gg // 4][0:64, (gg % 4) * 128:(gg % 4 + 1) * 128], Zi[k][:, gg * 64:(gg + 1) * 64], trib)
                for hh in range(2):
                    cb_r = carry[:, 0, 8 * o + 4 * hh:8 * o + 4 * hh + 4].unsqueeze(2).broadcast_to([64, 4, 128])
                    cb_i = carry[:, 1, 8 * o + 4 * hh:8 * o + 4 * hh + 4].unsqueeze(2).broadcast_to([64, 4, 128])
                    f.tt("dve", GCr[k][:, 4 * hh:4 * hh + 4, :], PS[2 + hh][0:64, :].rearrange("p (g t) -> p g t", g=4), cb_r, ALU.add)
                    f.tt("dve", GCi[k][:, 4 * hh:4 * hh + 4, :], PS[4 + hh][0:64, :].rearrange("p (g t) -> p g t", g=4), cb_i, ALU.add)
                f.copy("pool", gcl[:, 0, 8 * o:8 * o + 8], GCr[k][:, :, 127])
                f.copy("pool", gcl[:, 1, 8 * o:8 * o + 8], GCi[k][:, :, 127])
                dpr = Dpr[:, 8 * o:8 * o + 8, 0:128]
                dpi = Dpi[:, 8 * o:8 * o + 8, 0:128]
                f.tt("pool", ht1[k], GCr[k], dpr, ALU.mult)
                f.tt("dve", ht2[k], GCi[k], dpi, ALU.mult)
                f.tt("pool", Hr[k], ht1[k], ht2[k], ALU.subtract)
                f.tt("pool", ht1[k], GCr[k], dpi, ALU.mult)
                f.tt("dve", ht2[k], GCi[k], dpr, ALU.mult)
                f.tt("pool", Hi[k], ht1[k], ht2[k], ALU.add)
                yp = PS[6]
                for gg in range(8):
                    f.matmul(yp[:, 0:128], CTp[:, 0, 8 * o + gg, :], Hr[k][:, gg, :], start=(gg == 0), stop=False)
                    f.matmul(yp[:, 0:128], CTp[:, 1, 8 * o + gg, :], Hi[k][:, gg, :], start=False, stop=(gg == 7))
                f.ts("pool", ysb[:, o, :], us[:, o, :], dsk[:, o:o + 1], None, ALU.mult)
                f.tt("dve", ysb[:, o, :], yp[:, 0:128], ysb[:, o, :], ALU.add)
                f.act(zt_[:, o, :], ysb[:, o, :], AF.Gelu)
            l128r = Dpr[:, :, 128]
            l128i = Dpi[:, :, 128]
            f.tt("dve", ct1, gcl[:, 0, :], l128r, ALU.mult)
            f.tt("dve", ct2, gcl[:, 1, :], l128i, ALU.mult)
            f.tt("dve", carry[:, 0, :], ct1, ct2, ALU.subtract)
            f.tt("dve", ct1, gcl[:, 0, :], l128i, ALU.mult)
            f.tt("dve", ct2, gcl[:, 1, :], l128r, ALU.mult)
            f.tt("dve", carry[:, 1, :], ct1, ct2, ALU.add)
            oss = osst[ch % 2]
            for co in range(4):
                gp = PS[0] if co % 2 == 0 else PS[1]
                for ci_ in range(4):
                    f.matmul(gp[:, 0:128], wglu[:, ci_, co * 128:(co + 1) * 128], zt_[:, ci_, :], start=(ci_ == 0), stop=(ci_ == 3))
                f.act(sg, gp[:, 0:128], AF.Sigmoid, bias=bglu[:, co:co + 1])
                f.tt("pool", oss[:, co, :], zt_[:, co, :], sg, ALU.mult)
            f.dma("sp", osT_d.rearrange("(i p) s -> p i s", p=128)[:, :, ch * 128:(ch + 1) * 128], oss)
        f.barrier()
        st.close()
        if upto == 'P4b':
            f.finish()
            return nc, hc

        st = ExitStack()
        if SKIP3 or SKIP4B:
            zz = f.sb([128, 4, S], BF16, "zz", stack=st)
            f.memset("dve", zz, 0.0)
            if SKIP3:
                f.dma("sp", onT_d.rearrange("(i p) s -> p i s", p=128), zz)
            if SKIP4B:
                f.dma("sp", osT_d.rearrange("(i p) s -> p i s", p=128), zz)
            f.barrier()
        wbg = f.sb([128, 8, 3072], BF16, "wbg", stack=st)
        for kt in range(8):
            f.dma("pool", wbg[:, kt, :], wv[:, kt, C_BG:INW])
        wup = f.sb([128, 3, 4, D], BF16, "wup", stack=st)
        for bi, nm_ in enumerate(["w_up_nsa", "w_up_pool", "w_up_ssm"]):
            f.dma("pool", wup[:, bi], W[nm_][l].rearrange("(i p) o -> p i o", p=128))
        wo = f.sb([128, 8, D], BF16, "wo", stack=st)
        f.dma("pool", wo, W["w_out"][l].rearrange("(k p) o -> p k o", p=128))
        g1 = f.sb([128, D], F32, "g1", stack=st)
        b1_ = f.sb([128, D], F32, "b1_", stack=st)
        f.dma("sp", g1, W["ln1_g"][l:l + 1, :].partition_broadcast(128))
        f.dma("sp", b1_, W["ln1_b"][l:l + 1, :].partition_broadcast(128))
        xs_b = [f.sb([128, 8, 512], BF16, "p5x%d" % i, stack=st) for i in range(2)]
        ob_b = [f.sb([128, 3, 4, 512], BF16, "p5o%d" % i, stack=st) for i in range(2)]
        xr_b = [f.sb([128, D], F32, "p5r%d" % i, stack=st) for i in range(2)]
        sig = [f.sb([128, 512], F32, "sig%d" % i, stack=st) for i in range(2)]
        mg = [f.sb([128, 512], F32, "mg%d" % i, stack=st) for i in range(2)]
        tm5 = [f.sb([128, 512], F32, "tm5%d" % i, stack=st) for i in range(2)]
        mT = f.sb([128, 8, 512], BF16, "mT", stack=st)
        hb = [f.sb([128, D], F32, "hb%d" % i, stack=st) for i in range(2)]
        x1b = [f.sb([128, D], F32, "x1b%d" % i, stack=st) for i in range(2)]
        srcs = [onT_d, opT_d, osT_d]
        cnt = 0
        for tb in range(NTB):
            f.maybe_switch()
            xs = xs_b[tb % 2]
            ob = ob_b[tb % 2]
            tsl = slice(tb * 512, (tb + 1) * 512)
            f.dma("sp", xs, xT_d.rearrange("(k p) s -> p k s", p=128)[:, :, tsl])
            for bi in range(3):
                f.dma("sp", ob[:, bi], srcs[bi].rearrange("(i p) s -> p i s", p=128)[:, :, tsl])
            for co in range(8):
                m_ = mg[co % 2]
                for bi in range(3):
                    pg = PS[cnt % 2]
                    pu = PS[2 + cnt % 2]
                    cnt += 1
                    for kt in range(8):
                        f.matmul(pg, wbg[:, kt, bi * 1024 + co * 128: bi * 1024 + (co + 1) * 128], xs[:, kt, :], start=(kt == 0), stop=(kt == 7))
                    for i in range(4):
                        f.matmul(pu, wup[:, bi, i, co * 128:(co + 1) * 128], ob[:, bi, i, :], start=(i == 0), stop=(i == 3))
                    sg_ = sig[cnt % 2]
                    f.act(sg_, pg, AF.Sigmoid)
                    if bi == 0:
                        f.tt("dve", m_, pu, sg_, ALU.mult)
                    elif bi == 1:
                        t_ = tm5[cnt % 2]
                        f.tt("dve", t_, pu, sg_, ALU.mult)
                        f.tt("pool", m_, m_, t_, ALU.add)
                    else:
                        t_ = tm5[cnt % 2]
                        f.tt("dve", t_, pu, sg_, ALU.mult)
                        f.tt("pool", mT[:, co, :], m_, t_, ALU.add)
            for sub in range(4):
                t0 = tb * 512 + sub * 128
                xr = xr_b[sub % 2]
                f.dma("sp", xr, xsrc[t0:t0 + 128, :])
                h_ = hb[sub % 2]
                for hf in range(2):
                    po = PS[4 + hf]
                    for co in range(8):
                        f.matmul(po, mT[:, co, sub * 128:(sub + 1) * 128], wo[:, co, hf * 512:(hf + 1) * 512], start=(co == 0), stop=(co == 7))
                    f.stt("dve", h_[:, hf * 512:(hf + 1) * 512], po, 1.0 / ALPHA, xr[:, hf * 512:(hf + 1) * 512], ALU.mult, ALU.add)
                x1 = x1b[sub % 2]
                layer_norm(st, h_, g1, b1_, x1, eps=1e-5 / (ALPHA * ALPHA))
                f.dma("sp", x1_d[t0:t0 + 128, :], x1)
        f.barrier()
        st.close()
        if upto == 'P5':
            f.finish()
            return nc, hc

        st = ExitStack()
        TB = min(S, 2048)
        NSUB = TB // 128
        wr = f.sb([128, 8, 36], F32, "wr", stack=st)
        f.dma("sp", wr[:, :, 0:4], W["router_w_grp"][l].rearrange("(k p) n -> p k n", p=128))
        f.dma("sp", wr[:, :, 4:36], W["router_w_exp"][l].rearrange("(k p) n -> p k n", p=128))
        wrh = f.sb([128, 8, 36], BF16, "wrh", stack=st)
        wrl = f.sb([128, 8, 36], BF16, "wrl", stack=st)
        wrt = f.sb([128, 8, 36], F32, "wrt", stack=st)
        f.copy("dve", wrh, wr)
        f.copy("dve", wrt, wrh)
        f.tt("dve", wrt, wr, wrt, ALU.subtract)
        f.copy("dve", wrl, wrt)
        xsplit = [f.sb([128, 8, 128], F32, "xsp0", stack=st), f.sb([128, 8, 128], BF16, "xsp1", stack=st)]
        xb16 = f.sb([128, 8, 128], BF16, "xb16", stack=st)
        br_ = f.sb([128, 36], F32, "br_", stack=st)
        f.dma("sp", br_[:, 0:4], W["router_b_grp"][l:l + 1, :].partition_broadcast(128))
        f.dma("sp", br_[:, 4:36], W["router_b_exp"][l:l + 1, :].partition_broadcast(128))
        g2 = f.sb([128, D], F32, "g2", stack=st)
        b2_ = f.sb([128, D], F32, "b2_", stack=st)
        f.dma("sp", g2, W["ln2_g"][l:l + 1, :].partition_broadcast(128))
        f.dma("sp", b2_, W["ln2_b"][l:l + 1, :].partition_broadcast(128))
        x1T = f.sb([128, 8, TB], BF16, "x1T", stack=st)
        acc = f.sb([128, NSUB, D], F32, "acc", stack=st)
        gw = f.sb([128, NSUB, 32], F32, "gw", stack=st)
        x1f = [f.sb([128, D], F32, "x1f%d" % i, stack=st) for i in range(2)]
        x1Tf = [f.sb([128, 8, 128], F32, "x1Tf%d" % i, stack=st) for i in range(2)]
        wgb = [f.sb([128, 8, FF], BF16, "wgb%d" % i, stack=st) for i in range(2)]
        wub = [f.sb([128, 8, FF], BF16, "wub%d" % i, stack=st) for i in range(2)]
        wdb = [f.sb([128, 4, D], BF16, "wdb%d" % i, stack=st) for i in range(2)]
        sil = [f.sb([128, 512], F32, "sil%d" % i, stack=st) for i in range(2)]
        hT_ = [f.sb([128, 4, 512], BF16, "hT_%d" % i, stack=st) for i in range(2)]
        ytmp = [f.sb([128, 512], F32, "ytmp%d" % i, stack=st) for i in range(2)]
        lg = f.sb([128, 36], F32, "lg", stack=st)
        r4 = f.sb([128, 8], F32, "r4", stack=st)
        oh4 = f.sb([128, 4], F32, "oh4", stack=st)
        sl8 = f.sb([128, 8], F32, "sl8", stack=st)
        sl16 = f.sb([128, 16], F32, "sl16", stack=st)
        f.memset("dve", sl16, -1.0e30)
        e8 = f.sb([128, 8], F32, "e8", stack=st)
        mk1 = f.sb([128, 8], F32, "mk1", stack=st)
        mk2 = f.sb([128, 8], F32, "mk2", stack=st)
        w8 = f.sb([128, 8], F32, "w8", stack=st)
        if ARM == "P6":
            f.arm()
        for blk in range(S // TB):
            for sub in range(NSUB):
                t0 = blk * TB + sub * 128
                xf = x1f[sub % 2]
                f.dma("sp", xf, x1_d[t0:t0 + 128, :])
                xtf = x1Tf[sub % 2]
                for hh in range(2):
                    ps = PS[hh]
                    for k4 in range(4):
                        kt = hh * 4 + k4
                        f.transpose(ps[:, k4 * 128:(k4 + 1) * 128], xf[:, kt * 128:(kt + 1) * 128], identf)
                    f.act(xtf[:, hh * 4:(hh + 1) * 4, :], ps, AF.Copy)
                    f.copy("pool", x1T[:, hh * 4:(hh + 1) * 4, sub * 128:(sub + 1) * 128], xtf[:, hh * 4:(hh + 1) * 4, :])
                xh = x1T[:, :, sub * 128:(sub + 1) * 128]
                xhf = xsplit[0]
                f.copy("pool", xhf, xh)
                f.tt("pool", xhf, xtf, xhf, ALU.subtract)
                f.copy("pool", xsplit[1], xhf)
                pl = PS[2]
                n_ = 0
                for (xa, wa_) in ((xh, wrh), (xh, wrl), (xsplit[1], wrh)):
                    for kt in range(8):
                        f.matmul(pl[:, 0:36], xa[:, kt, :], wa_[:, kt, :], start=(n_ == 0), stop=(n_ == 23))
                        n_ += 1
                f.tt("dve", lg, pl[:, 0:36], br_, ALU.add)
                f.tt("dve", r4[:, 1:3], lg[:, 0:2], lg[:, 2:4], ALU.max)
                f.tt("dve", r4[:, 0:1], r4[:, 1:2], r4[:, 2:3], ALU.max)
                f.ts("dve", oh4, lg[:, 0:4], r4[:, 0:1], None, ALU.is_ge)
                f.ts("dve", r4[:, 1:5], lg[:, 0:4], r4[:, 0:1], None, ALU.subtract)
                f.act(r4[:, 1:5], r4[:, 1:5], AF.Exp)
                f.tt("dve", r4[:, 1:3], r4[:, 1:3], r4[:, 3:5], ALU.add)
                f.tt("dve", r4[:, 5:6], r4[:, 1:2], r4[:, 2:3], ALU.add)
                f.op("dve", lambda: nc.vector.reciprocal(out=r4.ap[:, 6:7], in_=r4.ap[:, 5:6]), [r4], [r4])
                f.ts("dve", sl8, lg[:, 4:12], oh4[:, 0:1], None, ALU.mult)
                for g_ in range(1, 4):
                    f.stt("dve", sl8, lg[:, 4 + 8 * g_:12 + 8 * g_], oh4[:, g_:g_ + 1], sl8, ALU.mult, ALU.add)
                f.copy("dve", sl16[:, 0:8], sl8)
                f.op("dve", lambda: nc.vector.max(out=e8.ap, in_=sl16.ap), [sl16], [e8])
                f.ts("dve", mk1, sl8, e8[:, 0:1], None, ALU.is_ge)
                f.ts("dve", mk2, sl8, e8[:, 1:2], None, ALU.is_ge)
                f.tt("dve", mk2, mk2, mk1, ALU.subtract)
                f.tt("dve", r4[:, 0:1], e8[:, 1:2], e8[:, 0:1], ALU.subtract)
                f.act(r4[:, 0:1], r4[:, 0:1], AF.Exp)
                f.ts("dve", r4[:, 1:2], r4[:, 0:1], 1.0, None, ALU.add)
                f.op("dve", lambda: nc.vector.reciprocal(out=r4.ap[:, 1:2], in_=r4.ap[:, 1:2]), [r4], [r4])
                f.tt("dve", r4[:, 1:2], r4[:, 1:2], r4[:, 6:7], ALU.mult)
                f.tt("dve", r4[:, 2:3], r4[:, 1:2], r4[:, 0:1], ALU.mult)
                f.ts("dve", w8, mk1, r4[:, 1:2], None, ALU.mult)
                f.stt("dve", w8, mk2, r4[:, 2:3], w8, ALU.mult, ALU.add)
                for g_ in range(4):
                    f.ts("dve", gw[:, sub, 8 * g_:8 * g_ + 8], w8, oh4[:, g_:g_ + 1], None, ALU.mult)
            if P6STOP == 1:
                f.finish()
                return nc, hc
            for e_ in range(NEXP):
                f.maybe_switch()
                wg_, wu_, wd_ = wgb[e_ % 2], wub[e_ % 2], wdb[e_ % 2]
                f.dma("pool", wg_, W["moe_w_gate"][l, e_].rearrange("(k p) n -> p k n", p=128))
                f.dma("pool", wu_, W["moe_w_up"][l, e_].rearrange("(k p) n -> p k n", p=128))
                f.dma("pool", wd_, W["moe_w_down"][l, e_].rearrange("(k p) n -> p k n", p=128))
                for cc in range(TB // 512):
                    hT = hT_[cc % 2]
                    for fi in range(4):
                        pg = PS[(fi % 2)]
                        pu = PS[2 + (fi % 2)]
                        for kt in range(8):
                            f.matmul(pg, wg_[:, kt, fi * 128:(fi + 1) * 128], x1T[:, kt, cc * 512:(cc + 1) * 512], start=(kt == 0), stop=(kt == 7))
                        for kt in range(8):
                            f.matmul(pu, wu_[:, kt, fi * 128:(fi + 1) * 128], x1T[:, kt, cc * 512:(cc + 1) * 512], start=(kt == 0), stop=(kt == 7))
                        s_ = sil[fi % 2]
                        f.act(s_, pg, AF.Silu)
                        f.tt("dve", hT[:, fi, :], pu, s_, ALU.mult)
                    for s4 in range(4):
                        sub = cc * 4 + s4
                        for hf in range(2):
                            py = PS[4 + (s4 * 2 + hf) % 3]
                            for fi in range(4):
                                f.matmul(py, hT[:, fi, s4 * 128:(s4 + 1) * 128], wd_[:, fi, hf * 512:(hf + 1) * 512], start=(fi == 0), stop=(fi == 3))
                            a_ = acc[:, sub, hf * 512:(hf + 1) * 512]
                            gsc = gw[:, sub, e_:e_ + 1]
                            if e_ == 0:
                                f.ts("dve", a_, py, gsc, None, ALU.mult)
                            elif (s4 * 2 + hf) % 2 == 0:
                                f.stt("dve", a_, py, gsc, a_, ALU.mult, ALU.add)
                            else:
                                yt = ytmp[s4 % 2]
                                f.act(yt, py, AF.Copy, scale=gsc)
                                f.tt("pool", a_, a_, yt, ALU.add)
            if P6STOP == 2:
                f.finish()
                return nc, hc
            for sub in range(NSUB):
                t0 = blk * TB + sub * 128
                xf = x1f[sub % 2]
                f.dma("sp", xf, x1_d[t0:t0 + 128, :])
                f.stt("dve", acc[:, sub, :], xf, ALPHA, acc[:, sub, :], ALU.mult, ALU.add)
                x2 = x1Tf[sub % 2].v().rearrange("p k t -> p (k t)")
                layer_norm(st, acc[:, sub, :], g2, b2_, x2)
                f.dma("sp", xdst[t0:t0 + 128, :], x2)
        f.barrier()
        st.close()
        if upto == 'P6':
            f.finish()
            return nc, hc

    f.finish()
    print('build done: ninst', f.ninst, 'nsem', f.nsem, {kk: len(v) for kk, v in f.semlist.items() if len(v) > 1})
    return nc, hc


_CACHE = {}


def _get(S, L):
    key = (S, L)
    if key not in _CACHE:
        _CACHE[key] = build(S, L)
    return _CACHE[key]


def kernel(**inputs):
    x = np.asarray(inputs["x"], dtype=np.float32)
    B, S, _ = x.shape
    L = inputs["w_in"].shape[0]
    nc, hc = _get(S, L)
    base = {k: np.ascontiguousarray(np.asarray(inputs[k], dtype=np.float32)) for k in WNAMES}
    for k, v in hc.items():
        base["c_" + k] = np.ascontiguousarray(v)
    ncores = B
    in_maps = []
    for c in range(ncores):
        m = dict(base)
        m["x"] = np.ascontiguousarray(x[c % B])
        in_maps.append(m)
    res = run_bass_kernel_spmd(nc, in_maps, core_ids=list(range(ncores)))
    out = np.stack([np.asarray(res.results[c]["out"]) for c in range(B)], axis=0)
    return out.astype(np.float32)
```

```python
import numpy as np
import concourse.bass as bass
import concourse.mybir as mybir
from contextlib import ExitStack

F32 = mybir.dt.float32
BF16 = mybir.dt.bfloat16
I32 = mybir.dt.int32
AF = mybir.ActivationFunctionType
ALU = mybir.AluOpType
AX = mybir.AxisListType


class Tile:
    def __init__(self, ap, name=""):
        self.ap = ap[:]
        self.name = name
        self.w = None
        self.r = {}

    def __getitem__(self, idx):
        return View(self, self.ap[idx])

    def v(self):
        return View(self, self.ap)


class View:
    def __init__(self, tile, ap):
        self.tile = tile
        self.ap = ap

    def __getitem__(self, idx):
        return View(self.tile, self.ap[idx])

    def rearrange(self, pat, **kw):
        return View(self.tile, self.ap.rearrange(pat, **kw))

    def unsqueeze(self, ax):
        return View(self.tile, self.ap.unsqueeze(ax))

    def broadcast_to(self, shp):
        return View(self.tile, self.ap.broadcast_to(list(shp)))

    def bitcast(self, dt):
        return View(self.tile, self.ap.bitcast(dt))


def _ap(v):
    return v.ap if isinstance(v, (View, Tile)) else v


def _tile(v):
    if isinstance(v, View):
        return v.tile
    if isinstance(v, Tile):
        return v
    return None


class FW:
    NDMA = 6
    import os as _os
    LIMIT = int(_os.environ.get('FWLIMIT', '40000'))

    def __init__(self, nc):
        self.nc = nc
        self.es = ExitStack()
        self.eng = {"pe": nc.tensor, "act": nc.scalar, "dve": nc.vector, "pool": nc.gpsimd, "sp": nc.sync}
        self.semlist = {}
        self.val = {}
        self.last = {}
        self.nsem = 0
        for k in ["pe", "act", "dve", "pool"]:
            self.semlist[k] = [self._newsem(k)]
            self.val[k] = 0
        self.dmaq = {}
        for q in ["sp", "pool"]:
            keys = []
            for i in range(self.NDMA):
                k = "d_%s%d" % (q, i)
                self.semlist[k] = [self._newsem(k)]
                self.val[k] = 0
                keys.append(k)
            self.dmaq[q] = [keys, 0]
        self.seen = {e: {} for e in self.eng}
        self.ninst = 0

    def _newsem(self, k):
        self.nsem += 1
        return self.es.enter_context(self.nc.semaphore("s_%s_%d" % (k, self.nsem)))

    def _bump(self, k, inc):
        if self.val[k] + inc > self.LIMIT:
            self.semlist[k].append(self._newsem(k))
            self.val[k] = 0
        self.val[k] += inc
        idx = len(self.semlist[k]) - 1
        ev = (k, idx, self.val[k])
        self.last[k] = ev
        return self.semlist[k][idx], ev

    def sb(self, shape, dt, name=None, stack=None):
        st = stack if stack is not None else self.es
        self.nalloc = getattr(self, "nalloc", 0) + 1
        nm = "%s_%d" % (name or "t", self.nalloc)
        t = st.enter_context(self.nc.sbuf_tensor(nm, list(shape), dt))
        return Tile(t, nm)

    def ps(self, shape, dt=F32, name=None, stack=None):
        st = stack if stack is not None else self.es
        t = st.enter_context(self.nc.psum_tensor(list(shape), dt))
        return Tile(t, name or "")

    def _wait(self, e, ev):
        if ev is None:
            return
        k, idx, v = ev
        if self.seen[e].get((k, idx), 0) >= v:
            return
        for (k2, i2), v2 in self.seen[e].items():
            if k2 == k and i2 > idx:
                return
        self.eng[e].wait_ge(self.semlist[k][idx], v)
        self.seen[e][(k, idx)] = v

    def _deps(self, e, reads, writes):
        for t in reads:
            if t is None:
                continue
            if t.w is not None and not (e == "pe" and t.w[0] == "pe"):
                self._wait(e, t.w)
        for t in writes:
            if t is None:
                continue
            if t.w is not None and not (e == "pe" and t.w[0] == "pe"):
                self._wait(e, t.w)
            for k, ev in t.r.items():
                if e == "pe" and k == "pe":
                    continue
                self._wait(e, ev)

    def _commit(self, ev, reads, writes):
        for t in reads:
            if t is None:
                continue
            t.r[ev[0]] = ev
        for t in writes:
            if t is None:
                continue
            t.w = ev
            t.r = {}

    armed = False
    budget = 10 ** 9

    def arm(self):
        import os
        self.armed = True
        self.budget = int(os.environ.get("FWBUDGET", str(10 ** 9)))
        self.skipped = 0

    def _spend(self):
        if not self.armed:
            return True
        if self.budget <= 0:
            self.skipped += 1
            return False
        self.budget -= 1
        return True

    def op(self, e, ins_fn, reads, writes):
        if not self._spend():
            return None
        reads = [_tile(r) for r in reads]
        writes = [_tile(w) for w in writes]
        self._deps(e, reads, writes)
        ins = ins_fn()
        sem, ev = self._bump(e, 1)
        ins.then_inc(sem, 1)
        self._commit(ev, reads, writes)
        self.ninst += 1
        return ins

    def dma(self, q, out, in_, **kw):
        if not self._spend():
            return None
        keys, idx = self.dmaq[q]
        k = keys[idx % len(keys)]
        self.dmaq[q][1] = idx + 1
        e = q
        reads = [_tile(in_)]
        writes = [_tile(out)]
        if k in self.last:
            self._wait(e, self.last[k])
        self._deps(e, reads, writes)
        ins = self.eng[e].dma_start(out=_ap(out), in_=_ap(in_), **kw)
        sem, ev = self._bump(k, 16)
        ins.then_inc(sem, 16)
        self._commit(ev, reads, writes)
        self.ninst += 1
        return ins

    def barrier(self, engines=None):
        engines = engines or list(self.eng.keys())
        for e in engines:
            for k, ev in self.last.items():
                self._wait(e, ev)

    def maybe_switch(self, force=False):
        return

    def matmul(self, out, lhsT, rhs, start=True, stop=True):
        return self.op("pe", lambda: self.nc.tensor.matmul(_ap(out), _ap(lhsT), _ap(rhs), start=start, stop=stop),
                       [lhsT, rhs], [out])

    def transpose(self, out, in_, ident):
        return self.op("pe", lambda: self.nc.tensor.transpose(_ap(out), _ap(in_), _ap(ident)), [in_, ident], [out])

    def act(self, out, in_, func, bias=None, scale=None, accum_out=None, e="act"):
        def _fl(v):
            a = _ap(v)
            if len(a.shape) == 3:
                return View(_tile(v), a.rearrange("p a b -> p (a b)"))
            return v
        out = _fl(out)
        in_ = _fl(in_)
        kw = {}
        rd = [in_]
        wr = [out]
        if bias is not None:
            kw["bias"] = _ap(bias)
            if not isinstance(bias, (int, float)):
                rd.append(bias)
        if scale is not None:
            kw["scale"] = _ap(scale)
            if not isinstance(scale, (int, float)):
                rd.append(scale)
        if accum_out is not None:
            kw["accum_out"] = _ap(accum_out)
            wr.append(accum_out)
        return self.op(e, lambda: self.nc.scalar.activation(_ap(out), _ap(in_), func, **kw), rd, wr)

    def tt(self, e, out, a, b, op):
        return self.op(e, lambda: self.eng[e].tensor_tensor(_ap(out), _ap(a), _ap(b), op), [a, b], [out])

    def ts(self, e, out, a, s1, s2, op0, op1=None, accum_out=None):
        rd = [a]
        wr = [out]
        if not isinstance(s1, (int, float)):
            rd.append(s1)
        if s2 is not None and not isinstance(s2, (int, float)):
            rd.append(s2)
        kw = {}
        if op1 is not None:
            kw["op1"] = op1
        if accum_out is not None:
            kw["accum_out"] = _ap(accum_out)
            wr.append(accum_out)
        return self.op(e, lambda: self.eng[e].tensor_scalar(_ap(out), _ap(a), _ap(s1), _ap(s2) if s2 is not None else None, op0, **kw), rd, wr)

    def stt(self, e, out, a, s, b, op0, op1):
        rd = [a, b]
        if not isinstance(s, (int, float)):
            rd.append(s)
        return self.op(e, lambda: self.eng[e].scalar_tensor_tensor(_ap(out), _ap(a), _ap(s), _ap(b), op0, op1), rd, [out])

    def copy(self, e, out, in_):
        if e == "act":
            return self.act(out, in_, AF.Copy)
        return self.op(e, lambda: self.eng[e].tensor_copy(_ap(out), _ap(in_)), [in_], [out])

    def memset(self, e, out, val):
        return self.op(e, lambda: self.eng[e].memset(_ap(out), val), [], [out])

    def reduce(self, e, out, in_, op, axis=AX.X):
        return self.op(e, lambda: self.eng[e].tensor_reduce(_ap(out), _ap(in_), axis, op), [in_], [out])

    def finish(self):
        self.barrier()

import math
import ml_dtypes
from concourse.bass_utils import run_bass_kernel_spmd

D = 1024
NH = 8
HD = 64
NG = 2
RR = 4
INW = 5400
NEXP = 32
FF = 512
DEPTH_FULL = 4
ALPHA = (2 * DEPTH_FULL) ** 0.25
NEG = -30000.0
SLOPES = [2.0 ** (-(h + 1)) for h in range(8)]
C_Q, C_KC, C_VC, C_KS, C_VS, C_KW, C_VW, C_NG, C_UP, C_US, C_BG = 0, 512, 640, 768, 896, 1024, 1152, 1280, 1304, 1816, 2328

WNAMES = ["w_in", "cmp_pe_k", "cmp_pe_v", "cmp_w1_k", "cmp_w2_k", "cmp_w1_v", "cmp_w2_v", "pool_w", "pool_scale",
          "ssm_lam_re", "ssm_lam_im", "ssm_log_dt", "ssm_b_re", "ssm_b_im", "ssm_c_re", "ssm_c_im", "ssm_d",
          "ssm_w_glu", "ssm_b_glu", "w_up_nsa", "w_up_pool", "w_up_ssm", "w_out", "ln1_g", "ln1_b",
          "router_w_grp", "router_b_grp", "router_w_exp", "router_b_exp", "moe_w_gate", "moe_w_up", "moe_w_down",
          "ln2_g", "ln2_b"]


def host_consts(S):
    bf = ml_dtypes.bfloat16
    NC_ = S // 16
    NJ = S // 64
    NT = S // 128
    c = {}
    t = np.arange(S)
    qa = np.zeros((8, 4, S), np.float32)
    for h in range(8):
        sl = SLOPES[h]
        qa[h, 0] = sl * 128
        qa[h, 1] = sl
        qa[h, 2] = -sl * 128 * (t // 128)
        qa[h, 3] = -sl * (t % 128)
    c["qaug"] = qa.astype(bf)
    ka = np.zeros((4, S + 16), np.float32)
    ka[0, :S] = t // 128
    ka[1, :S] = t % 128
    ka[2] = 1
    ka[3] = 1
    c["kaug"] = ka.astype(bf)
    pc = np.arange(NC_) * 16 + 31
    kc = np.zeros((4, NC_), np.float32)
    kc[0] = pc // 128
    kc[1] = pc % 128
    kc[2] = 1
    kc[3] = 1
    c["kaugc"] = kc.astype(bf)
    p = np.arange(128)[:, None]
    q = np.arange(128)[None, :]
    nm = np.zeros((128, 16, 512), np.float32)
    for m in range(16):
        valid = (16 * p + 31) <= (128 * m + q)
        nm[:, m, :] = np.tile(np.where(valid, 0.0, NEG), (1, 4))
    c["negcmp"] = nm.astype(bf)
    c["negcausal"] = np.tile(np.where(p <= q, 0.0, NEG), (1, 4)).astype(bf)
    c["negwinlo"] = np.tile(np.where(p > q, 0.0, NEG), (1, 4)).astype(bf)
    E = np.zeros((128, NT, 128), np.float32)
    for kt in range(NT):
        for pp in range(128):
            j = 2 * kt + pp // 64
            if j < 128:
                E[j, kt, pp] = 1.0
    c["etab"] = E.astype(bf)
    ci = np.arange(NC_)[:, None] * 16
    j0 = np.arange(NJ)[None, :] * 64
    ov = ((ci < j0 + 64) & (ci + 32 > j0)).astype(np.float32)
    nct = max(1, NC_ // 128)
    ovp = np.zeros((nct * 128, 128), np.float32)
    ovp[:NC_, :NJ] = ov
    c["overlap"] = ovp.reshape(nct, 128, 128).transpose(1, 0, 2).copy().astype(bf)
    qq = np.arange(128)[:, None]
    jr = np.arange(256)[None, :] - 128
    tbr = (qq >= 64).astype(np.int64)
    forced = (jr == tbr) | (jr == tbr - 1)
    valid = jr <= tbr
    c["vm"] = (valid & ~forced).astype(np.float32)
    c["am"] = np.where(forced, 1.0e4, np.where(valid, 0.0, -1.0)).astype(np.float32)
    c["svals"] = np.arange(128, dtype=np.float32)[:, None].copy()
    c["tvals"] = np.tile(np.arange(129, dtype=np.float32)[None, :], (64, 1)).copy()
    tri = (np.arange(128)[:, None] <= np.arange(128)[None, :]).astype(np.float32)
    c["tri"] = tri.astype(bf)
    pf = np.ones((128, 4, 16), np.float32)
    for g, w in enumerate((2, 4, 8, 16)):
        tt_ = np.arange(16)
        pf[:, g, :] = (w / np.minimum(tt_ + 1, w))[None, :]
    c["poolfix"] = pf
    gm = np.zeros((128, 8), np.float32)
    for pp in range(128):
        gm[pp, pp // 16] = 1.0
    c["gmask"] = gm
    return c


CONST_SPECS = None


import os
P0MODE = int(os.environ.get('P0MODE', '0'))
P4BSTOP = int(os.environ.get('P4BSTOP', '0'))
SKIP3 = int(os.environ.get('SKIP3', '0'))
SKIP4B = int(os.environ.get('SKIP4B', '0'))
P6STOP = int(os.environ.get('P6STOP', '0'))
ARM = os.environ.get('ARM', '')


def build(S, L, dump=False, upto=None):
    NT = S // 128
    NC_ = S // 16
    NCT = max(1, NC_ // 128)
    NJ = S // 64
    NTB = S // 512
    hc = host_consts(S)
    nc = bass.Bass("TRN2", target_bir_lowering=False)
    f = FW(nc)
    inp = {}

    def din(name, shape, dt=F32):
        inp[name] = nc.dram_tensor(name, list(shape), dt, kind="ExternalInput").ap()
        return inp[name]

    x_in = din("x", [S, D])
    full_shapes = {
        "w_in": [L, D, INW], "cmp_pe_k": [L, 32, 64], "cmp_pe_v": [L, 32, 64], "cmp_w1_k": [L, 2048, 128],
        "cmp_w2_k": [L, 128, 64], "cmp_w1_v": [L, 2048, 128], "cmp_w2_v": [L, 128, 64], "pool_w": [L, 4, 128, 128],
        "pool_scale": [L, 512], "ssm_lam_re": [L, 32, 64], "ssm_lam_im": [L, 32, 64], "ssm_log_dt": [L, 32],
        "ssm_b_re": [L, 32, 64, 16], "ssm_b_im": [L, 32, 64, 16], "ssm_c_re": [L, 32, 16, 64], "ssm_c_im": [L, 32, 16, 64],
        "ssm_d": [L, 512], "ssm_w_glu": [L, 512, 512], "ssm_b_glu": [L, 512], "w_up_nsa": [L, 512, D],
        "w_up_pool": [L, 512, D], "w_up_ssm": [L, 512, D], "w_out": [L, D, D], "ln1_g": [L, D], "ln1_b": [L, D],
        "router_w_grp": [L, D, 4], "router_b_grp": [L, 4], "router_w_exp": [L, D, 32], "router_b_exp": [L, 32],
        "moe_w_gate": [L, NEXP, D, FF], "moe_w_up": [L, NEXP, D, FF], "moe_w_down": [L, NEXP, FF, D],
        "ln2_g": [L, D], "ln2_b": [L, D]}
    W = {k: din(k, full_shapes[k]) for k in WNAMES}
    C = {}
    for k, v in hc.items():
        C[k] = din("c_" + k, v.shape, BF16 if v.dtype == ml_dtypes.bfloat16 else F32)
    out_d = nc.dram_tensor("out", [S, D], F32, kind="ExternalOutput").ap()

    def scratch(name, shape, dt):
        return nc.dram_tensor(name, list(shape), dt, kind="ExternalOutput" if dump else "Internal").ap()

    xT_d = scratch("xT_d", [D, S], BF16)
    qT_d = scratch("qT_d", [8, 68, S], BF16)
    kT_d = scratch("kT_d", [8, 68, S + 16], BF16)
    uT_d = scratch("uT_d", [D, 16 + S], BF16)
    v_d = scratch("v_d", [S, 4, 65], BF16)
    gate_d = scratch("gate_d", [S, 24], F32)
    kcmp_d = scratch("kcmp_d", [2, 68, NCT * 128], BF16)
    vcmp_d = scratch("vcmp_d", [NCT * 128, 2, 65], BF16)
    onT_d = scratch("onT_d", [512, S], BF16)
    opT_d = scratch("opT_d", [512, S], BF16)
    osT_d = scratch("osT_d", [512, S], BF16)
    dbg_d = scratch("dbg_d", [8, 128, 512], BF16)
    dbg2_d = scratch("dbg2_d", [3, 128, 260], F32)
    x1_d = scratch("x1_d", [S, D], F32)
    xr_d = scratch("xr_d", [S, D], F32)

    identf = f.sb([128, 128], F32, "identf")
    identb = f.sb([128, 128], BF16, "identb")
    f.memset("pool", identf, 0.0)
    f.op("pool", lambda: nc.gpsimd.affine_select(out=identf.ap, in_=identf.ap, pattern=[[-1, 128]], compare_op=ALU.not_equal,
                                                 fill=1.0, base=0, channel_multiplier=1), [identf], [identf])
    f.copy("dve", identb, identf)
    PS = [f.ps([128, 512], F32, "ps%d" % i) for i in range(7)]
    PSB = f.ps([128, 1024], BF16, "psb")

    st = ExitStack()
    t_qa = f.sb([4, 8, S], BF16, "t_qa", stack=st)
    f.dma("sp", t_qa, C["qaug"].rearrange("h a s -> a h s"))
    f.dma("sp", qT_d.rearrange("h d s -> d h s")[64:68, :, :], t_qa)
    t_ka = f.sb([4, S + 16], BF16, "t_ka", stack=st)
    f.dma("sp", t_ka, C["kaug"])
    for i in range(8):
        f.dma("sp", kT_d[i, 64:68, :], t_ka)
    t_kc = f.sb([4, NC_], BF16, "t_kc", stack=st)
    f.dma("sp", t_kc, C["kaugc"])
    for g in range(2):
        f.dma("sp", kcmp_d[g, 64:68, 0:NC_], t_kc)
    zt = f.sb([128, 8, 16], BF16, "zt", stack=st)
    f.memset("dve", zt, 0.0)
    f.dma("sp", kT_d.rearrange("i d s -> d i s")[0:64, :, S:S + 16], zt[0:64])
    f.dma("sp", uT_d.rearrange("(i p) s -> p i s", p=128)[:, :, 0:16], zt)
    f.barrier()
    st.close()
    if upto == "PRE":
        f.finish()
        return nc, hc

    ei = [0]

    def rot(engs):
        ei[0] += 1
        return engs[ei[0] % len(engs)]

    lnscr = {}

    def layer_norm(st, src, gbc, bbc, dst, eps=1e-5):
        if id(st) not in lnscr:
            lnscr[id(st)] = [[f.sb([128, 2, 6], F32, stack=st), f.sb([128, 2], F32, stack=st)] for _ in range(2)] + [0]
        sc_ = lnscr[id(st)]
        sc_[2] += 1
        stt_, mv = sc_[sc_[2] % 2]
        for c2 in range(2):
            f.op("dve", lambda c2=c2: nc.vector.bn_stats(out=stt_.ap[:, c2, :], in_=src.ap[:, c2 * 512:(c2 + 1) * 512]), [src], [stt_])
        f.op("dve", lambda: nc.vector.bn_aggr(out=mv.ap, in_=stt_.ap), [stt_], [mv])
        f.ts("dve", mv[:, 1:2], mv[:, 1:2], eps, None, ALU.add)
        f.act(mv[:, 1:2], mv[:, 1:2], AF.Sqrt)
        f.op("dve", lambda: nc.vector.reciprocal(out=mv.ap[:, 1:2], in_=mv.ap[:, 1:2]), [mv], [mv])
        f.ts("dve", dst, src, mv[:, 0:1], mv[:, 1:2], ALU.subtract, ALU.mult)
        f.tt("pool", dst, dst, gbc, ALU.mult)
        f.tt("pool", dst, dst, bbc, ALU.add)

    class LNCtx:
        pass

    for l in range(L):
        xsrc = x_in if l == 0 else xr_d
        xdst = out_d if l == L - 1 else xr_d

        st = ExitStack()
        xt_b = [f.sb([128, D], F32, "p0x%d" % i, stack=st) for i in range(2)]
        xT_s = [f.sb([128, 8, 512], BF16, "p0t%d" % i, stack=st) for i in range(2)]
        for tb in range(NTB):
            xs = xT_s[tb % 2]
            for sub in range(4):
                tt0 = tb * 512 + sub * 128
                xb = xt_b[sub % 2]
                f.dma("sp", xb, xsrc[tt0:tt0 + 128, :])
                for hh in range(2):
                    ps = PS[(sub * 2 + hh) % 4]
                    for k4 in range(4):
                        kt = hh * 4 + k4
                        f.transpose(ps[:, k4 * 128:(k4 + 1) * 128], xb[:, kt * 128:(kt + 1) * 128], identf)
                    if P0MODE != 1:
                        f.copy("dve", xs[:, hh * 4:(hh + 1) * 4, sub * 128:(sub + 1) * 128],
                               ps.v().rearrange("p (k t) -> p k t", k=4))
            if P0MODE not in (1, 2):
                f.dma("sp", xT_d.rearrange("(k p) s -> p k s", p=128)[:, :, tb * 512:(tb + 1) * 512], xs)
        f.barrier()
        st.close()
        if upto == 'P0':
            f.finish()
            return nc, hc

        st = ExitStack()
        NP1 = C_BG
        wq = f.sb([128, 8, NP1], BF16, "wq", stack=st)
        wv = W["w_in"][l].rearrange("(k p) n -> p k n", p=128)
        for kt in range(8):
            f.dma("pool", wq[:, kt, :], wv[:, kt, 0:NP1])
        xs_b = [f.sb([128, 8, 512], BF16, "p1x%d" % i, stack=st) for i in range(2)]
        qst = [f.sb([64, 8, 512], BF16, "qst%d" % i, stack=st) for i in range(2)]
        kst = [f.sb([64, 8, 512], BF16, "kst%d" % i, stack=st) for i in range(2)]
        ust = [f.sb([128, 8, 512], BF16, "ust%d" % i, stack=st) for i in range(2)]
        vst = [f.sb([128, 4, 4, 65], BF16, "vst%d" % i, stack=st) for i in range(2)]
        gst = [f.sb([128, 4, 24], F32, "gst%d" % i, stack=st) for i in range(2)]
        ngt = f.sb([128, 24], F32, "ngt", stack=st)
        for i in range(2):
            f.memset("dve", vst[i], 1.0)
        kcols = [C_KC, C_KC + 64, C_VC, C_VC + 64, C_KS, C_KS + 64, C_KW, C_KW + 64]
        pi = 0
        for tb in range(NTB):
            xs = xs_b[tb % 2]
            f.dma("sp", xs, xT_d.rearrange("(k p) s -> p k s", p=128)[:, :, tb * 512:(tb + 1) * 512])
            q_, k_, u_, v_, g_ = qst[tb % 2], kst[tb % 2], ust[tb % 2], vst[tb % 2], gst[tb % 2]
            jobs = []
            for h in range(8):
                jobs.append((C_Q + 64 * h, 64, q_, h, 0.125))
            for i in range(8):
                jobs.append((kcols[i], 64, k_, i, 1.0))
            for i in range(4):
                jobs.append((C_UP + 128 * i, 128, u_, i, 1.0))
            for i in range(4):
                jobs.append((C_US + 128 * i, 128, u_, 4 + i, 1.0))
            for (c0, M, dst, di, sc) in jobs:
                ps = PS[pi % 4]
                pi += 1
                for kt in range(8):
                    f.matmul(ps[0:M, :], wq[:, kt, c0:c0 + M], xs[:, kt, :], start=(kt == 0), stop=(kt == 7))
                e = rot(["dve", "act"])
                if e == "act":
                    f.act(dst[0:M, di, :], ps[0:M, :], AF.Copy, scale=sc)
                else:
                    f.ts("dve", dst[0:M, di, :], ps[0:M, :], sc, None, ALU.mult)
            for sub in range(4):
                ps = PS[4 + sub % 2]
                for kt in range(8):
                    f.matmul(ps[:, 0:408], xs[:, kt, sub * 128:(sub + 1) * 128], wq[:, kt, C_VS:C_VS + 408], start=(kt == 0), stop=(kt == 7))
                f.copy("dve", v_[:, sub, 0:2, 0:64], ps[:, 0:128].rearrange("p (g d) -> p g d", g=2))
                f.copy("dve", v_[:, sub, 2:4, 0:64], ps[:, 256:384].rearrange("p (g d) -> p g d", g=2))
                f.copy("dve", ngt, ps[:, 384:408])
                f.act(g_[:, sub, :], ngt, AF.Sigmoid)
            ts_ = slice(tb * 512, (tb + 1) * 512)
            f.dma("sp", qT_d.rearrange("h d s -> d h s")[0:64, :, ts_], q_)
            f.dma("sp", kT_d.rearrange("i d s -> d i s")[0:64, :, ts_], k_)
            f.dma("sp", uT_d.rearrange("(i p) s -> p i s", p=128)[:, :, 16 + tb * 512:16 + (tb + 1) * 512], u_)
            f.dma("sp", v_d[ts_].rearrange("(n p) j c -> p n j c", p=128), v_)
            f.dma("sp", gate_d[ts_].rearrange("(n p) c -> p n c", p=128), g_)
        f.barrier()
        st.close()
        if upto == 'P1':
            f.finish()
            return nc, hc

        st = ExitStack()
        for which, (w1n, w2n, pen, base) in enumerate([("cmp_w1_k", "cmp_w2_k", "cmp_pe_k", 0), ("cmp_w1_v", "cmp_w2_v", "cmp_pe_v", 2)]):
            w1b = f.sb([64, 32, 128], BF16, "w1b%d" % which, stack=st)
            f.dma("pool", w1b, W[w1n][l].rearrange("(l d) h -> d l h", d=64))
            w2b = f.sb([128, 64], BF16, "w2b%d" % which, stack=st)
            f.dma("pool", w2b, W[w2n][l])
            pe_s = f.sb([32, 64], F32, "pe%d" % which, stack=st)
            f.dma("sp", pe_s, W[pen][l])
            f.transpose(PS[0][0:64, 0:32], pe_s, identf[0:32, 0:32])
            peT = f.sb([64, 32], BF16, "peT%d" % which, stack=st)
            f.copy("dve", peT, PS[0][0:64, 0:32])
            for li in range(32):
                f.matmul(PS[1][:, 0:1], w1b[:, li, :], peT[:, li:li + 1], start=(li == 0), stop=(li == 31))
            b1 = f.sb([128, 1], F32, "b1%d" % which, stack=st)
            f.copy("dve", b1, PS[1][:, 0:1])
            for g in range(2):
                kc_s = f.sb([64, S + 16], BF16, "kcs%d%d" % (which, g), stack=st)
                f.dma("sp", kc_s, kT_d[base + g, 0:64, :])
                kcv = kc_s.v().rearrange("d (n r) -> d n r", r=16)
                for cb in range(0, NC_, 512):
                    nb = min(512, NC_ - cb)
                    for li in range(32):
                        f.matmul(PS[2][:, 0:nb], w1b[:, li, :], kcv[:, cb + li // 16: cb + li // 16 + nb, li % 16], start=(li == 0), stop=(li == 31))
                    hT = f.sb([128, 512], BF16, "hT%d%d" % (which, g), stack=st)
                    f.act(hT[:, 0:nb], PS[2][:, 0:nb], AF.Gelu, bias=b1[:, 0:1])
                    if which == 0:
                        f.matmul(PS[3][0:64, 0:nb], w2b, hT[:, 0:nb])
                        kcm = f.sb([64, 512], BF16, "kcm%d" % g, stack=st)
                        f.copy("dve", kcm[:, 0:nb], PS[3][0:64, 0:nb])
                        f.dma("sp", kcmp_d[g, 0:64, cb:cb + nb], kcm[:, 0:nb])
                    else:
                        vcm = f.sb([128, 4, 65], BF16, "vcm%d" % g, stack=st)
                        f.memset("pool", vcm, 1.0)
                        nbt = (nb + 127) // 128
                        for bt in range(nbt):
                            w_ = min(128, nb - bt * 128)
                            f.matmul(PS[3][0:w_, bt * 64:(bt + 1) * 64], hT[:, bt * 128:bt * 128 + w_], w2b)
                            f.copy("dve", vcm[0:w_, bt, 0:64], PS[3][0:w_, bt * 64:(bt + 1) * 64])
                        if nb >= 128:
                            f.dma("sp", vcmp_d[cb:cb + nb, g, :].rearrange("(n p) c -> p n c", p=128), vcm[:, 0:nbt, :])
                        else:
                            f.dma("sp", vcmp_d[cb:cb + nb, g, :], vcm[0:nb, 0, :])
        f.barrier()
        st.close()
        if upto == 'P2':
            f.finish()
            return nc, hc

        st = ExitStack()
        kT_s = f.sb([68, 4, S], BF16, "kT_s", stack=st)
        for i in range(4):
            f.dma("sp", kT_s[:, i, :], kT_d[4 + i, :, 0:S])
        va_s = f.sb([128, NT, 4, 65], BF16, "va_s", stack=st)
        vdv = v_d.rearrange("(n p) j c -> p n j c", p=128)
        for n0 in range(0, NT, 16):
            n1 = min(NT, n0 + 16)
            f.dma("sp", va_s[:, n0:n1], vdv[:, n0:n1])
        kc_s = f.sb([68, 2, NCT * 128], BF16, "kc_s", stack=st)
        f.memset("dve", kc_s, 0.0)
        f.dma("sp", kc_s[:, :, 0:NC_], kcmp_d.rearrange("g d c -> d g c")[:, :, 0:NC_])
        vc_s = f.sb([128, NCT, 2, 65], BF16, "vc_s", stack=st)
        f.memset("dve", vc_s, 0.0)
        if NC_ >= 128:
            f.dma("sp", vc_s, vcmp_d.rearrange("(n p) g c -> p n g c", p=128))
        else:
            f.dma("sp", vc_s[0:NC_, 0], vcmp_d[0:NC_])
        negcmp = f.sb([128, 16, 512], BF16, "negcmp", stack=st)
        f.dma("sp", negcmp, C["negcmp"])
        negcau = f.sb([128, 512], BF16, "negcau", stack=st)
        f.dma("sp", negcau, C["negcausal"])
        negwl = f.sb([128, 512], BF16, "negwl", stack=st)
        f.dma("sp", negwl, C["negwinlo"])
        etab = f.sb([128, NT, 128], BF16, "etab", stack=st)
        f.dma("sp", etab, C["etab"])
        ovl = f.sb([128, NCT, 128], BF16, "ovl", stack=st)
        f.dma("sp", ovl, C["overlap"])
        vm = f.sb([128, 256], F32, "vm", stack=st)
        f.dma("sp", vm, C["vm"])
        am = f.sb([128, 256], F32, "am", stack=st)
        f.dma("sp", am, C["am"])
        qT_b = [f.sb([68, 8 * 128], BF16, "qTb%d" % i, stack=st) for i in range(2)]
        gt_b = [f.sb([128, 24], F32, "gtb%d" % i, stack=st) for i in range(2)]
        PTc = [f.sb([128, 512], BF16, "ptc%d" % i, stack=st) for i in range(4)]
        PT = [f.sb([128, 512], BF16, "pt%d" % i, stack=st) for i in range(4)]
        oacc = [f.sb([128, 512], F32, "oacc%d" % i, stack=st) for i in range(2)]
        onb = [f.sb([128, 512], BF16, "onb%d" % i, stack=st) for i in range(2)]
        onT = [f.sb([128, 4, 128], BF16, "onT%d" % i, stack=st) for i in range(2)]
        imp = f.sb([128, 128], F32, "imp", stack=st)
        sc1 = f.sb([128, 128], F32, "sc1", stack=st)
        sc2 = f.sb([128, 128], F32, "sc2", stack=st)
        m16 = f.sb([128, 16], F32, "m16", stack=st)
        nsb = f.sb([128, 128], BF16, "nsb", stack=st)
        nselT = [f.sb([128, 512], BF16, "nselT%d" % i, stack=st) for i in range(2)]
        rden = [f.sb([128, 4], F32, "rden%d" % i, stack=st) for i in range(3)]
        scl = [f.sb([128, 4], F32, "scl%d" % i, stack=st) for i in range(3)]
        tmpo = [f.sb([128, 4, 64], F32, "tmpo%d" % i, stack=st) for i in range(2)]
        ST = [PS[0], PS[1]]
        OC, OS_, OW, IMP = PS[2], PS[3], PS[4], PS[5]
        sti = [0]
        pti = [0]

        def score_tile(lhs_k, rhs_q, extra):
            ps = ST[sti[0] % 2]
            sti[0] += 1
            n = 1 + len(extra)
            f.matmul(ps, lhs_k, rhs_q, start=True, stop=(n == 1))
            for i, (a, b) in enumerate(extra):
                f.matmul(ps, a, b, start=False, stop=(i == len(extra) - 1))
            return ps

        for qt in range(0 if SKIP3 else NT):
            f.maybe_switch()
            qTt = qT_b[qt % 2]
            f.dma("sp", qTt.v().rearrange("d (h t) -> d h t", h=8), qT_d.rearrange("h d s -> d h s")[:, :, qt * 128:(qt + 1) * 128])
            gt = gt_b[qt % 2]
            f.dma("sp", gt, gate_d[qt * 128:(qt + 1) * 128, :])
            oa = oacc[qt % 2]
            for g in range(2):
                rq = qTt[:, g * 512:(g + 1) * 512]
                ctl = qt // 16
                for ct in range(ctl + 1):
                    extra = [(identb, negcmp[:, qt % 16, :])] if ct == ctl else []
                    ps = score_tile(kc_s[:, g, ct * 128:(ct + 1) * 128], rq, extra)
                    pt = PTc[ct]
                    f.act(pt, ps, AF.Exp)
                    for r in range(4):
                        f.matmul(OC[:, r * 65:(r + 1) * 65], pt[:, r * 128:(r + 1) * 128], vc_s[:, ct, g, :], start=(ct == 0 and r == 0), stop=(ct == ctl))
                        f.matmul(IMP[:, r * 128:(r + 1) * 128], pt[:, r * 128:(r + 1) * 128], ovl[:, ct, :], start=(ct == 0 and r == 0), stop=(ct == ctl))
                ocv = OC[:, 0:260].rearrange("p (r c) -> p r c", r=4)
                f.ts("dve", rden[0], ocv[:, :, 64], 1e-30, None, ALU.max)
                f.op("dve", lambda: nc.vector.reciprocal(out=rden[0].ap, in_=rden[0].ap), [rden[0]], [rden[0]])
                f.ts("dve", imp, IMP[:, 0:128], rden[0][:, 0:1], None, ALU.mult)
                for r in range(1, 4):
                    f.stt("dve", imp, IMP[:, r * 128:(r + 1) * 128], rden[0][:, r:r + 1], imp, ALU.mult, ALU.add)
                off = 128 - 2 * qt
                f.tt("dve", sc1[:, 0:NJ], imp[:, 0:NJ], vm[:, off:off + NJ], ALU.mult)
                f.tt("dve", sc1[:, 0:NJ], sc1[:, 0:NJ], am[:, off:off + NJ], ALU.add)
                f.memset("dve", sc1[:, 0:1], 1.0e4)
                if NJ < 128:
                    f.memset("dve", sc1[:, NJ:128], -2.0)
                f.op("dve", lambda: nc.vector.max(out=m16.ap[:, 0:8], in_=sc1.ap), [sc1], [m16])
                f.op("dve", lambda: nc.vector.match_replace(out=sc2.ap, in_to_replace=m16.ap[:, 0:8], in_values=sc1.ap, imm_value=-1e9), [sc1, m16], [sc2])
                f.op("dve", lambda: nc.vector.max(out=m16.ap[:, 8:16], in_=sc2.ap), [sc2], [m16])
                f.op("dve", lambda: nc.vector.match_replace(out=sc1.ap, in_to_replace=m16.ap[:, 8:16], in_values=sc2.ap, imm_value=-1e9), [sc2, m16], [sc1])
                f.ts("dve", nsb, sc1, -1e8, NEG, ALU.is_gt, ALU.mult)
                f.transpose(PSB[:, 0:128], nsb, identb)
                nsT = nselT[g]
                for r in range(4):
                    f.copy(rot(["dve", "pool"]) if False else "dve", nsT[:, r * 128:(r + 1) * 128], PSB[:, 0:128])
                for kt in range(qt + 1):
                    extra = [(etab[:, kt, :], nsT)]
                    if kt == qt:
                        extra.append((identb, negcau))
                    ps = score_tile(kT_s[:, g, kt * 128:(kt + 1) * 128], rq, extra)
                    pt = PT[pti[0] % 4]
                    pti[0] += 1
                    f.act(pt, ps, AF.Exp)
                    if dump and qt == 1 and g == 0:
                        f.dma("sp", dbg_d[kt], pt)
                        if kt == 0:
                            f.dma("sp", dbg_d[4], nsT)
                    for r in range(4):
                        f.matmul(OS_[:, r * 65:(r + 1) * 65], pt[:, r * 128:(r + 1) * 128], va_s[:, kt, g, :], start=(kt == 0 and r == 0), stop=(kt == qt))
                k0 = max(0, qt - 4)
                for kt in range(k0, qt + 1):
                    extra = []
                    if kt == qt - 4:
                        extra.append((identb, negwl))
                    if kt == qt:
                        extra.append((identb, negcau))
                    ps = score_tile(kT_s[:, 2 + g, kt * 128:(kt + 1) * 128], rq, extra)
                    pt = PT[pti[0] % 4]
                    pti[0] += 1
                    f.act(pt, ps, AF.Exp)
                    if dump and qt == 1 and g == 0:
                        f.dma("sp", dbg_d[2 + kt], pt)
                    for r in range(4):
                        f.matmul(OW[:, r * 65:(r + 1) * 65], pt[:, r * 128:(r + 1) * 128], va_s[:, kt, 2 + g, :], start=(kt == k0 and r == 0), stop=(kt == qt))
                if dump and qt == 1 and g == 0:
                    for bi_, O_ in enumerate([OC, OS_, OW]):
                        dt_ = f.sb([128, 260], F32, "dbgt", stack=st)
                        f.copy("dve", dt_, O_[:, 0:260])
                        f.dma("sp", dbg2_d[bi_], dt_)
                gv = gt.v().rearrange("p (g r b) -> p g r b", g=2, r=4)
                for br, O in enumerate([OC, OS_, OW]):
                    ov_ = O[:, 0:260].rearrange("p (r c) -> p r c", r=4)
                    if br > 0:
                        f.ts("dve", rden[br], ov_[:, :, 64], 1e-30, None, ALU.max)
                        f.op("dve", lambda br=br: nc.vector.reciprocal(out=rden[br].ap, in_=rden[br].ap), [rden[br]], [rden[br]])
                    f.tt("dve", scl[br], rden[br], gv[:, g, :, br], ALU.mult)
                    oav = oa[:, g * 256:(g + 1) * 256].rearrange("p (r d) -> p r d", r=4)
                    sb_ = scl[br].v().unsqueeze(2).broadcast_to([128, 4, 64])
                    if br == 0:
                        f.tt("dve", oav, ov_[:, :, 0:64], sb_, ALU.mult)
                    else:
                        tm = tmpo[br % 2]
                        f.tt("dve", tm, ov_[:, :, 0:64], sb_, ALU.mult)
                        f.tt("pool", oav, oav, tm, ALU.add)
            ob = onb[qt % 2]
            f.copy("act", ob, oa)
            oT = onT[qt % 2]
            for i in range(4):
                f.transpose(PSB[:, 512 + i * 128:512 + (i + 1) * 128], ob[:, i * 128:(i + 1) * 128], identb)
            f.copy("dve", oT, PSB[:, 512:1024].rearrange("p (i t) -> p i t", i=4))
            f.dma("sp", onT_d.rearrange("(i p) s -> p i s", p=128)[:, :, qt * 128:(qt + 1) * 128], oT)
        f.barrier()
        st.close()
        if upto == 'P3':
            f.finish()
            return nc, hc

        st = ExitStack()
        pw = f.sb([128, 4, 128], BF16, "pw", stack=st)
        f.dma("pool", pw, W["pool_w"][l].rearrange("g i o -> i g o"))
        psc = f.sb([128, 4], F32, "psc", stack=st)
        f.dma("sp", psc, W["pool_scale"][l].rearrange("(g p) -> p g", p=128), allow_slow_non_contiguous=True)
        pfix = f.sb([128, 4, 16], F32, "pfix", stack=st)
        f.dma("sp", pfix, C["poolfix"])
        ub_b = [f.sb([128, 4, 528], BF16, "ub%d" % i, stack=st) for i in range(2)]
        wa = [f.sb([128, 528], F32, "wa%d" % i, stack=st) for i in range(2)]
        wb = [f.sb([128, 528], F32, "wb%d" % i, stack=st) for i in range(2)]
        pin = [f.sb([128, 512], BF16, "pin%d" % i, stack=st) for i in range(2)]
        opst = [f.sb([128, 4, 512], BF16, "opst%d" % i, stack=st) for i in range(2)]
        uTv = uT_d.rearrange("(i p) s -> p i s", p=128)
        for tb in range(NTB):
            ub = ub_b[tb % 2]
            f.dma("sp", ub, uTv[:, 0:4, tb * 512:tb * 512 + 528])
            os_ = opst[tb % 2]
            for g in range(4):
                e = "dve" if g % 2 == 0 else "pool"
                a, b = wa[g % 2], wb[g % 2]
                src = ub[:, g, :]
                sh = 1
                cur = None
                for step in range(g + 1):
                    dst = a if step % 2 == 0 else b
                    s_in = src if cur is None else cur
                    f.tt(e, dst[:, sh:528], s_in[:, sh:528], s_in[:, 0:528 - sh], ALU.add)
                    cur = dst
                    sh *= 2
                w_ = 2 ** (g + 1)
                if tb == 0:
                    f.tt(e, cur[:, 16:32], cur[:, 16:32], pfix[:, g, :], ALU.mult)
                f.stt("dve", pin[g % 2], cur[:, 16:528], 1.0 / w_, ub[:, g, 16:528], ALU.mult, ALU.subtract)
                ps = PS[g % 2]
                f.matmul(ps, pw[:, g, :], pin[g % 2])
                f.act(os_[:, g, :], ps, AF.Copy, scale=psc[:, g:g + 1])
            f.dma("sp", opT_d.rearrange("(i p) s -> p i s", p=128)[:, :, tb * 512:(tb + 1) * 512], os_)
        f.barrier()
        st.close()
        if upto == 'P4a':
            f.finish()
            return nc, hc

        st = ExitStack()
        lam_n = f.sb([32, 2, 64], F32, "lam_n", stack=st)
        f.dma("sp", lam_n[:, 0, :], W["ssm_lam_re"][l])
        f.dma("sp", lam_n[:, 1, :], W["ssm_lam_im"][l])
        lrT = f.sb([64, 32], F32, "lrT", stack=st)
        liT = f.sb([64, 32], F32, "liT", stack=st)
        f.transpose(PS[0][0:64, 0:32], lam_n[:, 0, :], identf[0:32, 0:32])
        f.copy("dve", lrT, PS[0][0:64, 0:32])
        f.transpose(PS[0][0:64, 32:64], lam_n[:, 1, :], identf[0:32, 0:32])
        f.copy("dve", liT, PS[0][0:64, 32:64])
        dtT = f.sb([64, 32], F32, "dtT", stack=st)
        f.dma("sp", dtT, W["ssm_log_dt"][l:l + 1, :].partition_broadcast(64))
        f.act(dtT, dtT, AF.Exp)
        sv = f.sb([128, 1], F32, "sv", stack=st)
        f.dma("sp", sv, C["svals"])
        tv = f.sb([64, 129], F32, "tv", stack=st)
        f.dma("sp", tv, C["tvals"])
        lrdt = f.sb([64, 32], F32, "lrdt", stack=st)
        th = f.sb([64, 32], F32, "th", stack=st)
        f.tt("dve", lrdt, lrT, dtT, ALU.mult)
        f.tt("dve", th, liT, dtT, ALU.mult)
        INV2PI = 1.0 / (2 * math.pi)

        def sincos(st2, arg, shape, sin_out, cos_out, e="dve"):
            ki = f.sb(shape, I32, stack=st2)
            kf = f.sb(shape, F32, stack=st2)
            r_ = f.sb(shape, F32, stack=st2)
            for (outp, shift) in ((sin_out, 0.0), (cos_out, math.pi / 2)):
                f.ts(e, kf, arg, shift, INV2PI, ALU.add, ALU.mult)
                f.copy(e, ki, kf)
                f.copy(e, kf, ki)
                f.ts(e, r_, arg, shift, None, ALU.add)
                f.stt(e, r_, kf, -2 * math.pi, r_, ALU.mult, ALU.add)
                f.ts(e, r_, r_, -3.1415925, 3.1415925, ALU.max, ALU.min)
                f.act(outp, r_, AF.Sin)

        Dpr = f.sb([64, 32, 129], F32, "Dpr", stack=st)
        Dpi = f.sb([64, 32, 129], F32, "Dpi", stack=st)
        st2 = ExitStack()
        argp = f.sb([64, 32, 129], F32, stack=st2)
        magp = f.sb([64, 32, 129], F32, stack=st2)
        tvb = tv.v().unsqueeze(1).broadcast_to([64, 32, 129])
        f.tt("dve", argp, th.v().unsqueeze(2).broadcast_to([64, 32, 129]), tvb, ALU.mult)
        f.tt("pool", magp, lrdt.v().unsqueeze(2).broadcast_to([64, 32, 129]), tvb, ALU.mult)
        f.act(magp, magp, AF.Exp)
        sincos(st2, argp, [64, 32, 129], Dpi, Dpr)
        f.tt("dve", Dpr, Dpr, magp, ALU.mult)
        f.tt("dve", Dpi, Dpi, magp, ALU.mult)
        f.barrier()
        st2.close()
        if P4BSTOP == 1:
            f.finish()
            return nc, hc
        Dmr = f.sb([128, 2048], F32, "Dmr", stack=st)
        Dmi = f.sb([128, 2048], F32, "Dmi", stack=st)
        st2 = ExitStack()
        lrow = f.sb([128, 2, 2048], F32, stack=st2)
        f.dma("sp", lrow[:, 0, :], W["ssm_lam_re"][l:l + 1].rearrange("o g p -> o (g p)").partition_broadcast(128))
        f.dma("sp", lrow[:, 1, :], W["ssm_lam_im"][l:l + 1].rearrange("o g p -> o (g p)").partition_broadcast(128))
        dtr = f.sb([128, 32], F32, stack=st2)
        f.dma("sp", dtr, W["ssm_log_dt"][l:l + 1, :].partition_broadcast(128))
        f.act(dtr, dtr, AF.Exp)
        f.ts("dve", dtr, dtr, sv[:, 0:1], None, ALU.mult)
        dtb = dtr.v().unsqueeze(2).broadcast_to([128, 32, 64])
        argm = f.sb([128, 2048], F32, stack=st2)
        magm = f.sb([128, 2048], F32, stack=st2)
        f.tt("dve", argm.v().rearrange("p (g q) -> p g q", g=32), lrow[:, 1, :].rearrange("p (g q) -> p g q", g=32), dtb, ALU.mult)
        f.tt("pool", magm.v().rearrange("p (g q) -> p g q", g=32), lrow[:, 0, :].rearrange("p (g q) -> p g q", g=32), dtb, ALU.mult)
        f.act(magm, magm, AF.Exp, scale=-1.0)
        sincos(st2, argm, [128, 2048], Dmi, Dmr)
        f.tt("dve", Dmr, Dmr, magm, ALU.mult)
        f.ts("dve", Dmi, Dmi, -1.0, None, ALU.mult)
        f.tt("dve", Dmi, Dmi, magm, ALU.mult)
        f.barrier()
        st2.close()
        if P4BSTOP == 2:
            f.finish()
            return nc, hc
        BD = f.sb([128, 4, 2, 512], BF16, "BD", stack=st)
        CTp = f.sb([64, 2, 32, 128], BF16, "CTp", stack=st)
        st2 = ExitStack()
        arb = f.sb([64, 32], F32, stack=st2)
        aib = f.sb([64, 32], F32, stack=st2)
        f.copy("dve", arb, Dpr[:, :, 1])
        f.copy("dve", aib, Dpi[:, :, 1])
        den = f.sb([64, 32], F32, stack=st2)
        t1 = f.sb([64, 32], F32, stack=st2)
        t2 = f.sb([64, 32], F32, stack=st2)
        crr = f.sb([64, 32], F32, stack=st2)
        cii = f.sb([64, 32], F32, stack=st2)
        f.tt("dve", den, lrT, lrT, ALU.mult)
        f.tt("dve", t1, liT, liT, ALU.mult)
        f.tt("dve", den, den, t1, ALU.add)
        f.op("dve", lambda: nc.vector.reciprocal(out=den.ap, in_=den.ap), [den], [den])
        f.ts("dve", arb, arb, -1.0, None, ALU.add)
        f.tt("dve", t1, arb, lrT, ALU.mult)
        f.tt("dve", t2, aib, liT, ALU.mult)
        f.tt("dve", crr, t1, t2, ALU.add)
        f.tt("dve", crr, crr, den, ALU.mult)
        f.tt("dve", t1, aib, lrT, ALU.mult)
        f.tt("dve", t2, arb, liT, ALU.mult)
        f.tt("dve", cii, t1, t2, ALU.subtract)
        f.tt("dve", cii, cii, den, ALU.mult)
        bre = f.sb([64, 32, 16], F32, stack=st2)
        bim = f.sb([64, 32, 16], F32, stack=st2)
        f.dma("sp", bre, W["ssm_b_re"][l].rearrange("g p h -> p g h"))
        f.dma("sp", bim, W["ssm_b_im"][l].rearrange("g p h -> p g h"))
        crb = crr.v().unsqueeze(2).broadcast_to([64, 32, 16])
        cib = cii.v().unsqueeze(2).broadcast_to([64, 32, 16])
        bbr = f.sb([64, 32, 16], F32, stack=st2)
        bbi = f.sb([64, 32, 16], F32, stack=st2)
        tb1 = f.sb([64, 32, 16], F32, stack=st2)
        f.tt("dve", bbr, bre, crb, ALU.mult)
        f.tt("dve", tb1, bim, cib, ALU.mult)
        f.tt("dve", bbr, bbr, tb1, ALU.subtract)
        f.tt("dve", bbi, bim, crb, ALU.mult)
        f.tt("dve", tb1, bre, cib, ALU.mult)
        f.tt("dve", bbi, bbi, tb1, ALU.add)
        gmask = f.sb([128, 8], F32, stack=st2)
        f.dma("sp", gmask, C["gmask"])
        for o in range(4):
            for ri, bb in enumerate((bbr, bbi)):
                f.transpose(PS[ri][:, 0:64], bb[:, 8 * o:8 * o + 8, :].rearrange("p g h -> p (g h)"), identf[0:64, 0:64])
                for gg in range(8):
                    f.ts("dve", BD[:, o, ri, gg * 64:(gg + 1) * 64], PS[ri][:, 0:64], gmask[:, gg:gg + 1], None, ALU.mult)
        f.memset("pool", CTp, 0.0)
        cn = f.sb([128, 4, 2, 64], F32, stack=st2)
        f.dma("sp", cn[:, :, 0, :], W["ssm_c_re"][l].rearrange("(o g) h p -> (g h) o p", o=4))
        f.dma("sp", cn[:, :, 1, :], W["ssm_c_im"][l].rearrange("(o g) h p -> (g h) o p", o=4))
        for o in range(4):
            for ri in range(2):
                f.transpose(PS[2 + ri][0:64, 0:128], cn[:, o, ri, :], identf)
                for gg in range(8):
                    g_ = 8 * o + gg
                    if ri == 0:
                        f.copy("dve", CTp[:, 0, g_, gg * 16:(gg + 1) * 16], PS[2][0:64, gg * 16:(gg + 1) * 16])
                    else:
                        f.ts("dve", CTp[:, 1, g_, gg * 16:(gg + 1) * 16], PS[3][0:64, gg * 16:(gg + 1) * 16], -1.0, None, ALU.mult)
        f.barrier()
        st2.close()
        if P4BSTOP == 3:
            f.finish()
            return nc, hc
        wglu = f.sb([128, 4, 512], BF16, "wglu", stack=st)
        f.dma("pool", wglu, W["ssm_w_glu"][l].rearrange("(i p) o -> p i o", p=128))
        bglu = f.sb([128, 4], F32, "bglu", stack=st)
        f.dma("sp", bglu, W["ssm_b_glu"][l].rearrange("(g p) -> p g", p=128), allow_slow_non_contiguous=True)
        dsk = f.sb([128, 4], F32, "dsk", stack=st)
        f.dma("sp", dsk, W["ssm_d"][l].rearrange("(g p) -> p g", p=128), allow_slow_non_contiguous=True)
        trib = f.sb([128, 128], BF16, "trib", stack=st)
        f.dma("sp", trib, C["tri"])
        carry = f.sb([64, 2, 32], F32, "carry", stack=st)
        f.memset("dve", carry, 0.0)
        gcl = f.sb([64, 2, 32], F32, "gcl", stack=st)
        us_b = [f.sb([128, 4, 128], BF16, "usb%d" % i, stack=st) for i in range(2)]
        Zr = [f.sb([128, 512], BF16, "Zr%d" % i, stack=st) for i in range(2)]
        Zi = [f.sb([128, 512], BF16, "Zi%d" % i, stack=st) for i in range(2)]
        zt1 = [f.sb([128, 512], F32, "zt1%d" % i, stack=st) for i in range(2)]
        zt2 = [f.sb([128, 512], F32, "zt2%d" % i, stack=st) for i in range(2)]
        GCr = [f.sb([64, 8, 128], F32, "GCr%d" % i, stack=st) for i in range(2)]
        GCi = [f.sb([64, 8, 128], F32, "GCi%d" % i, stack=st) for i in range(2)]
        ht1 = [f.sb([64, 8, 128], F32, "ht1%d" % i, stack=st) for i in range(2)]
        ht2 = [f.sb([64, 8, 128], F32, "ht2%d" % i, stack=st) for i in range(2)]
        Hr = [f.sb([64, 8, 128], BF16, "Hr%d" % i, stack=st) for i in range(2)]
        Hi = [f.sb([64, 8, 128], BF16, "Hi%d" % i, stack=st) for i in range(2)]
        ysb = f.sb([128, 4, 128], F32, "ysb", stack=st)
        zT = [f.sb([128, 4, 128], BF16, "zT%d" % i, stack=st) for i in range(2)]
        sg = f.sb([128, 128], F32, "sg", stack=st)
        osst = [f.sb([128, 4, 128], BF16, "osst%d" % i, stack=st) for i in range(2)]
        ct1 = f.sb([64, 32], F32, "ct1", stack=st)
        ct2 = f.sb([64, 32], F32, "ct2", stack=st)
        usv = uT_d.rearrange("(i p) s -> p i s", p=128)
        if ARM == "P4b":
            f.arm()
        for ch in range(0 if SKIP4B else NT):
            f.maybe_switch()
            us = us_b[ch % 2]
            f.dma("sp", us, usv[:, 4:8, 16 + ch * 128:16 + (ch + 1) * 128])
            zt_ = zT[ch % 2]
            for o in range(4):
                k = o % 2
                f.matmul(PS[0], us[:, o, :], BD[:, o, 0, :])
                f.matmul(PS[1], us[:, o, :], BD[:, o, 1, :])
                dmr = Dmr[:, o * 512:(o + 1) * 512]
                dmi = Dmi[:, o * 512:(o + 1) * 512]
                f.tt("dve", zt1[k], PS[0], dmr, ALU.mult)
                f.tt("dve", zt2[k], PS[1], dmi, ALU.mult)
                f.tt("pool", Zr[k], zt1[k], zt2[k], ALU.subtract)
                f.tt("dve", zt1[k], PS[0], dmi, ALU.mult)
                f.tt("dve", zt2[k], PS[1], dmr, ALU.mult)
                f.tt("pool", Zi[k], zt1[k], zt2[k], ALU.add)
                for gg in range(8):
                    f.matmul(PS[2 + gg // 4][0:64, (gg % 4) * 128:(gg % 4 + 1) * 128], Zr[k][:, gg * 64:(gg + 1) * 64], trib)
                    f.matmul(PS[4 + gg // 4][0:64, (gg % 4) * 128:(gg % 4 + 1) * 128], Zi[k][:, gg * 64:(gg + 1) * 64], trib)
                for hh in range(2):
                    cb_r = carry[:, 0, 8 * o + 4 * hh:8 * o + 4 * hh + 4].unsqueeze(2).broadcast_to([64, 4, 128])
                    cb_i = carry[:, 1, 8 * o + 4 * hh:8 * o + 4 * hh + 4].unsqueeze(2).broadcast_to([64, 4, 128])
                    f.tt("dve", GCr[k][:, 4 * hh:4 * hh + 4, :], PS[2 + hh][0:64, :].rearrange("p (g t) -> p g t", g=4), cb_r, ALU.add)
                    f.tt("dve", GCi[k][:, 4 * hh:4 * hh + 4, :], PS[4 + hh][0:64, :].rearrange("p (g t) -> p g t", g=4), cb_i, ALU.add)
                f.copy("pool", gcl[:, 0, 8 * o:8 * o + 8], GCr[k][:, :, 127])
                f.copy("pool", gcl[:, 1, 8 * o:8 * o + 8], GCi[k][:, :, 127])
                dpr = Dpr[:, 8 * o:8 * o + 8, 0:128]
                dpi = Dpi[:, 8 * o:8 * o + 8, 0:128]
                f.tt("pool", ht1[k], GCr[k], dpr, ALU.mult)
                f.tt("dve", ht2[k], GCi[k], dpi, ALU.mult)
                f.tt("pool", Hr[k], ht1[k], ht2[k], ALU.subtract)
                f.tt("pool", ht1[k], GCr[k], dpi, ALU.mult)
                f.tt("dve", ht2[k], GCi[k], dpr, ALU.mult)
                f.tt("pool", Hi[k], ht1[k], ht2[k], ALU.add)
                yp = PS[6]
                for gg in range(8):
                    f.matmul(yp[:, 0:128], CTp[:, 0, 8 * o + gg, :], Hr[k][:, gg, :], start=(gg == 0), stop=False)
                    f.matmul(yp[:, 0:128], CTp[:, 1, 8 * o + gg, :], Hi[k][:, gg, :], start=False, stop=(gg == 7))
                f.ts("pool", ysb[:, o, :], us[:, o, :], dsk[:, o:o + 1], None, ALU.mult)
                f.tt("dve", ysb[:, o, :], yp[:, 0:128], ysb[:, o, :], ALU.add)
                f.act(zt_[:, o, :], ysb[:, o, :], AF.Gelu)
            l128r = Dpr[:, :, 128]
            l128i = Dpi[:, :, 128]
            f.tt("dve", ct1, gcl[:, 0, :], l128r, ALU.mult)
            f.tt("dve", ct2, gcl[:, 1, :], l128i, ALU.mult)
            f.tt("dve", carry[:, 0, :], ct1, ct2, ALU.subtract)
            f.tt("dve", ct1, gcl[:, 0, :], l128i, ALU.mult)
            f.tt("dve", ct2, gcl[:, 1, :], l128r, ALU.mult)
            f.tt("dve", carry[:, 1, :], ct1, ct2, ALU.add)
            oss = osst[ch % 2]
            for co in range(4):
                gp = PS[0] if co % 2 == 0 else PS[1]
                for ci_ in range(4):
                    f.matmul(gp[:, 0:128], wglu[:, ci_, co * 128:(co + 1) * 128], zt_[:, ci_, :], start=(ci_ == 0), stop=(ci_ == 3))
                f.act(sg, gp[:, 0:128], AF.Sigmoid, bias=bglu[:, co:co + 1])
                f.tt("pool", oss[:, co, :], zt_[:, co, :], sg, ALU.mult)
            f.dma("sp", osT_d.rearrange("(i p) s -> p i s", p=128)[:, :, ch * 128:(ch + 1) * 128], oss)
        f.barrier()
        st.close()
        if upto == 'P4b':
            f.finish()
            return nc, hc

        st = ExitStack()
        if SKIP3 or SKIP4B:
            zz = f.sb([128, 4, S], BF16, "zz", stack=st)
            f.memset("dve", zz, 0.0)
            if SKIP3:
                f.dma("sp", onT_d.rearrange("(i p) s -> p i s", p=128), zz)
            if SKIP4B:
                f.dma("sp", osT_d.rearrange("(i p) s -> p i s", p=128), zz)
            f.barrier()
        wbg = f.sb([128, 8, 3072], BF16, "wbg", stack=st)
        for kt in range(8):
            f.dma("pool", wbg[:, kt, :], wv[:, kt, C_BG:INW])
        wup = f.sb([128, 3, 4, D], BF16, "wup", stack=st)
        for bi, nm_ in enumerate(["w_up_nsa", "w_up_pool", "w_up_ssm"]):
            f.dma("pool", wup[:, bi], W[nm_][l].rearrange("(i p) o -> p i o", p=128))
        wo = f.sb([128, 8, D], BF16, "wo", stack=st)
        f.dma("pool", wo, W["w_out"][l].rearrange("(k p) o -> p k o", p=128))
        g1 = f.sb([128, D], F32, "g1", stack=st)
        b1_ = f.sb([128, D], F32, "b1_", stack=st)
        f.dma("sp", g1, W["ln1_g"][l:l + 1, :].partition_broadcast(128))
        f.dma("sp", b1_, W["ln1_b"][l:l + 1, :].partition_broadcast(128))
        xs_b = [f.sb([128, 8, 512], BF16, "p5x%d" % i, stack=st) for i in range(2)]
        ob_b = [f.sb([128, 3, 4, 512], BF16, "p5o%d" % i, stack=st) for i in range(2)]
        xr_b = [f.sb([128, D], F32, "p5r%d" % i, stack=st) for i in range(2)]
        sig = [f.sb([128, 512], F32, "sig%d" % i, stack=st) for i in range(2)]
        mg = [f.sb([128, 512], F32, "mg%d" % i, stack=st) for i in range(2)]
        tm5 = [f.sb([128, 512], F32, "tm5%d" % i, stack=st) for i in range(2)]
        mT = f.sb([128, 8, 512], BF16, "mT", stack=st)
        hb = [f.sb([128, D], F32, "hb%d" % i, stack=st) for i in range(2)]
        x1b = [f.sb([128, D], F32, "x1b%d" % i, stack=st) for i in range(2)]
        srcs = [onT_d, opT_d, osT_d]
        cnt = 0
        for tb in range(NTB):
            f.maybe_switch()
            xs = xs_b[tb % 2]
            ob = ob_b[tb % 2]
            tsl = slice(tb * 512, (tb + 1) * 512)
            f.dma("sp", xs, xT_d.rearrange("(k p) s -> p k s", p=128)[:, :, tsl])
            for bi in range(3):
                f.dma("sp", ob[:, bi], srcs[bi].rearrange("(i p) s -> p i s", p=128)[:, :, tsl])
            for co in range(8):
                m_ = mg[co % 2]
                for bi in range(3):
                    pg = PS[cnt % 2]
                    pu = PS[2 + cnt % 2]
                    cnt += 1
                    for kt in range(8):
                        f.matmul(pg, wbg[:, kt, bi * 1024 + co * 128: bi * 1024 + (co + 1) * 128], xs[:, kt, :], start=(kt == 0), stop=(kt == 7))
                    for i in range(4):
                        f.matmul(pu, wup[:, bi, i, co * 128:(co + 1) * 128], ob[:, bi, i, :], start=(i == 0), stop=(i == 3))
                    sg_ = sig[cnt % 2]
                    f.act(sg_, pg, AF.Sigmoid)
                    if bi == 0:
                        f.tt("dve", m_, pu, sg_, ALU.mult)
                    elif bi == 1:
                        t_ = tm5[cnt % 2]
                        f.tt("dve", t_, pu, sg_, ALU.mult)
                        f.tt("pool", m_, m_, t_, ALU.add)
                    else:
                        t_ = tm5[cnt % 2]
                        f.tt("dve", t_, pu, sg_, ALU.mult)
                        f.tt("pool", mT[:, co, :], m_, t_, ALU.add)
            for sub in range(4):
                t0 = tb * 512 + sub * 128
                xr = xr_b[sub % 2]
                f.dma("sp", xr, xsrc[t0:t0 + 128, :])
                h_ = hb[sub % 2]
                for hf in range(2):
                    po = PS[4 + hf]
                    for co in range(8):
                        f.matmul(po, mT[:, co, sub * 128:(sub + 1) * 128], wo[:, co, hf * 512:(hf + 1) * 512], start=(co == 0), stop=(co == 7))
                    f.stt("dve", h_[:, hf * 512:(hf + 1) * 512], po, 1.0 / ALPHA, xr[:, hf * 512:(hf + 1) * 512], ALU.mult, ALU.add)
                x1 = x1b[sub % 2]
                layer_norm(st, h_, g1, b1_, x1, eps=1e-5 / (ALPHA * ALPHA))
                f.dma("sp", x1_d[t0:t0 + 128, :], x1)
        f.barrier()
        st.close()
        if upto == 'P5':
            f.finish()
            return nc, hc

        st = ExitStack()
        TB = min(S, 2048)
        NSUB = TB // 128
        wr = f.sb([128, 8, 36], F32, "wr", stack=st)
        f.dma("sp", wr[:, :, 0:4], W["router_w_grp"][l].rearrange("(k p) n -> p k n", p=128))
        f.dma("sp", wr[:, :, 4:36], W["router_w_exp"][l].rearrange("(k p) n -> p k n", p=128))
        wrh = f.sb([128, 8, 36], BF16, "wrh", stack=st)
        wrl = f.sb([128, 8, 36], BF16, "wrl", stack=st)
        wrt = f.sb([128, 8, 36], F32, "wrt", stack=st)
        f.copy("dve", wrh, wr)
        f.copy("dve", wrt, wrh)
        f.tt("dve", wrt, wr, wrt, ALU.subtract)
        f.copy("dve", wrl, wrt)
        xsplit = [f.sb([128, 8, 128], F32, "xsp0", stack=st), f.sb([128, 8, 128], BF16, "xsp1", stack=st)]
        xb16 = f.sb([128, 8, 128], BF16, "xb16", stack=st)
        br_ = f.sb([128, 36], F32, "br_", stack=st)
        f.dma("sp", br_[:, 0:4], W["router_b_grp"][l:l + 1, :].partition_broadcast(128))
        f.dma("sp", br_[:, 4:36], W["router_b_exp"][l:l + 1, :].partition_broadcast(128))
        g2 = f.sb([128, D], F32, "g2", stack=st)
        b2_ = f.sb([128, D], F32, "b2_", stack=st)
        f.dma("sp", g2, W["ln2_g"][l:l + 1, :].partition_broadcast(128))
        f.dma("sp", b2_, W["ln2_b"][l:l + 1, :].partition_broadcast(128))
        x1T = f.sb([128, 8, TB], BF16, "x1T", stack=st)
        acc = f.sb([128, NSUB, D], F32, "acc", stack=st)
        gw = f.sb([128, NSUB, 32], F32, "gw", stack=st)
        x1f = [f.sb([128, D], F32, "x1f%d" % i, stack=st) for i in range(2)]
        x1Tf = [f.sb([128, 8, 128], F32, "x1Tf%d" % i, stack=st) for i in range(2)]
        wgb = [f.sb([128, 8, FF], BF16, "wgb%d" % i, stack=st) for i in range(2)]
        wub = [f.sb([128, 8, FF], BF16, "wub%d" % i, stack=st) for i in range(2)]
        wdb = [f.sb([128, 4, D], BF16, "wdb%d" % i, stack=st) for i in range(2)]
        sil = [f.sb([128, 512], F32, "sil%d" % i, stack=st) for i in range(2)]
        hT_ = [f.sb([128, 4, 512], BF16, "hT_%d" % i, stack=st) for i in range(2)]
        ytmp = [f.sb([128, 512], F32, "ytmp%d" % i, stack=st) for i in range(2)]
        lg = f.sb([128, 36], F32, "lg", stack=st)
        r4 = f.sb([128, 8], F32, "r4", stack=st)
        oh4 = f.sb([128, 4], F32, "oh4", stack=st)
        sl8 = f.sb([128, 8], F32, "sl8", stack=st)
        sl16 = f.sb([128, 16], F32, "sl16", stack=st)
        f.memset("dve", sl16, -1.0e30)
        e8 = f.sb([128, 8], F32, "e8", stack=st)
        mk1 = f.sb([128, 8], F32, "mk1", stack=st)
        mk2 = f.sb([128, 8], F32, "mk2", stack=st)
        w8 = f.sb([128, 8], F32, "w8", stack=st)
        if ARM == "P6":
            f.arm()
        for blk in range(S // TB):
            for sub in range(NSUB):
                t0 = blk * TB + sub * 128
                xf = x1f[sub % 2]
                f.dma("sp", xf, x1_d[t0:t0 + 128, :])
                xtf = x1Tf[sub % 2]
                for hh in range(2):
                    ps = PS[hh]
                    for k4 in range(4):
                        kt = hh * 4 + k4
                        f.transpose(ps[:, k4 * 128:(k4 + 1) * 128], xf[:, kt * 128:(kt + 1) * 128], identf)
                    f.act(xtf[:, hh * 4:(hh + 1) * 4, :], ps, AF.Copy)
                    f.copy("pool", x1T[:, hh * 4:(hh + 1) * 4, sub * 128:(sub + 1) * 128], xtf[:, hh * 4:(hh + 1) * 4, :])
                xh = x1T[:, :, sub * 128:(sub + 1) * 128]
                xhf = xsplit[0]
                f.copy("pool", xhf, xh)
                f.tt("pool", xhf, xtf, xhf, ALU.subtract)
                f.copy("pool", xsplit[1], xhf)
                pl = PS[2]
                n_ = 0
                for (xa, wa_) in ((xh, wrh), (xh, wrl), (xsplit[1], wrh)):
                    for kt in range(8):
                        f.matmul(pl[:, 0:36], xa[:, kt, :], wa_[:, kt, :], start=(n_ == 0), stop=(n_ == 23))
                        n_ += 1
                f.tt("dve", lg, pl[:, 0:36], br_, ALU.add)
                f.tt("dve", r4[:, 1:3], lg[:, 0:2], lg[:, 2:4], ALU.max)
                f.tt("dve", r4[:, 0:1], r4[:, 1:2], r4[:, 2:3], ALU.max)
                f.ts("dve", oh4, lg[:, 0:4], r4[:, 0:1], None, ALU.is_ge)
                f.ts("dve", r4[:, 1:5], lg[:, 0:4], r4[:, 0:1], None, ALU.subtract)
                f.act(r4[:, 1:5], r4[:, 1:5], AF.Exp)
                f.tt("dve", r4[:, 1:3], r4[:, 1:3], r4[:, 3:5], ALU.add)
                f.tt("dve", r4[:, 5:6], r4[:, 1:2], r4[:, 2:3], ALU.add)
                f.op("dve", lambda: nc.vector.reciprocal(out=r4.ap[:, 6:7], in_=r4.ap[:, 5:6]), [r4], [r4])
                f.ts("dve", sl8, lg[:, 4:12], oh4[:, 0:1], None, ALU.mult)
                for g_ in range(1, 4):
                    f.stt("dve", sl8, lg[:, 4 + 8 * g_:12 + 8 * g_], oh4[:, g_:g_ + 1], sl8, ALU.mult, ALU.add)
                f.copy("dve", sl16[:, 0:8], sl8)
                f.op("dve", lambda: nc.vector.max(out=e8.ap, in_=sl16.ap), [sl16], [e8])
                f.ts("dve", mk1, sl8, e8[:, 0:1], None, ALU.is_ge)
                f.ts("dve", mk2, sl8, e8[:, 1:2], None, ALU.is_ge)
                f.tt("dve", mk2, mk2, mk1, ALU.subtract)
                f.tt("dve", r4[:, 0:1], e8[:, 1:2], e8[:, 0:1], ALU.subtract)
                f.act(r4[:, 0:1], r4[:, 0:1], AF.Exp)
                f.ts("dve", r4[:, 1:2], r4[:, 0:1], 1.0, None, ALU.add)
                f.op("dve", lambda: nc.vector.reciprocal(out=r4.ap[:, 1:2], in_=r4.ap[:, 1:2]), [r4], [r4])
                f.tt("dve", r4[:, 1:2], r4[:, 1:2], r4[:, 6:7], ALU.mult)
                f.tt("dve", r4[:, 2:3], r4[:, 1:2], r4[:, 0:1], ALU.mult)
                f.ts("dve", w8, mk1, r4[:, 1:2], None, ALU.mult)
                f.stt("dve", w8, mk2, r4[:, 2:3], w8, ALU.mult, ALU.add)
                for g_ in range(4):
                    f.ts("dve", gw[:, sub, 8 * g_:8 * g_ + 8], w8, oh4[:, g_:g_ + 1], None, ALU.mult)
            if P6STOP == 1:
                f.finish()
                return nc, hc
            def load_expert(ee):
                f.dma("pool", wgb[ee % 2], W["moe_w_gate"][l, ee].rearrange("(k p) n -> p k n", p=128))
                f.dma("pool", wub[ee % 2], W["moe_w_up"][l, ee].rearrange("(k p) n -> p k n", p=128))
                f.dma("pool", wdb[ee % 2], W["moe_w_down"][l, ee].rearrange("(k p) n -> p k n", p=128))

            load_expert(0)
            for e_ in range(NEXP):
                wg_, wu_, wd_ = wgb[e_ % 2], wub[e_ % 2], wdb[e_ % 2]
                if e_ + 1 < NEXP:
                    load_expert(e_ + 1)
                for cc in range(TB // 512):
                    hT = hT_[cc % 2]
                    for fi in range(4):
                        pg = PS[(fi % 2)]
                        pu = PS[2 + (fi % 2)]
                        for kt in range(8):
                            f.matmul(pg, wg_[:, kt, fi * 128:(fi + 1) * 128], x1T[:, kt, cc * 512:(cc + 1) * 512], start=(kt == 0), stop=(kt == 7))
                        for kt in range(8):
                            f.matmul(pu, wu_[:, kt, fi * 128:(fi + 1) * 128], x1T[:, kt, cc * 512:(cc + 1) * 512], start=(kt == 0), stop=(kt == 7))
                        s_ = sil[fi % 2]
                        f.act(s_, pg, AF.Silu)
                        f.tt("dve", hT[:, fi, :], pu, s_, ALU.mult)
                    for s4 in range(4):
                        sub = cc * 4 + s4
                        for hf in range(2):
                            py = PS[4 + (s4 * 2 + hf) % 3]
                            for fi in range(4):
                                f.matmul(py, hT[:, fi, s4 * 128:(s4 + 1) * 128], wd_[:, fi, hf * 512:(hf + 1) * 512], start=(fi == 0), stop=(fi == 3))
                            a_ = acc[:, sub, hf * 512:(hf + 1) * 512]
                            gsc = gw[:, sub, e_:e_ + 1]
                            if e_ == 0:
                                f.ts("dve", a_, py, gsc, None, ALU.mult)
                            elif (s4 * 2 + hf) % 2 == 0:
                                f.stt("dve", a_, py, gsc, a_, ALU.mult, ALU.add)
                            else:
                                yt = ytmp[s4 % 2]
                                f.act(yt, py, AF.Copy, scale=gsc)
                                f.tt("pool", a_, a_, yt, ALU.add)
            if P6STOP == 2:
                f.finish()
                return nc, hc
            for sub in range(NSUB):
                t0 = blk * TB + sub * 128
                xf = x1f[sub % 2]
                f.dma("sp", xf, x1_d[t0:t0 + 128, :])
                f.stt("dve", acc[:, sub, :], xf, ALPHA, acc[:, sub, :], ALU.mult, ALU.add)
                x2 = x1Tf[sub % 2].v().rearrange("p k t -> p (k t)")
                layer_norm(st, acc[:, sub, :], g2, b2_, x2)
                f.dma("sp", xdst[t0:t0 + 128, :], x2)
        f.barrier()
        st.close()
        if upto == 'P6':
            f.finish()
            return nc, hc

    f.finish()
    print('build done: ninst', f.ninst, 'nsem', f.nsem, {kk: len(v) for kk, v in f.semlist.items() if len(v) > 1})
    return nc, hc


_CACHE = {}


def _get(S, L):
    key = (S, L)
    if key not in _CACHE:
        _CACHE[key] = build(S, L)
    return _CACHE[key]


def kernel(**inputs):
    x = np.asarray(inputs["x"], dtype=np.float32)
    B, S, _ = x.shape
    L = inputs["w_in"].shape[0]
    nc, hc = _get(S, L)
    base = {k: np.ascontiguousarray(np.asarray(inputs[k], dtype=np.float32)) for k in WNAMES}
    for k, v in hc.items():
        base["c_" + k] = np.ascontiguousarray(v)
    ncores = B
    in_maps = []
    for c in range(ncores):
        m = dict(base)
        m["x"] = np.ascontiguousarray(x[c % B])
        in_maps.append(m)
    res = run_bass_kernel_spmd(nc, in_maps, core_ids=list(range(ncores)))
    out = np.stack([np.asarray(res.results[c]["out"]) for c in range(B)], axis=0)
    return out.astype(np.float32)
```

```python
import numpy as np
import concourse.bass as bass
import concourse.mybir as mybir
from contextlib import ExitStack

F32 = mybir.dt.float32
BF16 = mybir.dt.bfloat16
I32 = mybir.dt.int32
AF = mybir.ActivationFunctionType
ALU = mybir.AluOpType
AX = mybir.AxisListType


class Tile:
    def __init__(self, ap, name=""):
        self.ap = ap[:]
        self.name = name
        self.w = None
        self.r = {}

    def __getitem__(self, idx):
        return View(self, self.ap[idx])

    def v(self):
        return View(self, self.ap)


class View:
    def __init__(self, tile, ap):
        self.tile = tile
        self.ap = ap

    def __getitem__(self, idx):
        return View(self.tile, self.ap[idx])

    def rearrange(self, pat, **kw):
        return View(self.tile, self.ap.rearrange(pat, **kw))

    def unsqueeze(self, ax):
        return View(self.tile, self.ap.unsqueeze(ax))

    def broadcast_to(self, shp):
        return View(self.tile, self.ap.broadcast_to(list(shp)))

    def bitcast(self, dt):
        return View(self.tile, self.ap.bitcast(dt))


def _ap(v):
    return v.ap if isinstance(v, (View, Tile)) else v


def _tile(v):
    if isinstance(v, View):
        return v.tile
    if isinstance(v, Tile):
        return v
    return None


class FW:
    NDMA = 6
    import os as _os
    LIMIT = int(_os.environ.get('FWLIMIT', '40000'))

    def __init__(self, nc):
        self.nc = nc
        self.es = ExitStack()
        self.eng = {"pe": nc.tensor, "act": nc.scalar, "dve": nc.vector, "pool": nc.gpsimd, "sp": nc.sync}
        self.semlist = {}
        self.val = {}
        self.last = {}
        self.nsem = 0
        for k in ["pe", "act", "dve", "pool"]:
            self.semlist[k] = [self._newsem(k)]
            self.val[k] = 0
        self.dmaq = {}
        for q in ["sp", "pool"]:
            keys = []
            for i in range(self.NDMA):
                k = "d_%s%d" % (q, i)
                self.semlist[k] = [self._newsem(k)]
                self.val[k] = 0
                keys.append(k)
            self.dmaq[q] = [keys, 0]
        self.seen = {e: {} for e in self.eng}
        self.ninst = 0

    def _newsem(self, k):
        self.nsem += 1
        return self.es.enter_context(self.nc.semaphore("s_%s_%d" % (k, self.nsem)))

    def _bump(self, k, inc):
        if self.val[k] + inc > self.LIMIT:
            self.semlist[k].append(self._newsem(k))
            self.val[k] = 0
        self.val[k] += inc
        idx = len(self.semlist[k]) - 1
        ev = (k, idx, self.val[k])
        self.last[k] = ev
        return self.semlist[k][idx], ev

    def sb(self, shape, dt, name=None, stack=None):
        st = stack if stack is not None else self.es
        self.nalloc = getattr(self, "nalloc", 0) + 1
        nm = "%s_%d" % (name or "t", self.nalloc)
        t = st.enter_context(self.nc.sbuf_tensor(nm, list(shape), dt))
        return Tile(t, nm)

    def ps(self, shape, dt=F32, name=None, stack=None):
        st = stack if stack is not None else self.es
        t = st.enter_context(self.nc.psum_tensor(list(shape), dt))
        return Tile(t, name or "")

    def _wait(self, e, ev):
        if ev is None:
            return
        k, idx, v = ev
        if self.seen[e].get((k, idx), 0) >= v:
            return
        for (k2, i2), v2 in self.seen[e].items():
            if k2 == k and i2 > idx:
                return
        self.eng[e].wait_ge(self.semlist[k][idx], v)
        self.seen[e][(k, idx)] = v

    def _deps(self, e, reads, writes):
        for t in reads:
            if t is None:
                continue
            if t.w is not None and not (e == "pe" and t.w[0] == "pe"):
                self._wait(e, t.w)
        for t in writes:
            if t is None:
                continue
            if t.w is not None and not (e == "pe" and t.w[0] == "pe"):
                self._wait(e, t.w)
            for k, ev in t.r.items():
                if e == "pe" and k == "pe":
                    continue
                self._wait(e, ev)

    def _commit(self, ev, reads, writes):
        for t in reads:
            if t is None:
                continue
            t.r[ev[0]] = ev
        for t in writes:
            if t is None:
                continue
            t.w = ev
            t.r = {}

    armed = False
    budget = 10 ** 9

    def arm(self):
        import os
        self.armed = True
        self.budget = int(os.environ.get("FWBUDGET", str(10 ** 9)))
        self.skipped = 0

    def _spend(self):
        if not self.armed:
            return True
        if self.budget <= 0:
            self.skipped += 1
            return False
        self.budget -= 1
        return True

    def op(self, e, ins_fn, reads, writes):
        if not self._spend():
            return None
        reads = [_tile(r) for r in reads]
        writes = [_tile(w) for w in writes]
        self._deps(e, reads, writes)
        ins = ins_fn()
        sem, ev = self._bump(e, 1)
        ins.then_inc(sem, 1)
        self._commit(ev, reads, writes)
        self.ninst += 1
        return ins

    def dma(self, q, out, in_, **kw):
        if not self._spend():
            return None
        keys, idx = self.dmaq[q]
        k = keys[idx % len(keys)]
        self.dmaq[q][1] = idx + 1
        e = q
        reads = [_tile(in_)]
        writes = [_tile(out)]
        if k in self.last:
            self._wait(e, self.last[k])
        self._deps(e, reads, writes)
        ins = self.eng[e].dma_start(out=_ap(out), in_=_ap(in_), **kw)
        sem, ev = self._bump(k, 16)
        ins.then_inc(sem, 16)
        self._commit(ev, reads, writes)
        self.ninst += 1
        return ins

    def barrier(self, engines=None):
        engines = engines or list(self.eng.keys())
        for e in engines:
            for k, ev in self.last.items():
                self._wait(e, ev)

    def maybe_switch(self, force=False):
        return

    def matmul(self, out, lhsT, rhs, start=True, stop=True):
        return self.op("pe", lambda: self.nc.tensor.matmul(_ap(out), _ap(lhsT), _ap(rhs), start=start, stop=stop),
                       [lhsT, rhs], [out])

    def transpose(self, out, in_, ident):
        return self.op("pe", lambda: self.nc.tensor.transpose(_ap(out), _ap(in_), _ap(ident)), [in_, ident], [out])

    def act(self, out, in_, func, bias=None, scale=None, accum_out=None, e="act"):
        def _fl(v):
            a = _ap(v)
            if len(a.shape) == 3:
                return View(_tile(v), a.rearrange("p a b -> p (a b)"))
            return v
        out = _fl(out)
        in_ = _fl(in_)
        kw = {}
        rd = [in_]
        wr = [out]
        if bias is not None:
            kw["bias"] = _ap(bias)
            if not isinstance(bias, (int, float)):
                rd.append(bias)
        if scale is not None:
            kw["scale"] = _ap(scale)
            if not isinstance(scale, (int, float)):
                rd.append(scale)
        if accum_out is not None:
            kw["accum_out"] = _ap(accum_out)
            wr.append(accum_out)
        return self.op(e, lambda: self.nc.scalar.activation(_ap(out), _ap(in_), func, **kw), rd, wr)

    def tt(self, e, out, a, b, op):
        return self.op(e, lambda: self.eng[e].tensor_tensor(_ap(out), _ap(a), _ap(b), op), [a, b], [out])

    def ts(self, e, out, a, s1, s2, op0, op1=None, accum_out=None):
        rd = [a]
        wr = [out]
        if not isinstance(s1, (int, float)):
            rd.append(s1)
        if s2 is not None and not isinstance(s2, (int, float)):
            rd.append(s2)
        kw = {}
        if op1 is not None:
            kw["op1"] = op1
        if accum_out is not None:
            kw["accum_out"] = _ap(accum_out)
            wr.append(accum_out)
        return self.op(e, lambda: self.eng[e].tensor_scalar(_ap(out), _ap(a), _ap(s1), _ap(s2) if s2 is not None else None, op0, **kw), rd, wr)

    def stt(self, e, out, a, s, b, op0, op1):
        rd = [a, b]
        if not isinstance(s, (int, float)):
            rd.append(s)
        return self.op(e, lambda: self.eng[e].scalar_tensor_tensor(_ap(out), _ap(a), _ap(s), _ap(b), op0, op1), rd, [out])

    def copy(self, e, out, in_):
        if e == "act":
            return self.act(out, in_, AF.Copy)
        return self.op(e, lambda: self.eng[e].tensor_copy(_ap(out), _ap(in_)), [in_], [out])

    def memset(self, e, out, val):
        return self.op(e, lambda: self.eng[e].memset(_ap(out), val), [], [out])

    def reduce(self, e, out, in_, op, axis=AX.X):
        return self.op(e, lambda: self.eng[e].tensor_reduce(_ap(out), _ap(in_), axis, op), [in_], [out])

    def finish(self):
        self.barrier()

import math
import ml_dtypes
from concourse.bass_utils import run_bass_kernel_spmd

D = 1024
NH = 8
HD = 64
NG = 2
RR = 4
INW = 5400
NEXP = 32
FF = 512
DEPTH_FULL = 4
ALPHA = (2 * DEPTH_FULL) ** 0.25
NEG = -30000.0
SLOPES = [2.0 ** (-(h + 1)) for h in range(8)]
C_Q, C_KC, C_VC, C_KS, C_VS, C_KW, C_VW, C_NG, C_UP, C_US, C_BG = 0, 512, 640, 768, 896, 1024, 1152, 1280, 1304, 1816, 2328

WNAMES = ["w_in", "cmp_pe_k", "cmp_pe_v", "cmp_w1_k", "cmp_w2_k", "cmp_w1_v", "cmp_w2_v", "pool_w", "pool_scale",
          "ssm_lam_re", "ssm_lam_im", "ssm_log_dt", "ssm_b_re", "ssm_b_im", "ssm_c_re", "ssm_c_im", "ssm_d",
          "ssm_w_glu", "ssm_b_glu", "w_up_nsa", "w_up_pool", "w_up_ssm", "w_out", "ln1_g", "ln1_b",
          "router_w_grp", "router_b_grp", "router_w_exp", "router_b_exp", "moe_w_gate", "moe_w_up", "moe_w_down",
          "ln2_g", "ln2_b"]


def host_consts(S):
    bf = ml_dtypes.bfloat16
    NC_ = S // 16
    NJ = S // 64
    NT = S // 128
    c = {}
    t = np.arange(S)
    qa = np.zeros((8, 4, S), np.float32)
    for h in range(8):
        sl = SLOPES[h]
        qa[h, 0] = sl * 128
        qa[h, 1] = sl
        qa[h, 2] = -sl * 128 * (t // 128)
        qa[h, 3] = -sl * (t % 128)
    c["qaug"] = qa.astype(bf)
    ka = np.zeros((4, S + 16), np.float32)
    ka[0, :S] = t // 128
    ka[1, :S] = t % 128
    ka[2] = 1
    ka[3] = 1
    c["kaug"] = ka.astype(bf)
    pc = np.arange(NC_) * 16 + 31
    kc = np.zeros((4, NC_), np.float32)
    kc[0] = pc // 128
    kc[1] = pc % 128
    kc[2] = 1
    kc[3] = 1
    c["kaugc"] = kc.astype(bf)
    p = np.arange(128)[:, None]
    q = np.arange(128)[None, :]
    nm = np.zeros((128, 16, 512), np.float32)
    for m in range(16):
        valid = (16 * p + 31) <= (128 * m + q)
        nm[:, m, :] = np.tile(np.where(valid, 0.0, NEG), (1, 4))
    c["negcmp"] = nm.astype(bf)
    c["negcausal"] = np.tile(np.where(p <= q, 0.0, NEG), (1, 4)).astype(bf)
    c["negwinlo"] = np.tile(np.where(p > q, 0.0, NEG), (1, 4)).astype(bf)
    E = np.zeros((128, NT, 128), np.float32)
    for kt in range(NT):
        for pp in range(128):
            j = 2 * kt + pp // 64
            if j < 128:
                E[j, kt, pp] = 1.0
    c["etab"] = E.astype(bf)
    ci = np.arange(NC_)[:, None] * 16
    j0 = np.arange(NJ)[None, :] * 64
    ov = ((ci < j0 + 64) & (ci + 32 > j0)).astype(np.float32)
    nct = max(1, NC_ // 128)
    ovp = np.zeros((nct * 128, 128), np.float32)
    ovp[:NC_, :NJ] = ov
    c["overlap"] = ovp.reshape(nct, 128, 128).transpose(1, 0, 2).copy().astype(bf)
    qq = np.arange(128)[:, None]
    jr = np.arange(256)[None, :] - 128
    tbr = (qq >= 64).astype(np.int64)
    forced = (jr == tbr) | (jr == tbr - 1)
    valid = jr <= tbr
    c["vm"] = (valid & ~forced).astype(np.float32)
    c["am"] = np.where(forced, 1.0e4, np.where(valid, 0.0, -1.0)).astype(np.float32)
    c["svals"] = np.arange(128, dtype=np.float32)[:, None].copy()
    c["tvals"] = np.tile(np.arange(129, dtype=np.float32)[None, :], (64, 1)).copy()
    tri = (np.arange(128)[:, None] <= np.arange(128)[None, :]).astype(np.float32)
    c["tri"] = tri.astype(bf)
    pf = np.ones((128, 4, 16), np.float32)
    for g, w in enumerate((2, 4, 8, 16)):
        tt_ = np.arange(16)
        pf[:, g, :] = (w / np.minimum(tt_ + 1, w))[None, :]
    c["poolfix"] = pf
    gm = np.zeros((128, 8), np.float32)
    for pp in range(128):
        gm[pp, pp // 16] = 1.0
    c["gmask"] = gm
    return c


CONST_SPECS = None


import os
P0MODE = int(os.environ.get('P0MODE', '0'))
P4BSTOP = int(os.environ.get('P4BSTOP', '0'))
SKIP3 = int(os.environ.get('SKIP3', '0'))
SKIP4B = int(os.environ.get('SKIP4B', '0'))
P6STOP = int(os.environ.get('P6STOP', '0'))
ARM = os.environ.get('ARM', '')


def build(S, L, dump=False, upto=None):
    NT = S // 128
    NC_ = S // 16
    NCT = max(1, NC_ // 128)
    NJ = S // 64
    NTB = S // 512
    hc = host_consts(S)
    nc = bass.Bass("TRN2", target_bir_lowering=False)
    f = FW(nc)
    inp = {}

    def din(name, shape, dt=F32):
        inp[name] = nc.dram_tensor(name, list(shape), dt, kind="ExternalInput").ap()
        return inp[name]

    x_in = din("x", [S, D])
    full_shapes = {
        "w_in": [L, D, INW], "cmp_pe_k": [L, 32, 64], "cmp_pe_v": [L, 32, 64], "cmp_w1_k": [L, 2048, 128],
        "cmp_w2_k": [L, 128, 64], "cmp_w1_v": [L, 2048, 128], "cmp_w2_v": [L, 128, 64], "pool_w": [L, 4, 128, 128],
        "pool_scale": [L, 512], "ssm_lam_re": [L, 32, 64], "ssm_lam_im": [L, 32, 64], "ssm_log_dt": [L, 32],
        "ssm_b_re": [L, 32, 64, 16], "ssm_b_im": [L, 32, 64, 16], "ssm_c_re": [L, 32, 16, 64], "ssm_c_im": [L, 32, 16, 64],
        "ssm_d": [L, 512], "ssm_w_glu": [L, 512, 512], "ssm_b_glu": [L, 512], "w_up_nsa": [L, 512, D],
        "w_up_pool": [L, 512, D], "w_up_ssm": [L, 512, D], "w_out": [L, D, D], "ln1_g": [L, D], "ln1_b": [L, D],
        "router_w_grp": [L, D, 4], "router_b_grp": [L, 4], "router_w_exp": [L, D, 32], "router_b_exp": [L, 32],
        "moe_w_gate": [L, NEXP, D, FF], "moe_w_up": [L, NEXP, D, FF], "moe_w_down": [L, NEXP, FF, D],
        "ln2_g": [L, D], "ln2_b": [L, D]}
    W = {k: din(k, full_shapes[k]) for k in WNAMES}
    C = {}
    for k, v in hc.items():
        C[k] = din("c_" + k, v.shape, BF16 if v.dtype == ml_dtypes.bfloat16 else F32)
    out_d = nc.dram_tensor("out", [S, D], F32, kind="ExternalOutput").ap()

    def scratch(name, shape, dt):
        return nc.dram_tensor(name, list(shape), dt, kind="ExternalOutput" if dump else "Internal").ap()

    xT_d = scratch("xT_d", [D, S], BF16)
    qT_d = scratch("qT_d", [8, 68, S], BF16)
    kT_d = scratch("kT_d", [8, 68, S + 16], BF16)
    uT_d = scratch("uT_d", [D, 16 + S], BF16)
    v_d = scratch("v_d", [S, 4, 65], BF16)
    gate_d = scratch("gate_d", [S, 24], F32)
    kcmp_d = scratch("kcmp_d", [2, 68, NCT * 128], BF16)
    vcmp_d = scratch("vcmp_d", [NCT * 128, 2, 65], BF16)
    onT_d = scratch("onT_d", [512, S], BF16)
    opT_d = scratch("opT_d", [512, S], BF16)
    osT_d = scratch("osT_d", [512, S], BF16)
    dbg_d = scratch("dbg_d", [8, 128, 512], BF16)
    dbg2_d = scratch("dbg2_d", [3, 128, 260], F32)
    x1_d = scratch("x1_d", [S, D], F32)
    xr_d = scratch("xr_d", [S, D], F32)

    identf = f.sb([128, 128], F32, "identf")
    identb = f.sb([128, 128], BF16, "identb")
    f.memset("pool", identf, 0.0)
    f.op("pool", lambda: nc.gpsimd.affine_select(out=identf.ap, in_=identf.ap, pattern=[[-1, 128]], compare_op=ALU.not_equal,
                                                 fill=1.0, base=0, channel_multiplier=1), [identf], [identf])
    f.copy("dve", identb, identf)
    PS = [f.ps([128, 512], F32, "ps%d" % i) for i in range(7)]
    PSB = f.ps([128, 1024], BF16, "psb")

    st = ExitStack()
    t_qa = f.sb([4, 8, S], BF16, "t_qa", stack=st)
    f.dma("sp", t_qa, C["qaug"].rearrange("h a s -> a h s"))
    f.dma("sp", qT_d.rearrange("h d s -> d h s")[64:68, :, :], t_qa)
    t_ka = f.sb([4, S + 16], BF16, "t_ka", stack=st)
    f.dma("sp", t_ka, C["kaug"])
    for i in range(8):
        f.dma("sp", kT_d[i, 64:68, :], t_ka)
    t_kc = f.sb([4, NC_], BF16, "t_kc", stack=st)
    f.dma("sp", t_kc, C["kaugc"])
    for g in range(2):
        f.dma("sp", kcmp_d[g, 64:68, 0:NC_], t_kc)
    zt = f.sb([128, 8, 16], BF16, "zt", stack=st)
    f.memset("dve", zt, 0.0)
    f.dma("sp", kT_d.rearrange("i d s -> d i s")[0:64, :, S:S + 16], zt[0:64])
    f.dma("sp", uT_d.rearrange("(i p) s -> p i s", p=128)[:, :, 0:16], zt)
    f.barrier()
    st.close()
    if upto == "PRE":
        f.finish()
        return nc, hc

    ei = [0]

    def rot(engs):
        ei[0] += 1
        return engs[ei[0] % len(engs)]

    lnscr = {}

    def layer_norm(st, src, gbc, bbc, dst, eps=1e-5):
        if id(st) not in lnscr:
            lnscr[id(st)] = [[f.sb([128, 2, 6], F32, stack=st), f.sb([128, 2], F32, stack=st)] for _ in range(2)] + [0]
        sc_ = lnscr[id(st)]
        sc_[2] += 1
        stt_, mv = sc_[sc_[2] % 2]
        for c2 in range(2):
            f.op("dve", lambda c2=c2: nc.vector.bn_stats(out=stt_.ap[:, c2, :], in_=src.ap[:, c2 * 512:(c2 + 1) * 512]), [src], [stt_])
        f.op("dve", lambda: nc.vector.bn_aggr(out=mv.ap, in_=stt_.ap), [stt_], [mv])
        f.ts("dve", mv[:, 1:2], mv[:, 1:2], eps, None, ALU.add)
        f.act(mv[:, 1:2], mv[:, 1:2], AF.Sqrt)
        f.op("dve", lambda: nc.vector.reciprocal(out=mv.ap[:, 1:2], in_=mv.ap[:, 1:2]), [mv], [mv])
        f.ts("dve", dst, src, mv[:, 0:1], mv[:, 1:2], ALU.subtract, ALU.mult)
        f.tt("pool", dst, dst, gbc, ALU.mult)
        f.tt("pool", dst, dst, bbc, ALU.add)

    class LNCtx:
        pass

    for l in range(L):
        xsrc = x_in if l == 0 else xr_d
        xdst = out_d if l == L - 1 else xr_d

        st = ExitStack()
        xt_b = [f.sb([128, D], F32, "p0x%d" % i, stack=st) for i in range(2)]
        xT_s = [f.sb([128, 8, 512], BF16, "p0t%d" % i, stack=st) for i in range(2)]
        for tb in range(NTB):
            xs = xT_s[tb % 2]
            for sub in range(4):
                tt0 = tb * 512 + sub * 128
                xb = xt_b[sub % 2]
                f.dma("sp", xb, xsrc[tt0:tt0 + 128, :])
                for hh in range(2):
                    ps = PS[(sub * 2 + hh) % 4]
                    for k4 in range(4):
                        kt = hh * 4 + k4
                        f.transpose(ps[:, k4 * 128:(k4 + 1) * 128], xb[:, kt * 128:(kt + 1) * 128], identf)
                    if P0MODE != 1:
                        f.copy("dve", xs[:, hh * 4:(hh + 1) * 4, sub * 128:(sub + 1) * 128],
                               ps.v().rearrange("p (k t) -> p k t", k=4))
            if P0MODE not in (1, 2):
                f.dma("sp", xT_d.rearrange("(k p) s -> p k s", p=128)[:, :, tb * 512:(tb + 1) * 512], xs)
        f.barrier()
        st.close()
        if upto == 'P0':
            f.finish()
            return nc, hc

        st = ExitStack()
        NP1 = C_BG
        wq = f.sb([128, 8, NP1], BF16, "wq", stack=st)
        wv = W["w_in"][l].rearrange("(k p) n -> p k n", p=128)
        for kt in range(8):
            f.dma("pool", wq[:, kt, :], wv[:, kt, 0:NP1])
        xs_b = [f.sb([128, 8, 512], BF16, "p1x%d" % i, stack=st) for i in range(2)]
        qst = [f.sb([64, 8, 512], BF16, "qst%d" % i, stack=st) for i in range(2)]
        kst = [f.sb([64, 8, 512], BF16, "kst%d" % i, stack=st) for i in range(2)]
        ust = [f.sb([128, 8, 512], BF16, "ust%d" % i, stack=st) for i in range(2)]
        vst = [f.sb([128, 4, 4, 65], BF16, "vst%d" % i, stack=st) for i in range(2)]
        gst = [f.sb([128, 4, 24], F32, "gst%d" % i, stack=st) for i in range(2)]
        ngt = f.sb([128, 24], F32, "ngt", stack=st)
        for i in range(2):
            f.memset("dve", vst[i], 1.0)
        kcols = [C_KC, C_KC + 64, C_VC, C_VC + 64, C_KS, C_KS + 64, C_KW, C_KW + 64]
        pi = 0
        for tb in range(NTB):
            xs = xs_b[tb % 2]
            f.dma("sp", xs, xT_d.rearrange("(k p) s -> p k s", p=128)[:, :, tb * 512:(tb + 1) * 512])
            q_, k_, u_, v_, g_ = qst[tb % 2], kst[tb % 2], ust[tb % 2], vst[tb % 2], gst[tb % 2]
            jobs = []
            for h in range(8):
                jobs.append((C_Q + 64 * h, 64, q_, h, 0.125))
            for i in range(8):
                jobs.append((kcols[i], 64, k_, i, 1.0))
            for i in range(4):
                jobs.append((C_UP + 128 * i, 128, u_, i, 1.0))
            for i in range(4):
                jobs.append((C_US + 128 * i, 128, u_, 4 + i, 1.0))
            for (c0, M, dst, di, sc) in jobs:
                ps = PS[pi % 4]
                pi += 1
                for kt in range(8):
                    f.matmul(ps[0:M, :], wq[:, kt, c0:c0 + M], xs[:, kt, :], start=(kt == 0), stop=(kt == 7))
                e = rot(["dve", "act"])
                if e == "act":
                    f.act(dst[0:M, di, :], ps[0:M, :], AF.Copy, scale=sc)
                else:
                    f.ts("dve", dst[0:M, di, :], ps[0:M, :], sc, None, ALU.mult)
            for sub in range(4):
                ps = PS[4 + sub % 2]
                for kt in range(8):
                    f.matmul(ps[:, 0:408], xs[:, kt, sub * 128:(sub + 1) * 128], wq[:, kt, C_VS:C_VS + 408], start=(kt == 0), stop=(kt == 7))
                f.copy("dve", v_[:, sub, 0:2, 0:64], ps[:, 0:128].rearrange("p (g d) -> p g d", g=2))
                f.copy("dve", v_[:, sub, 2:4, 0:64], ps[:, 256:384].rearrange("p (g d) -> p g d", g=2))
                f.copy("dve", ngt, ps[:, 384:408])
                f.act(g_[:, sub, :], ngt, AF.Sigmoid)
            ts_ = slice(tb * 512, (tb + 1) * 512)
            f.dma("sp", qT_d.rearrange("h d s -> d h s")[0:64, :, ts_], q_)
            f.dma("sp", kT_d.rearrange("i d s -> d i s")[0:64, :, ts_], k_)
            f.dma("sp", uT_d.rearrange("(i p) s -> p i s", p=128)[:, :, 16 + tb * 512:16 + (tb + 1) * 512], u_)
            f.dma("sp", v_d[ts_].rearrange("(n p) j c -> p n j c", p=128), v_)
            f.dma("sp", gate_d[ts_].rearrange("(n p) c -> p n c", p=128), g_)
        f.barrier()
        st.close()
        if upto == 'P1':
            f.finish()
            return nc, hc

        st = ExitStack()
        for which, (w1n, w2n, pen, base) in enumerate([("cmp_w1_k", "cmp_w2_k", "cmp_pe_k", 0), ("cmp_w1_v", "cmp_w2_v", "cmp_pe_v", 2)]):
            w1b = f.sb([64, 32, 128], BF16, "w1b%d" % which, stack=st)
            f.dma("pool", w1b, W[w1n][l].rearrange("(l d) h -> d l h", d=64))
            w2b = f.sb([128, 64], BF16, "w2b%d" % which, stack=st)
            f.dma("pool", w2b, W[w2n][l])
            pe_s = f.sb([32, 64], F32, "pe%d" % which, stack=st)
            f.dma("sp", pe_s, W[pen][l])
            f.transpose(PS[0][0:64, 0:32], pe_s, identf[0:32, 0:32])
            peT = f.sb([64, 32], BF16, "peT%d" % which, stack=st)
            f.copy("dve", peT, PS[0][0:64, 0:32])
            for li in range(32):
                f.matmul(PS[1][:, 0:1], w1b[:, li, :], peT[:, li:li + 1], start=(li == 0), stop=(li == 31))
            b1 = f.sb([128, 1], F32, "b1%d" % which, stack=st)
            f.copy("dve", b1, PS[1][:, 0:1])
            for g in range(2):
                kc_s = f.sb([64, S + 16], BF16, "kcs%d%d" % (which, g), stack=st)
                f.dma("sp", kc_s, kT_d[base + g, 0:64, :])
                kcv = kc_s.v().rearrange("d (n r) -> d n r", r=16)
                for cb in range(0, NC_, 512):
                    nb = min(512, NC_ - cb)
                    for li in range(32):
                        f.matmul(PS[2][:, 0:nb], w1b[:, li, :], kcv[:, cb + li // 16: cb + li // 16 + nb, li % 16], start=(li == 0), stop=(li == 31))
                    hT = f.sb([128, 512], BF16, "hT%d%d" % (which, g), stack=st)
                    f.act(hT[:, 0:nb], PS[2][:, 0:nb], AF.Gelu, bias=b1[:, 0:1])
                    if which == 0:
                        f.matmul(PS[3][0:64, 0:nb], w2b, hT[:, 0:nb])
                        kcm = f.sb([64, 512], BF16, "kcm%d" % g, stack=st)
                        f.copy("dve", kcm[:, 0:nb], PS[3][0:64, 0:nb])
                        f.dma("sp", kcmp_d[g, 0:64, cb:cb + nb], kcm[:, 0:nb])
                    else:
                        vcm = f.sb([128, 4, 65], BF16, "vcm%d" % g, stack=st)
                        f.memset("pool", vcm, 1.0)
                        nbt = (nb + 127) // 128
                        for bt in range(nbt):
                            w_ = min(128, nb - bt * 128)
                            f.matmul(PS[3][0:w_, bt * 64:(bt + 1) * 64], hT[:, bt * 128:bt * 128 + w_], w2b)
                            f.copy("dve", vcm[0:w_, bt, 0:64], PS[3][0:w_, bt * 64:(bt + 1) * 64])
                        if nb >= 128:
                            f.dma("sp", vcmp_d[cb:cb + nb, g, :].rearrange("(n p) c -> p n c", p=128), vcm[:, 0:nbt, :])
                        else:
                            f.dma("sp", vcmp_d[cb:cb + nb, g, :], vcm[0:nb, 0, :])
        f.barrier()
        st.close()
        if upto == 'P2':
            f.finish()
            return nc, hc

        st = ExitStack()
        kT_s = f.sb([68, 4, S], BF16, "kT_s", stack=st)
        for i in range(4):
            f.dma("sp", kT_s[:, i, :], kT_d[4 + i, :, 0:S])
        va_s = f.sb([128, NT, 4, 65], BF16, "va_s", stack=st)
        vdv = v_d.rearrange("(n p) j c -> p n j c", p=128)
        for n0 in range(0, NT, 16):
            n1 = min(NT, n0 + 16)
            f.dma("sp", va_s[:, n0:n1], vdv[:, n0:n1])
        kc_s = f.sb([68, 2, NCT * 128], BF16, "kc_s", stack=st)
        f.memset("dve", kc_s, 0.0)
        f.dma("sp", kc_s[:, :, 0:NC_], kcmp_d.rearrange("g d c -> d g c")[:, :, 0:NC_])
        vc_s = f.sb([128, NCT, 2, 65], BF16, "vc_s", stack=st)
        f.memset("dve", vc_s, 0.0)
        if NC_ >= 128:
            f.dma("sp", vc_s, vcmp_d.rearrange("(n p) g c -> p n g c", p=128))
        else:
            f.dma("sp", vc_s[0:NC_, 0], vcmp_d[0:NC_])
        negcmp = f.sb([128, 16, 512], BF16, "negcmp", stack=st)
        f.dma("sp", negcmp, C["negcmp"])
        negcau = f.sb([128, 512], BF16, "negcau", stack=st)
        f.dma("sp", negcau, C["negcausal"])
        negwl = f.sb([128, 512], BF16, "negwl", stack=st)
        f.dma("sp", negwl, C["negwinlo"])
        etab = f.sb([128, NT, 128], BF16, "etab", stack=st)
        f.dma("sp", etab, C["etab"])
        ovl = f.sb([128, NCT, 128], BF16, "ovl", stack=st)
        f.dma("sp", ovl, C["overlap"])
        vm = f.sb([128, 256], F32, "vm", stack=st)
        f.dma("sp", vm, C["vm"])
        am = f.sb([128, 256], F32, "am", stack=st)
        f.dma("sp", am, C["am"])
        qT_b = [f.sb([68, 8 * 128], BF16, "qTb%d" % i, stack=st) for i in range(2)]
        gt_b = [f.sb([128, 24], F32, "gtb%d" % i, stack=st) for i in range(2)]
        PTc = [f.sb([128, 512], BF16, "ptc%d" % i, stack=st) for i in range(4)]
        PT = [f.sb([128, 512], BF16, "pt%d" % i, stack=st) for i in range(4)]
        oacc = [f.sb([128, 512], F32, "oacc%d" % i, stack=st) for i in range(2)]
        onb = [f.sb([128, 512], BF16, "onb%d" % i, stack=st) for i in range(2)]
        onT = [f.sb([128, 4, 128], BF16, "onT%d" % i, stack=st) for i in range(2)]
        imp = f.sb([128, 128], F32, "imp", stack=st)
        sc1 = f.sb([128, 128], F32, "sc1", stack=st)
        sc2 = f.sb([128, 128], F32, "sc2", stack=st)
        m16 = f.sb([128, 16], F32, "m16", stack=st)
        nsb = f.sb([128, 128], BF16, "nsb", stack=st)
        nselT = [f.sb([128, 512], BF16, "nselT%d" % i, stack=st) for i in range(2)]
        rden = [f.sb([128, 4], F32, "rden%d" % i, stack=st) for i in range(3)]
        scl = [f.sb([128, 4], F32, "scl%d" % i, stack=st) for i in range(3)]
        tmpo = [f.sb([128, 4, 64], F32, "tmpo%d" % i, stack=st) for i in range(2)]
        ST = [PS[0], PS[1]]
        OC, OS_, OW, IMP = PS[2], PS[3], PS[4], PS[5]
        sti = [0]
        pti = [0]

        def score_tile(lhs_k, rhs_q, extra):
            ps = ST[sti[0] % 2]
            sti[0] += 1
            n = 1 + len(extra)
            f.matmul(ps, lhs_k, rhs_q, start=True, stop=(n == 1))
            for i, (a, b) in enumerate(extra):
                f.matmul(ps, a, b, start=False, stop=(i == len(extra) - 1))
            return ps

        for qt in range(0 if SKIP3 else NT):
            f.maybe_switch()
            qTt = qT_b[qt % 2]
            f.dma("sp", qTt.v().rearrange("d (h t) -> d h t", h=8), qT_d.rearrange("h d s -> d h s")[:, :, qt * 128:(qt + 1) * 128])
            gt = gt_b[qt % 2]
            f.dma("sp", gt, gate_d[qt * 128:(qt + 1) * 128, :])
            oa = oacc[qt % 2]
            for g in range(2):
                rq = qTt[:, g * 512:(g + 1) * 512]
                ctl = qt // 16
                for ct in range(ctl + 1):
                    extra = [(identb, negcmp[:, qt % 16, :])] if ct == ctl else []
                    ps = score_tile(kc_s[:, g, ct * 128:(ct + 1) * 128], rq, extra)
                    pt = PTc[ct]
                    f.act(pt, ps, AF.Exp)
                    for r in range(4):
                        f.matmul(OC[:, r * 65:(r + 1) * 65], pt[:, r * 128:(r + 1) * 128], vc_s[:, ct, g, :], start=(ct == 0 and r == 0), stop=(ct == ctl))
                        f.matmul(IMP[:, r * 128:(r + 1) * 128], pt[:, r * 128:(r + 1) * 128], ovl[:, ct, :], start=(ct == 0 and r == 0), stop=(ct == ctl))
                ocv = OC[:, 0:260].rearrange("p (r c) -> p r c", r=4)
                f.ts("dve", rden[0], ocv[:, :, 64], 1e-30, None, ALU.max)
                f.op("dve", lambda: nc.vector.reciprocal(out=rden[0].ap, in_=rden[0].ap), [rden[0]], [rden[0]])
                f.ts("dve", imp, IMP[:, 0:128], rden[0][:, 0:1], None, ALU.mult)
                for r in range(1, 4):
                    f.stt("dve", imp, IMP[:, r * 128:(r + 1) * 128], rden[0][:, r:r + 1], imp, ALU.mult, ALU.add)
                off = 128 - 2 * qt
                f.tt("dve", sc1[:, 0:NJ], imp[:, 0:NJ], vm[:, off:off + NJ], ALU.mult)
                f.tt("dve", sc1[:, 0:NJ], sc1[:, 0:NJ], am[:, off:off + NJ], ALU.add)
                f.memset("dve", sc1[:, 0:1], 1.0e4)
                if NJ < 128:
                    f.memset("dve", sc1[:, NJ:128], -2.0)
                f.op("dve", lambda: nc.vector.max(out=m16.ap[:, 0:8], in_=sc1.ap), [sc1], [m16])
                f.op("dve", lambda: nc.vector.match_replace(out=sc2.ap, in_to_replace=m16.ap[:, 0:8], in_values=sc1.ap, imm_value=-1e9), [sc1, m16], [sc2])
                f.op("dve", lambda: nc.vector.max(out=m16.ap[:, 8:16], in_=sc2.ap), [sc2], [m16])
                f.op("dve", lambda: nc.vector.match_replace(out=sc1.ap, in_to_replace=m16.ap[:, 8:16], in_values=sc2.ap, imm_value=-1e9), [sc2, m16], [sc1])
                f.ts("dve", nsb, sc1, -1e8, NEG, ALU.is_gt, ALU.mult)
                f.transpose(PSB[:, 0:128], nsb, identb)
                nsT = nselT[g]
                for r in range(4):
                    f.copy(rot(["dve", "pool"]) if False else "dve", nsT[:, r * 128:(r + 1) * 128], PSB[:, 0:128])
                k0 = max(0, qt - 4)
                for kt in range(k0, qt + 1):
                    extra = []
                    if kt == qt - 4:
                        extra.append((identb, negwl))
                    if kt == qt:
                        extra.append((identb, negcau))
                    ps = score_tile(kT_s[:, 2 + g, kt * 128:(kt + 1) * 128], rq, extra)
                    pt = PT[pti[0] % 4]
                    pti[0] += 1
                    f.act(pt, ps, AF.Exp)
                    if dump and qt == 1 and g == 0:
                        f.dma("sp", dbg_d[2 + kt], pt)
                    for r in range(4):
                        f.matmul(OW[:, r * 65:(r + 1) * 65], pt[:, r * 128:(r + 1) * 128], va_s[:, kt, 2 + g, :], start=(kt == k0 and r == 0), stop=(kt == qt))
                for kt in range(qt + 1):
                    extra = [(etab[:, kt, :], nsT)]
                    if kt == qt:
                        extra.append((identb, negcau))
                    ps = score_tile(kT_s[:, g, kt * 128:(kt + 1) * 128], rq, extra)
                    pt = PT[pti[0] % 4]
                    pti[0] += 1
                    f.act(pt, ps, AF.Exp)
                    if dump and qt == 1 and g == 0:
                        f.dma("sp", dbg_d[kt], pt)
                        if kt == 0:
                            f.dma("sp", dbg_d[4], nsT)
                    for r in range(4):
                        f.matmul(OS_[:, r * 65:(r + 1) * 65], pt[:, r * 128:(r + 1) * 128], va_s[:, kt, g, :], start=(kt == 0 and r == 0), stop=(kt == qt))
                if dump and qt == 1 and g == 0:
                    for bi_, O_ in enumerate([OC, OS_, OW]):
                        dt_ = f.sb([128, 260], F32, "dbgt", stack=st)
                        f.copy("dve", dt_, O_[:, 0:260])
                        f.dma("sp", dbg2_d[bi_], dt_)
                gv = gt.v().rearrange("p (g r b) -> p g r b", g=2, r=4)
                for br, O in enumerate([OC, OS_, OW]):
                    ov_ = O[:, 0:260].rearrange("p (r c) -> p r c", r=4)
                    if br > 0:
                        f.ts("dve", rden[br], ov_[:, :, 64], 1e-30, None, ALU.max)
                        f.op("dve", lambda br=br: nc.vector.reciprocal(out=rden[br].ap, in_=rden[br].ap), [rden[br]], [rden[br]])
                    f.tt("dve", scl[br], rden[br], gv[:, g, :, br], ALU.mult)
                    oav = oa[:, g * 256:(g + 1) * 256].rearrange("p (r d) -> p r d", r=4)
                    sb_ = scl[br].v().unsqueeze(2).broadcast_to([128, 4, 64])
                    if br == 0:
                        f.tt("dve", oav, ov_[:, :, 0:64], sb_, ALU.mult)
                    else:
                        tm = tmpo[br % 2]
                        f.tt("dve", tm, ov_[:, :, 0:64], sb_, ALU.mult)
                        f.tt("pool", oav, oav, tm, ALU.add)
            ob = onb[qt % 2]
            f.copy("act", ob, oa)
            oT = onT[qt % 2]
            for i in range(4):
                f.transpose(PSB[:, 512 + i * 128:512 + (i + 1) * 128], ob[:, i * 128:(i + 1) * 128], identb)
            f.copy("dve", oT, PSB[:, 512:1024].rearrange("p (i t) -> p i t", i=4))
            f.dma("sp", onT_d.rearrange("(i p) s -> p i s", p=128)[:, :, qt * 128:(qt + 1) * 128], oT)
        f.barrier()
        st.close()
        if upto == 'P3':
            f.finish()
            return nc, hc

        st = ExitStack()
        pw = f.sb([128, 4, 128], BF16, "pw", stack=st)
        f.dma("pool", pw, W["pool_w"][l].rearrange("g i o -> i g o"))
        psc = f.sb([128, 4], F32, "psc", stack=st)
        f.dma("sp", psc, W["pool_scale"][l].rearrange("(g p) -> p g", p=128), allow_slow_non_contiguous=True)
        pfix = f.sb([128, 4, 16], F32, "pfix", stack=st)
        f.dma("sp", pfix, C["poolfix"])
        ub_b = [f.sb([128, 4, 528], BF16, "ub%d" % i, stack=st) for i in range(2)]
        wa = [f.sb([128, 528], F32, "wa%d" % i, stack=st) for i in range(2)]
        wb = [f.sb([128, 528], F32, "wb%d" % i, stack=st) for i in range(2)]
        pin = [f.sb([128, 512], BF16, "pin%d" % i, stack=st) for i in range(2)]
        opst = [f.sb([128, 4, 512], BF16, "opst%d" % i, stack=st) for i in range(2)]
        uTv = uT_d.rearrange("(i p) s -> p i s", p=128)
        for tb in range(NTB):
            ub = ub_b[tb % 2]
            f.dma("sp", ub, uTv[:, 0:4, tb * 512:tb * 512 + 528])
            os_ = opst[tb % 2]
            for g in range(4):
                e = "dve" if g % 2 == 0 else "pool"
                a, b = wa[g % 2], wb[g % 2]
                src = ub[:, g, :]
                sh = 1
                cur = None
                for step in range(g + 1):
                    dst = a if step % 2 == 0 else b
                    s_in = src if cur is None else cur
                    f.tt(e, dst[:, sh:528], s_in[:, sh:528], s_in[:, 0:528 - sh], ALU.add)
                    cur = dst
                    sh *= 2
                w_ = 2 ** (g + 1)
                if tb == 0:
                    f.tt(e, cur[:, 16:32], cur[:, 16:32], pfix[:, g, :], ALU.mult)
                f.stt("dve", pin[g % 2], cur[:, 16:528], 1.0 / w_, ub[:, g, 16:528], ALU.mult, ALU.subtract)
                ps = PS[g % 2]
                f.matmul(ps, pw[:, g, :], pin[g % 2])
                f.act(os_[:, g, :], ps, AF.Copy, scale=psc[:, g:g + 1])
            f.dma("sp", opT_d.rearrange("(i p) s -> p i s", p=128)[:, :, tb * 512:(tb + 1) * 512], os_)
        f.barrier()
        st.close()
        if upto == 'P4a':
            f.finish()
            return nc, hc

        st = ExitStack()
        lam_n = f.sb([32, 2, 64], F32, "lam_n", stack=st)
        f.dma("sp", lam_n[:, 0, :], W["ssm_lam_re"][l])
        f.dma("sp", lam_n[:, 1, :], W["ssm_lam_im"][l])
        lrT = f.sb([64, 32], F32, "lrT", stack=st)
        liT = f.sb([64, 32], F32, "liT", stack=st)
        f.transpose(PS[0][0:64, 0:32], lam_n[:, 0, :], identf[0:32, 0:32])
        f.copy("dve", lrT, PS[0][0:64, 0:32])
        f.transpose(PS[0][0:64, 32:64], lam_n[:, 1, :], identf[0:32, 0:32])
        f.copy("dve", liT, PS[0][0:64, 32:64])
        dtT = f.sb([64, 32], F32, "dtT", stack=st)
        f.dma("sp", dtT, W["ssm_log_dt"][l:l + 1, :].partition_broadcast(64))
        f.act(dtT, dtT, AF.Exp)
        sv = f.sb([128, 1], F32, "sv", stack=st)
        f.dma("sp", sv, C["svals"])
        tv = f.sb([64, 129], F32, "tv", stack=st)
        f.dma("sp", tv, C["tvals"])
        lrdt = f.sb([64, 32], F32, "lrdt", stack=st)
        th = f.sb([64, 32], F32, "th", stack=st)
        f.tt("dve", lrdt, lrT, dtT, ALU.mult)
        f.tt("dve", th, liT, dtT, ALU.mult)
        INV2PI = 1.0 / (2 * math.pi)

        def sincos(st2, arg, shape, sin_out, cos_out, e="dve"):
            ki = f.sb(shape, I32, stack=st2)
            kf = f.sb(shape, F32, stack=st2)
            r_ = f.sb(shape, F32, stack=st2)
            for (outp, shift) in ((sin_out, 0.0), (cos_out, math.pi / 2)):
                f.ts(e, kf, arg, shift, INV2PI, ALU.add, ALU.mult)
                f.copy(e, ki, kf)
                f.copy(e, kf, ki)
                f.ts(e, r_, arg, shift, None, ALU.add)
                f.stt(e, r_, kf, -2 * math.pi, r_, ALU.mult, ALU.add)
                f.ts(e, r_, r_, -3.1415925, 3.1415925, ALU.max, ALU.min)
                f.act(outp, r_, AF.Sin)

        Dpr = f.sb([64, 32, 129], F32, "Dpr", stack=st)
        Dpi = f.sb([64, 32, 129], F32, "Dpi", stack=st)
        st2 = ExitStack()
        argp = f.sb([64, 32, 129], F32, stack=st2)
        magp = f.sb([64, 32, 129], F32, stack=st2)
        tvb = tv.v().unsqueeze(1).broadcast_to([64, 32, 129])
        f.tt("dve", argp, th.v().unsqueeze(2).broadcast_to([64, 32, 129]), tvb, ALU.mult)
        f.tt("pool", magp, lrdt.v().unsqueeze(2).broadcast_to([64, 32, 129]), tvb, ALU.mult)
        f.act(magp, magp, AF.Exp)
        sincos(st2, argp, [64, 32, 129], Dpi, Dpr)
        f.tt("dve", Dpr, Dpr, magp, ALU.mult)
        f.tt("dve", Dpi, Dpi, magp, ALU.mult)
        f.barrier()
        st2.close()
        if P4BSTOP == 1:
            f.finish()
            return nc, hc
        Dmr = f.sb([128, 2048], F32, "Dmr", stack=st)
        Dmi = f.sb([128, 2048], F32, "Dmi", stack=st)
        st2 = ExitStack()
        lrow = f.sb([128, 2, 2048], F32, stack=st2)
        f.dma("sp", lrow[:, 0, :], W["ssm_lam_re"][l:l + 1].rearrange("o g p -> o (g p)").partition_broadcast(128))
        f.dma("sp", lrow[:, 1, :], W["ssm_lam_im"][l:l + 1].rearrange("o g p -> o (g p)").partition_broadcast(128))
        dtr = f.sb([128, 32], F32, stack=st2)
        f.dma("sp", dtr, W["ssm_log_dt"][l:l + 1, :].partition_broadcast(128))
        f.act(dtr, dtr, AF.Exp)
        f.ts("dve", dtr, dtr, sv[:, 0:1], None, ALU.mult)
        dtb = dtr.v().unsqueeze(2).broadcast_to([128, 32, 64])
        argm = f.sb([128, 2048], F32, stack=st2)
        magm = f.sb([128, 2048], F32, stack=st2)
        f.tt("dve", argm.v().rearrange("p (g q) -> p g q", g=32), lrow[:, 1, :].rearrange("p (g q) -> p g q", g=32), dtb, ALU.mult)
        f.tt("pool", magm.v().rearrange("p (g q) -> p g q", g=32), lrow[:, 0, :].rearrange("p (g q) -> p g q", g=32), dtb, ALU.mult)
        f.act(magm, magm, AF.Exp, scale=-1.0)
        sincos(st2, argm, [128, 2048], Dmi, Dmr)
        f.tt("dve", Dmr, Dmr, magm, ALU.mult)
        f.ts("dve", Dmi, Dmi, -1.0, None, ALU.mult)
        f.tt("dve", Dmi, Dmi, magm, ALU.mult)
        f.barrier()
        st2.close()
        if P4BSTOP == 2:
            f.finish()
            return nc, hc
        BD = f.sb([128, 4, 2, 512], BF16, "BD", stack=st)
        CTp = f.sb([64, 2, 32, 128], BF16, "CTp", stack=st)
        st2 = ExitStack()
        arb = f.sb([64, 32], F32, stack=st2)
        aib = f.sb([64, 32], F32, stack=st2)
        f.copy("dve", arb, Dpr[:, :, 1])
        f.copy("dve", aib, Dpi[:, :, 1])
        den = f.sb([64, 32], F32, stack=st2)
        t1 = f.sb([64, 32], F32, stack=st2)
        t2 = f.sb([64, 32], F32, stack=st2)
        crr = f.sb([64, 32], F32, stack=st2)
        cii = f.sb([64, 32], F32, stack=st2)
        f.tt("dve", den, lrT, lrT, ALU.mult)
        f.tt("dve", t1, liT, liT, ALU.mult)
        f.tt("dve", den, den, t1, ALU.add)
        f.op("dve", lambda: nc.vector.reciprocal(out=den.ap, in_=den.ap), [den], [den])
        f.ts("dve", arb, arb, -1.0, None, ALU.add)
        f.tt("dve", t1, arb, lrT, ALU.mult)
        f.tt("dve", t2, aib, liT, ALU.mult)
        f.tt("dve", crr, t1, t2, ALU.add)
        f.tt("dve", crr, crr, den, ALU.mult)
        f.tt("dve", t1, aib, lrT, ALU.mult)
        f.tt("dve", t2, arb, liT, ALU.mult)
        f.tt("dve", cii, t1, t2, ALU.subtract)
        f.tt("dve", cii, cii, den, ALU.mult)
        bre = f.sb([64, 32, 16], F32, stack=st2)
        bim = f.sb([64, 32, 16], F32, stack=st2)
        f.dma("sp", bre, W["ssm_b_re"][l].rearrange("g p h -> p g h"))
        f.dma("sp", bim, W["ssm_b_im"][l].rearrange("g p h -> p g h"))
        crb = crr.v().unsqueeze(2).broadcast_to([64, 32, 16])
        cib = cii.v().unsqueeze(2).broadcast_to([64, 32, 16])
        bbr = f.sb([64, 32, 16], F32, stack=st2)
        bbi = f.sb([64, 32, 16], F32, stack=st2)
        tb1 = f.sb([64, 32, 16], F32, stack=st2)
        f.tt("dve", bbr, bre, crb, ALU.mult)
        f.tt("dve", tb1, bim, cib, ALU.mult)
        f.tt("dve", bbr, bbr, tb1, ALU.subtract)
        f.tt("dve", bbi, bim, crb, ALU.mult)
        f.tt("dve", tb1, bre, cib, ALU.mult)
        f.tt("dve", bbi, bbi, tb1, ALU.add)
        gmask = f.sb([128, 8], F32, stack=st2)
        f.dma("sp", gmask, C["gmask"])
        for o in range(4):
            for ri, bb in enumerate((bbr, bbi)):
                f.transpose(PS[ri][:, 0:64], bb[:, 8 * o:8 * o + 8, :].rearrange("p g h -> p (g h)"), identf[0:64, 0:64])
                for gg in range(8):
                    f.ts("dve", BD[:, o, ri, gg * 64:(gg + 1) * 64], PS[ri][:, 0:64], gmask[:, gg:gg + 1], None, ALU.mult)
        f.memset("pool", CTp, 0.0)
        cn = f.sb([128, 4, 2, 64], F32, stack=st2)
        f.dma("sp", cn[:, :, 0, :], W["ssm_c_re"][l].rearrange("(o g) h p -> (g h) o p", o=4))
        f.dma("sp", cn[:, :, 1, :], W["ssm_c_im"][l].rearrange("(o g) h p -> (g h) o p", o=4))
        for o in range(4):
            for ri in range(2):
                f.transpose(PS[2 + ri][0:64, 0:128], cn[:, o, ri, :], identf)
                for gg in range(8):
                    g_ = 8 * o + gg
                    if ri == 0:
                        f.copy("dve", CTp[:, 0, g_, gg * 16:(gg + 1) * 16], PS[2][0:64, gg * 16:(gg + 1) * 16])
                    else:
                        f.ts("dve", CTp[:, 1, g_, gg * 16:(gg + 1) * 16], PS[3][0:64, gg * 16:(gg + 1) * 16], -1.0, None, ALU.mult)
        f.barrier()
        st2.close()
        if P4BSTOP == 3:
            f.finish()
            return nc, hc
        wglu = f.sb([128, 4, 512], BF16, "wglu", stack=st)
        f.dma("pool", wglu, W["ssm_w_glu"][l].rearrange("(i p) o -> p i o", p=128))
        bglu = f.sb([128, 4], F32, "bglu", stack=st)
        f.dma("sp", bglu, W["ssm_b_glu"][l].rearrange("(g p) -> p g", p=128), allow_slow_non_contiguous=True)
        dsk = f.sb([128, 4], F32, "dsk", stack=st)
        f.dma("sp", dsk, W["ssm_d"][l].rearrange("(g p) -> p g", p=128), allow_slow_non_contiguous=True)
        trib = f.sb([128, 128], BF16, "trib", stack=st)
        f.dma("sp", trib, C["tri"])
        carry = f.sb([64, 2, 32], F32, "carry", stack=st)
        f.memset("dve", carry, 0.0)
        gcl = f.sb([64, 2, 32], F32, "gcl", stack=st)
        us_b = [f.sb([128, 4, 128], BF16, "usb%d" % i, stack=st) for i in range(2)]
        Zr = [f.sb([128, 512], BF16, "Zr%d" % i, stack=st) for i in range(2)]
        Zi = [f.sb([128, 512], BF16, "Zi%d" % i, stack=st) for i in range(2)]
        zt1 = [f.sb([128, 512], F32, "zt1%d" % i, stack=st) for i in range(2)]
        zt2 = [f.sb([128, 512], F32, "zt2%d" % i, stack=st) for i in range(2)]
        GCr = [f.sb([64, 8, 128], F32, "GCr%d" % i, stack=st) for i in range(2)]
        GCi = [f.sb([64, 8, 128], F32, "GCi%d" % i, stack=st) for i in range(2)]
        ht1 = [f.sb([64, 8, 128], F32, "ht1%d" % i, stack=st) for i in range(2)]
        ht2 = [f.sb([64, 8, 128], F32, "ht2%d" % i, stack=st) for i in range(2)]
        Hr = [f.sb([64, 8, 128], BF16, "Hr%d" % i, stack=st) for i in range(2)]
        Hi = [f.sb([64, 8, 128], BF16, "Hi%d" % i, stack=st) for i in range(2)]
        ysb = f.sb([128, 4, 128], F32, "ysb", stack=st)
        zT = [f.sb([128, 4, 128], BF16, "zT%d" % i, stack=st) for i in range(2)]
        sg = f.sb([128, 128], F32, "sg", stack=st)
        osst = [f.sb([128, 4, 128], BF16, "osst%d" % i, stack=st) for i in range(2)]
        ct1 = f.sb([64, 32], F32, "ct1", stack=st)
        ct2 = f.sb([64, 32], F32, "ct2", stack=st)
        usv = uT_d.rearrange("(i p) s -> p i s", p=128)
        if ARM == "P4b":
            f.arm()
        for ch in range(0 if SKIP4B else NT):
            f.maybe_switch()
            us = us_b[ch % 2]
            f.dma("sp", us, usv[:, 4:8, 16 + ch * 128:16 + (ch + 1) * 128])
            zt_ = zT[ch % 2]
            for o in range(4):
                k = o % 2
                f.matmul(PS[0], us[:, o, :], BD[:, o, 0, :])
                f.matmul(PS[1], us[:, o, :], BD[:, o, 1, :])
                dmr = Dmr[:, o * 512:(o + 1) * 512]
                dmi = Dmi[:, o * 512:(o + 1) * 512]
                f.tt("dve", zt1[k], PS[0], dmr, ALU.mult)
                f.tt("dve", zt2[k], PS[1], dmi, ALU.mult)
                f.tt("pool", Zr[k], zt1[k], zt2[k], ALU.subtract)
                f.tt("dve", zt1[k], PS[0], dmi, ALU.mult)
                f.tt("dve", zt2[k], PS[1], dmr, ALU.mult)
                f.tt("pool", Zi[k], zt1[k], zt2[k], ALU.add)
                for gg in range(8):
                    f.matmul(PS[2 + gg // 4][0:64, (gg % 4) * 128:(gg % 4 + 1) * 128], Zr[k][:, gg * 64:(gg + 1) * 64], trib)
                    f.matmul(PS[4 + gg // 4][0:64, (gg % 4) * 128:(gg % 4 + 1) * 128], Zi[k][:, gg * 64:(gg + 1) * 64], trib)
                for hh in range(2):
                    cb_r = carry[:, 0, 8 * o + 4 * hh:8 * o + 4 * hh + 4].unsqueeze(2).broadcast_to([64, 4, 128])
                    cb_i = carry[:, 1, 8 * o + 4 * hh:8 * o + 4 * hh + 4].unsqueeze(2).broadcast_to([64, 4, 128])
                    f.tt("dve", GCr[k][:, 4 * hh:4 * hh + 4, :], PS[2 + hh][0:64, :].rearrange("p (g t) -> p g t", g=4), cb_r, ALU.add)
                    f.tt("dve", GCi[k][:, 4 * hh:4 * hh + 4, :], PS[4 + hh][0:64, :].rearrange("p (g t) -> p g t", g=4), cb_i, ALU.add)
                f.copy("pool", gcl[:, 0, 8 * o:8 * o + 8], GCr[k][:, :, 127])
                f.copy("pool", gcl[:, 1, 8 * o:8 * o + 8], GCi[k][:, :, 127])
                dpr = Dpr[:, 8 * o:8 * o + 8, 0:128]
                dpi = Dpi[:, 8 * o:8 * o + 8, 0:128]
                f.tt("pool", ht1[k], GCr[k], dpr, ALU.mult)
                f.tt("dve", ht2[k], GCi[k], dpi, ALU.mult)
                f.tt("pool", Hr[k], ht1[k], ht2[k], ALU.subtract)
                f.tt("pool", ht1[k], GCr[k], dpi, ALU.mult)
                f.tt("dve", ht2[k], GCi[k], dpr, ALU.mult)
                f.tt("pool", Hi[k], ht1[k], ht2[k], ALU.add)
                yp = PS[6]
                for gg in range(8):
                    f.matmul(yp[:, 0:128], CTp[:, 0, 8 * o + gg, :], Hr[k][:, gg, :], start=(gg == 0), stop=False)
                    f.matmul(yp[:, 0:128], CTp[:, 1, 8 * o + gg, :], Hi[k][:, gg, :], start=False, stop=(gg == 7))
                f.ts("pool", ysb[:, o, :], us[:, o, :], dsk[:, o:o + 1], None, ALU.mult)
                f.tt("dve", ysb[:, o, :], yp[:, 0:128], ysb[:, o, :], ALU.add)
                f.act(zt_[:, o, :], ysb[:, o, :], AF.Gelu)
            l128r = Dpr[:, :, 128]
            l128i = Dpi[:, :, 128]
            f.tt("dve", ct1, gcl[:, 0, :], l128r, ALU.mult)
            f.tt("dve", ct2, gcl[:, 1, :], l128i, ALU.mult)
            f.tt("dve", carry[:, 0, :], ct1, ct2, ALU.subtract)
            f.tt("dve", ct1, gcl[:, 0, :], l128i, ALU.mult)
            f.tt("dve", ct2, gcl[:, 1, :], l128r, ALU.mult)
            f.tt("dve", carry[:, 1, :], ct1, ct2, ALU.add)
            oss = osst[ch % 2]
            for co in range(4):
                gp = PS[0] if co % 2 == 0 else PS[1]
                for ci_ in range(4):
                    f.matmul(gp[:, 0:128], wglu[:, ci_, co * 128:(co + 1) * 128], zt_[:, ci_, :], start=(ci_ == 0), stop=(ci_ == 3))
                f.act(sg, gp[:, 0:128], AF.Sigmoid, bias=bglu[:, co:co + 1])
                f.tt("pool", oss[:, co, :], zt_[:, co, :], sg, ALU.mult)
            f.dma("sp", osT_d.rearrange("(i p) s -> p i s", p=128)[:, :, ch * 128:(ch + 1) * 128], oss)
        f.barrier()
        st.close()
        if upto == 'P4b':
            f.finish()
            return nc, hc

        st = ExitStack()
        if SKIP3 or SKIP4B:
            zz = f.sb([128, 4, S], BF16, "zz", stack=st)
            f.memset("dve", zz, 0.0)
            if SKIP3:
                f.dma("sp", onT_d.rearrange("(i p) s -> p i s", p=128), zz)
            if SKIP4B:
                f.dma("sp", osT_d.rearrange("(i p) s -> p i s", p=128), zz)
            f.barrier()
        wbg = f.sb([128, 8, 3072], BF16, "wbg", stack=st)
        for kt in range(8):
            f.dma("pool", wbg[:, kt, :], wv[:, kt, C_BG:INW])
        wup = f.sb([128, 3, 4, D], BF16, "wup", stack=st)
        for bi, nm_ in enumerate(["w_up_nsa", "w_up_pool", "w_up_ssm"]):
            f.dma("pool", wup[:, bi], W[nm_][l].rearrange("(i p) o -> p i o", p=128))
        wo = f.sb([128, 8, D], BF16, "wo", stack=st)
        f.dma("pool", wo, W["w_out"][l].rearrange("(k p) o -> p k o", p=128))
        g1 = f.sb([128, D], F32, "g1", stack=st)
        b1_ = f.sb([128, D], F32, "b1_", stack=st)
        f.dma("sp", g1, W["ln1_g"][l:l + 1, :].partition_broadcast(128))
        f.dma("sp", b1_, W["ln1_b"][l:l + 1, :].partition_broadcast(128))
        xs_b = [f.sb([128, 8, 512], BF16, "p5x%d" % i, stack=st) for i in range(2)]
        ob_b = [f.sb([128, 3, 4, 512], BF16, "p5o%d" % i, stack=st) for i in range(2)]
        xr_b = [f.sb([128, D], F32, "p5r%d" % i, stack=st) for i in range(2)]
        sig = [f.sb([128, 512], F32, "sig%d" % i, stack=st) for i in range(2)]
        mg = [f.sb([128, 512], F32, "mg%d" % i, stack=st) for i in range(2)]
        tm5 = [f.sb([128, 512], F32, "tm5%d" % i, stack=st) for i in range(2)]
        mT = f.sb([128, 8, 512], BF16, "mT", stack=st)
        hb = [f.sb([128, D], F32, "hb%d" % i, stack=st) for i in range(2)]
        x1b = [f.sb([128, D], F32, "x1b%d" % i, stack=st) for i in range(2)]
        srcs = [onT_d, opT_d, osT_d]
        cnt = 0
        for tb in range(NTB):
            f.maybe_switch()
            xs = xs_b[tb % 2]
            ob = ob_b[tb % 2]
            tsl = slice(tb * 512, (tb + 1) * 512)
            f.dma("sp", xs, xT_d.rearrange("(k p) s -> p k s", p=128)[:, :, tsl])
            for bi in range(3):
                f.dma("sp", ob[:, bi], srcs[bi].rearrange("(i p) s -> p i s", p=128)[:, :, tsl])
            for co in range(8):
                m_ = mg[co % 2]
                for bi in range(3):
                    pg = PS[cnt % 2]
                    pu = PS[2 + cnt % 2]
                    cnt += 1
                    for kt in range(8):
                        f.matmul(pg, wbg[:, kt, bi * 1024 + co * 128: bi * 1024 + (co + 1) * 128], xs[:, kt, :], start=(kt == 0), stop=(kt == 7))
                    for i in range(4):
                        f.matmul(pu, wup[:, bi, i, co * 128:(co + 1) * 128], ob[:, bi, i, :], start=(i == 0), stop=(i == 3))
                    sg_ = sig[cnt % 2]
                    f.act(sg_, pg, AF.Sigmoid)
                    if bi == 0:
                        f.tt("dve", m_, pu, sg_, ALU.mult)
                    elif bi == 1:
                        t_ = tm5[cnt % 2]
                        f.tt("dve", t_, pu, sg_, ALU.mult)
                        f.tt("pool", m_, m_, t_, ALU.add)
                    else:
                        t_ = tm5[cnt % 2]
                        f.tt("dve", t_, pu, sg_, ALU.mult)
                        f.tt("pool", mT[:, co, :], m_, t_, ALU.add)
            for sub in range(4):
                t0 = tb * 512 + sub * 128
                xr = xr_b[sub % 2]
                f.dma("sp", xr, xsrc[t0:t0 + 128, :])
                h_ = hb[sub % 2]
                for hf in range(2):
                    po = PS[4 + hf]
                    for co in range(8):
                        f.matmul(po, mT[:, co, sub * 128:(sub + 1) * 128], wo[:, co, hf * 512:(hf + 1) * 512], start=(co == 0), stop=(co == 7))
                    f.stt("dve", h_[:, hf * 512:(hf + 1) * 512], po, 1.0 / ALPHA, xr[:, hf * 512:(hf + 1) * 512], ALU.mult, ALU.add)
                x1 = x1b[sub % 2]
                layer_norm(st, h_, g1, b1_, x1, eps=1e-5 / (ALPHA * ALPHA))
                f.dma("sp", x1_d[t0:t0 + 128, :], x1)
        f.barrier()
        st.close()
        if upto == 'P5':
            f.finish()
            return nc, hc

        st = ExitStack()
        TB = min(S, 2048)
        NSUB = TB // 128
        wr = f.sb([128, 8, 36], F32, "wr", stack=st)
        f.dma("sp", wr[:, :, 0:4], W["router_w_grp"][l].rearrange("(k p) n -> p k n", p=128))
        f.dma("sp", wr[:, :, 4:36], W["router_w_exp"][l].rearrange("(k p) n -> p k n", p=128))
        wrh = f.sb([128, 8, 36], BF16, "wrh", stack=st)
        wrl = f.sb([128, 8, 36], BF16, "wrl", stack=st)
        wrt = f.sb([128, 8, 36], F32, "wrt", stack=st)
        f.copy("dve", wrh, wr)
        f.copy("dve", wrt, wrh)
        f.tt("dve", wrt, wr, wrt, ALU.subtract)
        f.copy("dve", wrl, wrt)
        xsplit = [f.sb([128, 8, 128], F32, "xsp0", stack=st), f.sb([128, 8, 128], BF16, "xsp1", stack=st)]
        xb16 = f.sb([128, 8, 128], BF16, "xb16", stack=st)
        br_ = f.sb([128, 36], F32, "br_", stack=st)
        f.dma("sp", br_[:, 0:4], W["router_b_grp"][l:l + 1, :].partition_broadcast(128))
        f.dma("sp", br_[:, 4:36], W["router_b_exp"][l:l + 1, :].partition_broadcast(128))
        g2 = f.sb([128, D], F32, "g2", stack=st)
        b2_ = f.sb([128, D], F32, "b2_", stack=st)
        f.dma("sp", g2, W["ln2_g"][l:l + 1, :].partition_broadcast(128))
        f.dma("sp", b2_, W["ln2_b"][l:l + 1, :].partition_broadcast(128))
        x1T = f.sb([128, 8, TB], BF16, "x1T", stack=st)
        acc = f.sb([128, NSUB, D], F32, "acc", stack=st)
        gw = f.sb([128, NSUB, 32], F32, "gw", stack=st)
        x1f = [f.sb([128, D], F32, "x1f%d" % i, stack=st) for i in range(2)]
        x1Tf = [f.sb([128, 8, 128], F32, "x1Tf%d" % i, stack=st) for i in range(2)]
        wgb = [f.sb([128, 8, FF], BF16, "wgb%d" % i, stack=st) for i in range(2)]
        wub = [f.sb([128, 8, FF], BF16, "wub%d" % i, stack=st) for i in range(2)]
        wdb = [f.sb([128, 4, D], BF16, "wdb%d" % i, stack=st) for i in range(2)]
        sil = [f.sb([128, 512], F32, "sil%d" % i, stack=st) for i in range(2)]
        hT_ = [f.sb([128, 4, 512], BF16, "hT_%d" % i, stack=st) for i in range(2)]
        ytmp = [f.sb([128, 512], F32, "ytmp%d" % i, stack=st) for i in range(2)]
        lg = f.sb([128, 36], F32, "lg", stack=st)
        r4 = f.sb([128, 8], F32, "r4", stack=st)
        oh4 = f.sb([128, 4], F32, "oh4", stack=st)
        sl8 = f.sb([128, 8], F32, "sl8", stack=st)
        sl16 = f.sb([128, 16], F32, "sl16", stack=st)
        f.memset("dve", sl16, -1.0e30)
        e8 = f.sb([128, 8], F32, "e8", stack=st)
        mk1 = f.sb([128, 8], F32, "mk1", stack=st)
        mk2 = f.sb([128, 8], F32, "mk2", stack=st)
        w8 = f.sb([128, 8], F32, "w8", stack=st)
        if ARM == "P6":
            f.arm()
        for blk in range(S // TB):
            for sub in range(NSUB):
                t0 = blk * TB + sub * 128
                xf = x1f[sub % 2]
                f.dma("sp", xf, x1_d[t0:t0 + 128, :])
                xtf = x1Tf[sub % 2]
                for hh in range(2):
                    ps = PS[hh]
                    for k4 in range(4):
                        kt = hh * 4 + k4
                        f.transpose(ps[:, k4 * 128:(k4 + 1) * 128], xf[:, kt * 128:(kt + 1) * 128], identf)
                    f.act(xtf[:, hh * 4:(hh + 1) * 4, :], ps, AF.Copy)
                    f.copy("pool", x1T[:, hh * 4:(hh + 1) * 4, sub * 128:(sub + 1) * 128], xtf[:, hh * 4:(hh + 1) * 4, :])
                xh = x1T[:, :, sub * 128:(sub + 1) * 128]
                xhf = xsplit[0]
                f.copy("pool", xhf, xh)
                f.tt("pool", xhf, xtf, xhf, ALU.subtract)
                f.copy("pool", xsplit[1], xhf)
                pl = PS[2]
                n_ = 0
                for (xa, wa_) in ((xh, wrh), (xh, wrl), (xsplit[1], wrh)):
                    for kt in range(8):
                        f.matmul(pl[:, 0:36], xa[:, kt, :], wa_[:, kt, :], start=(n_ == 0), stop=(n_ == 23))
                        n_ += 1
                f.tt("dve", lg, pl[:, 0:36], br_, ALU.add)
                f.tt("dve", r4[:, 1:3], lg[:, 0:2], lg[:, 2:4], ALU.max)
                f.tt("dve", r4[:, 0:1], r4[:, 1:2], r4[:, 2:3], ALU.max)
                f.ts("dve", oh4, lg[:, 0:4], r4[:, 0:1], None, ALU.is_ge)
                f.ts("dve", r4[:, 1:5], lg[:, 0:4], r4[:, 0:1], None, ALU.subtract)
                f.act(r4[:, 1:5], r4[:, 1:5], AF.Exp)
                f.tt("dve", r4[:, 1:3], r4[:, 1:3], r4[:, 3:5], ALU.add)
                f.tt("dve", r4[:, 5:6], r4[:, 1:2], r4[:, 2:3], ALU.add)
                f.op("dve", lambda: nc.vector.reciprocal(out=r4.ap[:, 6:7], in_=r4.ap[:, 5:6]), [r4], [r4])
                f.ts("dve", sl8, lg[:, 4:12], oh4[:, 0:1], None, ALU.mult)
                for g_ in range(1, 4):
                    f.stt("dve", sl8, lg[:, 4 + 8 * g_:12 + 8 * g_], oh4[:, g_:g_ + 1], sl8, ALU.mult, ALU.add)
                f.copy("dve", sl16[:, 0:8], sl8)
                f.op("dve", lambda: nc.vector.max(out=e8.ap, in_=sl16.ap), [sl16], [e8])
                f.ts("dve", mk1, sl8, e8[:, 0:1], None, ALU.is_ge)
                f.ts("dve", mk2, sl8, e8[:, 1:2], None, ALU.is_ge)
                f.tt("dve", mk2, mk2, mk1, ALU.subtract)
                f.tt("dve", r4[:, 0:1], e8[:, 1:2], e8[:, 0:1], ALU.subtract)
                f.act(r4[:, 0:1], r4[:, 0:1], AF.Exp)
                f.ts("dve", r4[:, 1:2], r4[:, 0:1], 1.0, None, ALU.add)
                f.op("dve", lambda: nc.vector.reciprocal(out=r4.ap[:, 1:2], in_=r4.ap[:, 1:2]), [r4], [r4])
                f.tt("dve", r4[:, 1:2], r4[:, 1:2], r4[:, 6:7], ALU.mult)
                f.tt("dve", r4[:, 2:3], r4[:, 1:2], r4[:, 0:1], ALU.mult)
                f.ts("dve", w8, mk1, r4[:, 1:2], None, ALU.mult)
                f.stt("dve", w8, mk2, r4[:, 2:3], w8, ALU.mult, ALU.add)
                for g_ in range(4):
                    f.ts("dve", gw[:, sub, 8 * g_:8 * g_ + 8], w8, oh4[:, g_:g_ + 1], None, ALU.mult)
            if P6STOP == 1:
                f.finish()
                return nc, hc
            def load_expert(ee):
                f.dma("pool", wgb[ee % 2], W["moe_w_gate"][l, ee].rearrange("(k p) n -> p k n", p=128))
                f.dma("pool", wub[ee % 2], W["moe_w_up"][l, ee].rearrange("(k p) n -> p k n", p=128))
                f.dma("pool", wdb[ee % 2], W["moe_w_down"][l, ee].rearrange("(k p) n -> p k n", p=128))

            load_expert(0)
            for e_ in range(NEXP):
                wg_, wu_, wd_ = wgb[e_ % 2], wub[e_ % 2], wdb[e_ % 2]
                if e_ + 1 < NEXP:
                    load_expert(e_ + 1)
                def gate_up(cc):
                    hT = hT_[cc % 2]
                    for fi in range(4):
                        pg = PS[(fi % 2)]
                        pu = PS[2 + (fi % 2)]
                        for kt in range(8):
                            f.matmul(pg, wg_[:, kt, fi * 128:(fi + 1) * 128], x1T[:, kt, cc * 512:(cc + 1) * 512], start=(kt == 0), stop=(kt == 7))
                        for kt in range(8):
                            f.matmul(pu, wu_[:, kt, fi * 128:(fi + 1) * 128], x1T[:, kt, cc * 512:(cc + 1) * 512], start=(kt == 0), stop=(kt == 7))
                        s_ = sil[fi % 2]
                        f.act(s_, pg, AF.Silu)
                        f.tt("dve", hT[:, fi, :], pu, s_, ALU.mult)

                def down(cc):
                    hT = hT_[cc % 2]
                    for s4 in range(4):
                        sub = cc * 4 + s4
                        for hf in range(2):
                            py = PS[4 + (s4 * 2 + hf) % 3]
                            for fi in range(4):
                                f.matmul(py, hT[:, fi, s4 * 128:(s4 + 1) * 128], wd_[:, fi, hf * 512:(hf + 1) * 512], start=(fi == 0), stop=(fi == 3))
                            a_ = acc[:, sub, hf * 512:(hf + 1) * 512]
                            gsc = gw[:, sub, e_:e_ + 1]
                            if e_ == 0:
                                f.ts("dve", a_, py, gsc, None, ALU.mult)
                            elif (s4 * 2 + hf) % 2 == 0:
                                f.stt("dve", a_, py, gsc, a_, ALU.mult, ALU.add)
                            else:
                                yt = ytmp[s4 % 2]
                                f.act(yt, py, AF.Copy, scale=gsc)
                                f.tt("pool", a_, a_, yt, ALU.add)

                prev_cc = None
                for cc in range(TB // 512):
                    gate_up(cc)
                    if prev_cc is not None:
                        down(prev_cc)
                    prev_cc = cc
                down(prev_cc)
            if P6STOP == 2:
                f.finish()
                return nc, hc
            for sub in range(NSUB):
                t0 = blk * TB + sub * 128
                xf = x1f[sub % 2]
                f.dma("sp", xf, x1_d[t0:t0 + 128, :])
                f.stt("dve", acc[:, sub, :], xf, ALPHA, acc[:, sub, :], ALU.mult, ALU.add)
                x2 = x1Tf[sub % 2].v().rearrange("p k t -> p (k t)")
                layer_norm(st, acc[:, sub, :], g2, b2_, x2)
                f.dma("sp", xdst[t0:t0 + 128, :], x2)
        f.barrier()
        st.close()
        if upto == 'P6':
            f.finish()
            return nc, hc

    f.finish()
    print('build done: ninst', f.ninst, 'nsem', f.nsem, {kk: len(v) for kk, v in f.semlist.items() if len(v) > 1})
    return nc, hc


_CACHE = {}


def _get(S, L):
    key = (S, L)
    if key not in _CACHE:
        _CACHE[key] = build(S, L)
    return _CACHE[key]


def kernel(**inputs):
    x = np.asarray(inputs["x"], dtype=np.float32)
    B, S, _ = x.shape
    L = inputs["w_in"].shape[0]
    nc, hc = _get(S, L)
    base = {k: np.ascontiguousarray(np.asarray(inputs[k], dtype=np.float32)) for k in WNAMES}
    for k, v in hc.items():
        base["c_" + k] = np.ascontiguousarray(v)
    ncores = B
    in_maps = []
    for c in range(ncores):
        m = dict(base)
        m["x"] = np.ascontiguousarray(x[c % B])
        in_maps.append(m)
    res = run_bass_kernel_spmd(nc, in_maps, core_ids=list(range(ncores)))
    out = np.stack([np.asarray(res.results[c]["out"]) for c in range(B)], axis=0)
    return out.astype(np.float32)
```
